# Optimizing a Trainium2 kernel written in Bass

```python
import jax, jax.numpy as jnp
from jax import lax
import numpy as np

D_MODEL = 1024
BATCH = 2
SEQ = 8192
DEPTH = 1

GRID_W = 64
CTX_LEN = 256
MLA_HEADS = 8
MLA_NOPE = 64
MLA_ROPE = 32
MLA_QK = MLA_NOPE + MLA_ROPE
MLA_V = 64
Q_LORA = 384
KV_LORA = 256
NA_HEADS = 8
NA_DIM = 64
NA_WIN_H = 8
NA_WIN_W = 16
MIX_WIDTH = MLA_HEADS * MLA_V + NA_HEADS * NA_DIM
Q_COLS = Q_LORA + NA_HEADS * NA_DIM
KV_COLS = KV_LORA + MLA_ROPE + 2 * NA_HEADS * NA_DIM
N_GROUPS = 4
EXPERTS_PER_GROUP = 4
N_EXPERTS = N_GROUPS * EXPERTS_PER_GROUP
TOP_K_IN_GROUP = 2
D_EXPERT = 512
Q_BLOCK = 128
ROPE_THETA = 10000.0
NORM_EPS = 1e-6
MASK_VALUE = -1e30

kernel_name = 'hybrid_mla_natten_hmoe_dit'


def rmsnorm(x, g):
    xf = x.astype(jnp.float32)
    y = xf * lax.rsqrt(jnp.mean(xf * xf, axis=-1, keepdims=True) + NORM_EPS)
    return (y * g.astype(jnp.float32)).astype(x.dtype)


def adaln(cond, w_mod, b_mod):
    return jnp.split(jax.nn.silu(cond) @ w_mod + b_mod, 6, axis=-1)


def modulate(h, shift, scale):
    return h * (1 + scale) + shift


def axial_rope_tables(row, col, dtype):
    half = MLA_ROPE // 2
    inv_freq = ROPE_THETA ** (-jnp.arange(0, half, 2, dtype=jnp.float32) / half)
    ang = jnp.concatenate([row.astype(jnp.float32)[:, None] * inv_freq,
                           col.astype(jnp.float32)[:, None] * inv_freq], axis=-1)
    return jnp.cos(ang).astype(dtype), jnp.sin(ang).astype(dtype)


def apply_rope(x, cos, sin):
    x1, x2 = jnp.split(x, 2, axis=-1)
    c, s = cos[:, None, :], sin[:, None, :]
    return jnp.concatenate([x1 * c - x2 * s, x1 * s + x2 * c], axis=-1)


def mixer_queries(pq, q_a_g, w_uq):
    B, L, _ = pq.shape
    q_lat, na_q = jnp.split(pq, [Q_LORA], axis=-1)
    q_mla = (rmsnorm(q_lat, q_a_g) @ w_uq).reshape(B, L, MLA_HEADS, MLA_QK)
    return q_mla, na_q.reshape(B, L, NA_HEADS, NA_DIM)


def mixer_keys_values(pkv, kv_a_g, w_ukv):
    B, L, _ = pkv.shape
    kv_lat, k_rope, na_k, na_v = jnp.split(
        pkv, [KV_LORA, KV_LORA + MLA_ROPE, KV_LORA + MLA_ROPE + NA_HEADS * NA_DIM], axis=-1)
    kv = (rmsnorm(kv_lat, kv_a_g) @ w_ukv).reshape(B, L, MLA_HEADS, MLA_NOPE + MLA_V)
    k_nope, v_mla = jnp.split(kv, [MLA_NOPE], axis=-1)
    return (k_nope, k_rope, v_mla,
            na_k.reshape(B, L, NA_HEADS, NA_DIM), na_v.reshape(B, L, NA_HEADS, NA_DIM))


def mla_keys(k_nope, k_rope_b):
    return jnp.concatenate([k_nope, jnp.broadcast_to(k_rope_b, k_nope.shape[:3] + (MLA_ROPE,))], axis=-1)


def dense_attention(q, k, v):
    s = jnp.einsum('bqhd,bkhd->bhqk', q, k).astype(jnp.float32) * (q.shape[-1] ** -0.5)
    p = jax.nn.softmax(s, axis=-1).astype(v.dtype)
    return jnp.einsum('bhqk,bkhd->bqhd', p, v)


def mla_latent_attention(q, k, v, k_ctx, v_ctx):
    B, N, H, Dq = q.shape
    k_all = jnp.concatenate([k_ctx, k], axis=1)
    v_all = jnp.concatenate([v_ctx, v], axis=1)
    q_blocks = jnp.moveaxis(q.reshape(B, N // Q_BLOCK, Q_BLOCK, H, Dq), 1, 0)
    out = lax.map(lambda qb: dense_attention(qb, k_all, v_all), q_blocks)
    return jnp.moveaxis(out, 0, 1).reshape(B, N, H, v.shape[-1])


def neighbourhood_attention(q, k, v, k_ctx, v_ctx, rel_bias):
    B, N, H, Dh = q.shape
    rows = N // GRID_W
    kh = min(NA_WIN_H, rows)
    kw = NA_WIN_W
    scale = Dh ** -0.5
    qg = jnp.moveaxis(q.reshape(B, rows, GRID_W, H, Dh), 1, 0)
    kg = k.reshape(B, rows, GRID_W, H, Dh)
    vg = v.reshape(B, rows, GRID_W, H, Dh)
    col = jnp.arange(GRID_W)
    col_start = jnp.clip(col - kw // 2, 0, GRID_W - kw)
    col_in = (col[None, :] >= col_start[:, None]) & (col[None, :] < col_start[:, None] + kw)
    bias_cols = rel_bias[:, :, col[None, :] - col[:, None] + (NA_WIN_W - 1)]

    def one_row(args):
        r, qr = args
        start = jnp.clip(r - kh // 2, 0, rows - kh)
        kr = lax.dynamic_slice_in_dim(kg, start, kh, axis=1)
        vr = lax.dynamic_slice_in_dim(vg, start, kh, axis=1)
        bias = bias_cols[:, start + jnp.arange(kh) - r + (NA_WIN_H - 1)]
        s = jnp.einsum('bqhd,bjkhd->bhqjk', qr, kr).astype(jnp.float32) * scale
        s = s + jnp.transpose(bias, (0, 2, 1, 3)).astype(jnp.float32)[None]
        s = jnp.where(col_in[None, None, :, None, :], s, MASK_VALUE).reshape(B, H, GRID_W, kh * GRID_W)
        s_ctx = jnp.einsum('bqhd,bchd->bhqc', qr, k_ctx).astype(jnp.float32) * scale
        p = jax.nn.softmax(jnp.concatenate([s, s_ctx], axis=-1), axis=-1).astype(v.dtype)
        p_win = p[..., :kh * GRID_W].reshape(B, H, GRID_W, kh, GRID_W)
        p_ctx = p[..., kh * GRID_W:]
        return (jnp.einsum('bhqjk,bjkhd->bqhd', p_win, vr)
                + jnp.einsum('bhqc,bchd->bqhd', p_ctx, v_ctx))

    out = lax.map(one_row, (jnp.arange(rows), qg))
    return jnp.moveaxis(out, 0, 1).reshape(B, N, H, Dh)


def hierarchical_moe(h, w_rg, b_rg, w_re, b_re, w_gate, w_up, w_down):
    B, L, D = h.shape
    t = h.reshape(B * L, D)
    T = t.shape[0]
    g_logits = (t @ w_rg + b_rg).astype(jnp.float32)
    g_prob = jax.nn.softmax(g_logits, axis=-1)
    g_sel = jnp.argmax(g_logits, axis=-1)
    g_w = jnp.take_along_axis(g_prob, g_sel[:, None], axis=-1)
    e_logits = (t @ w_re + b_re).astype(jnp.float32).reshape(T, N_GROUPS, EXPERTS_PER_GROUP)
    e_logits = jnp.take_along_axis(e_logits, g_sel[:, None, None], axis=1)[:, 0]
    top_p, top_i = lax.top_k(jax.nn.softmax(e_logits, axis=-1), TOP_K_IN_GROUP)
    top_p = top_p / jnp.sum(top_p, axis=-1, keepdims=True)
    within = jnp.sum(jax.nn.one_hot(top_i, EXPERTS_PER_GROUP, dtype=jnp.float32) * top_p[..., None], axis=1)
    combine = (jax.nn.one_hot(g_sel, N_GROUPS, dtype=jnp.float32)[:, :, None]
               * (g_w * within)[:, None, :]).astype(h.dtype)
    wg = w_gate.reshape(N_GROUPS, EXPERTS_PER_GROUP, D, D_EXPERT)
    wu = w_up.reshape(N_GROUPS, EXPERTS_PER_GROUP, D, D_EXPERT)
    wd = w_down.reshape(N_GROUPS, EXPERTS_PER_GROUP, D_EXPERT, D)

    def one_group(args):
        wg_g, wu_g, wd_g, comb_g = args
        hid = jax.nn.silu(jnp.einsum('td,edf->tef', t, wg_g)) * jnp.einsum('td,edf->tef', t, wu_g)
        return jnp.einsum('tef,efd->td', hid * comb_g[:, :, None], wd_g)

    y = jnp.sum(lax.map(one_group, (wg, wu, wd, jnp.moveaxis(combine, 1, 0))), axis=0)
    return y.reshape(B, L, D)


def setup_inputs(seed: int = 0) -> dict:
    key = jax.random.key(seed)
    ks = jax.random.split(key, 24)
    D = D_MODEL

    def nrm(k, shape, scale):
        return jax.random.normal(k, shape, jnp.float32) * scale

    return {
        'x': nrm(ks[0], (BATCH, SEQ, D), 1.0),
        'c': nrm(ks[1], (BATCH, D), 1.0),
        'ctx': nrm(ks[2], (BATCH, CTX_LEN, D), 1.0),
        'c_ctx': nrm(ks[3], (D,), 1.0),
        'w_mod': nrm(ks[4], (DEPTH, D, 6 * D), 0.5 * D ** -0.5),
        'b_mod': nrm(ks[5], (DEPTH, 6 * D), 0.02),
        'norm_attn_g': 1.0 + nrm(ks[6], (DEPTH, D), 0.02),
        'norm_ffn_g': 1.0 + nrm(ks[7], (DEPTH, D), 0.02),
        'w_in': nrm(ks[8], (DEPTH, D, Q_COLS + KV_COLS), D ** -0.5),
        'q_a_norm_g': 1.0 + nrm(ks[9], (DEPTH, Q_LORA), 0.02),
        'kv_a_norm_g': 1.0 + nrm(ks[10], (DEPTH, KV_LORA), 0.02),
        'w_uq': nrm(ks[11], (DEPTH, Q_LORA, MLA_HEADS * MLA_QK), Q_LORA ** -0.5),
        'w_ukv': nrm(ks[12], (DEPTH, KV_LORA, MLA_HEADS * (MLA_NOPE + MLA_V)), KV_LORA ** -0.5),
        'na_rel_bias': nrm(ks[13], (DEPTH, NA_HEADS, 2 * NA_WIN_H - 1, 2 * NA_WIN_W - 1), 0.5),
        'w_out': nrm(ks[14], (DEPTH, MIX_WIDTH, D), MIX_WIDTH ** -0.5),
        'w_router_group': nrm(ks[15], (DEPTH, D, N_GROUPS), D ** -0.5),
        'b_router_group': nrm(ks[16], (DEPTH, N_GROUPS), 0.01),
        'w_router_expert': nrm(ks[17], (DEPTH, D, N_EXPERTS), D ** -0.5),
        'b_router_expert': nrm(ks[18], (DEPTH, N_EXPERTS), 0.01),
        'w_gate': nrm(ks[19], (DEPTH, N_EXPERTS, D, D_EXPERT), D ** -0.5),
        'w_up': nrm(ks[20], (DEPTH, N_EXPERTS, D, D_EXPERT), D ** -0.5),
        'w_down': nrm(ks[21], (DEPTH, N_EXPERTS, D_EXPERT, D), D_EXPERT ** -0.5),
        'final_norm_g': 1.0 + nrm(ks[22], (D,), 0.02),
    }


def reference(x, c, ctx, c_ctx, w_mod, b_mod, norm_attn_g, norm_ffn_g, w_in, q_a_norm_g, kv_a_norm_g,
              w_uq, w_ukv, na_rel_bias, w_out, w_router_group, b_router_group, w_router_expert,
              b_router_expert, w_gate, w_up, w_down, final_norm_g):
    B, N, D = x.shape
    pos = jnp.arange(N)
    cos, sin = axial_rope_tables(pos // GRID_W, pos % GRID_W, x.dtype)
    for l in range(DEPTH):
        sh1, sc1, g1, sh2, sc2, g2 = [m[:, None, :] for m in adaln(c, w_mod[l], b_mod[l])]
        csh1, csc1, cg1, csh2, csc2, cg2 = adaln(c_ctx, w_mod[l], b_mod[l])
        hx = modulate(rmsnorm(x, norm_attn_g[l]), sh1, sc1)
        hc = modulate(rmsnorm(ctx, norm_attn_g[l]), csh1, csc1)

        kn_c, kr_c, v_c, nk_c, nv_c = mixer_keys_values(hc @ w_in[l][:, Q_COLS:], kv_a_norm_g[l], w_ukv[l])
        k_c = mla_keys(kn_c, kr_c[:, :, None, :])

        proj = hx @ w_in[l]
        q_m, nq = mixer_queries(proj[..., :Q_COLS], q_a_norm_g[l], w_uq[l])
        kn, kr, v_m, nk, nv = mixer_keys_values(proj[..., Q_COLS:], kv_a_norm_g[l], w_ukv[l])
        q_m = jnp.concatenate([q_m[..., :MLA_NOPE], apply_rope(q_m[..., MLA_NOPE:], cos, sin)], axis=-1)
        k_m = mla_keys(kn, apply_rope(kr[:, :, None, :], cos, sin))
        o_mla = mla_latent_attention(q_m, k_m, v_m, k_c, v_c)
        o_na = neighbourhood_attention(nq, nk, nv, nk_c, nv_c, na_rel_bias[l])
        mixed = jnp.concatenate([o_mla.reshape(B, N, -1), o_na.reshape(B, N, -1)], axis=-1) @ w_out[l]

        if l < DEPTH - 1:
            Bc, C = ctx.shape[:2]
            q_c, nq_c = mixer_queries(hc @ w_in[l][:, :Q_COLS], q_a_norm_g[l], w_uq[l])
            mixed_c = jnp.concatenate([dense_attention(q_c, k_c, v_c).reshape(Bc, C, -1),
                                       dense_attention(nq_c, nk_c, nv_c).reshape(Bc, C, -1)], axis=-1) @ w_out[l]
            ctx = ctx + cg1 * mixed_c
            hc2 = modulate(rmsnorm(ctx, norm_ffn_g[l]), csh2, csc2)
            ctx = ctx + cg2 * hierarchical_moe(hc2, w_router_group[l], b_router_group[l], w_router_expert[l],
                                               b_router_expert[l], w_gate[l], w_up[l], w_down[l])

        x = x + g1 * mixed
        hx2 = modulate(rmsnorm(x, norm_ffn_g[l]), sh2, sc2)
        x = x + g2 * hierarchical_moe(hx2, w_router_group[l], b_router_group[l], w_router_expert[l],
                                      b_router_expert[l], w_gate[l], w_up[l], w_down[l])
    return rmsnorm(x, final_norm_g)
```

```python
import numpy as np
import concourse.bass as bass
import concourse.mybir as mybir
from concourse.bass_utils import run_bass_kernel_spmd

F32, BF16 = mybir.dt.float32, mybir.dt.bfloat16
AF = mybir.ActivationFunctionType
ALU = mybir.AluOpType

D = 1024
NT = 68
NS = 17
NSLOT = NT * 128
QC0, KV0 = 0, 896
C_KVLAT, C_KR, C_NK, C_NV = 896, 1152, 1184, 1696
EPS = 1e-6
import os
EVAC_MODE = int(os.environ.get('EVAC_MODE', '2'))
NEG = -1e30


class Trk:
    def __init__(self, nc):
        self.nc = nc
        self.eng = {}
        for name, h in (("pe", nc.tensor), ("act", nc.scalar), ("dve", nc.vector), ("pool", nc.gpsimd), ("sp", nc.sync)):
            self.eng[name] = dict(h=h, sem=nc.alloc_semaphore("s_" + name), cnt=0, seen={})
        self.res = {}
        self.dsem = {}

    def _r(self, k):
        if k not in self.res:
            self.res[k] = dict(w=None, r={})
        return self.res[k]

    def _waits(self, en, reads, writes):
        e = self.eng[en]
        need = {}
        def add(ev):
            if ev is None:
                return
            sem, val, src = ev
            if src == "pe" and en == "pe":
                return
            if need.get(sem.name, (None, 0))[1] < val:
                need[sem.name] = (sem, val)
        for k in reads:
            add(self._r(k)["w"])
        for k in writes:
            r = self._r(k)
            add(r["w"])
            for ev in r["r"].values():
                add(ev)
        for sn, (sem, val) in need.items():
            if e["seen"].get(sn, 0) < val:
                e["h"].wait_ge(sem, val)
                e["seen"][sn] = val

    def _mark(self, ev, reads, writes):
        for k in writes:
            r = self._r(k)
            r["w"] = ev
            r["r"] = {}
        for k in reads:
            r = self._r(k)
            r["r"][ev[0].name + ev[2]] = ev

    def op(self, en, fn, reads=(), writes=()):
        e = self.eng[en]
        self._waits(en, reads, writes)
        ins = fn(e["h"])
        e["cnt"] += 1
        ins.then_inc(e["sem"], 1)
        self._mark((e["sem"], e["cnt"], en), reads, writes)

    def group(self, en, fns, reads=(), writes=()):
        e = self.eng[en]
        self._waits(en, reads, writes)
        ins = None
        for fn in fns:
            ins = fn(e["h"])
        e["cnt"] += 1
        ins.then_inc(e["sem"], 1)
        self._mark((e["sem"], e["cnt"], en), reads, writes)

    def dma(self, qn, out, in_, reads=(), writes=(), key=None):
        e = self.eng[qn]
        self._waits(qn, reads, writes)
        key = key or (writes[0] if writes else reads[0])
        if key not in self.dsem:
            self.dsem[key] = [self.nc.alloc_semaphore("d%d" % len(self.dsem)), 0]
        ds = self.dsem[key]
        e["h"].dma_start(out=out, in_=in_).then_inc(ds[0], 16)
        ds[1] += 16
        self._mark((ds[0], ds[1], "dma"), reads, writes)

    def barrier(self):
        for en, e in self.eng.items():
            for fn, f in self.eng.items():
                if fn != en and f["cnt"] > e["seen"].get(f["sem"].name, 0):
                    e["h"].wait_ge(f["sem"], f["cnt"])
                    e["seen"][f["sem"].name] = f["cnt"]
            for k, (sem, tot) in self.dsem.items():
                if tot > e["seen"].get(sem.name, 0):
                    e["h"].wait_ge(sem, tot)
                    e["seen"][sem.name] = tot

    def finish(self):
        e = self.eng["sp"]
        for fn, f in self.eng.items():
            if fn != "sp" and f["cnt"] > e["seen"].get(f["sem"].name, 0):
                e["h"].wait_ge(f["sem"], f["cnt"])
        for k, (sem, tot) in self.dsem.items():
            if tot > e["seen"].get(sem.name, 0):
                e["h"].wait_ge(sem, tot)


def na_row_slot(l):
    if 4 <= l < 36:
        return (l - 4) * 64
    if l < 4:
        return 2048 + l * 64
    return 2304 + (l - 36) * 64


def build(stop_after=None, dbg=None):
    nc = bass.Bass("TRN2", target_bir_lowering=False)
    T = Trk(nc)

    def din(name, shape, dt=F32):
        return nc.dram_tensor(name, list(shape), dt, kind="ExternalInput").ap()

    xs = din("xs", [NSLOT, D])
    cvec = din("cvec", [2, D])
    w_mod = din("w_mod", [D, 6 * D]); b_mod = din("b_mod", [6 * D])
    g_attn = din("g_attn", [D]); g_ffn = din("g_ffn", [D]); g_fin = din("g_fin", [D])
    w_in = din("w_in", [D, 2208]); w_krs = din("w_krs", [D, 32])
    gq = din("gq", [384]); gkv = din("gkv", [256])
    w_uq = din("w_uq", [384, 768]); w_uqs = din("w_uqs", [384, 8, 32]); w_ukv = din("w_ukv", [256, 1024])
    w_out = din("w_out", [D, D])
    w_r = din("w_r", [D, 20]); b_r = din("b_r", [20])
    w_gate = din("w_gate", [16, D, 512]); w_up = din("w_up", [16, D, 512]); w_down = din("w_down", [16, 512, D])
    kmask_d = din("kmask", [128, NT])
    ctab = din("ctab", [32, NSLOT]); stab = din("stab", [32, NSLOT])
    nab = din("nab", [8, 8, 128, 512]); rmc_d = din("rmc", [128, 4, 64])
    out = nc.dram_tensor("out", [2048, D], F32, kind="ExternalOutput").ap()
    dbg_out = {}

    def sbat(name, shape, dt, at):
        nbytes = int(np.prod(shape[1:])) * (2 if dt == BF16 else 4)
        assert at % 32 == 0 and at + nbytes <= 229344, (name, at, nbytes)
        return nc.alloc_sbuf_tensor_at(name, list(shape), dt, offset=at)
    cur = [16512]
    def sb(name, shape, dt):
        nbytes = (int(np.prod(shape[1:])) * (2 if dt == BF16 else 4) + 31) // 32 * 32
        off = cur[0]; cur[0] += nbytes
        return sbat(name, shape, dt, off)
    ident = sb("ident", [128, 128], BF16)
    ones_bf = sb("ones_bf", [128, 128], BF16)
    onesf = sb("onesf", [128, 128], F32)
    epsc = sb("epsc", [128, 1], F32)
    modc = sb("modc", [128, 64], F32)
    gqc = sb("gqc", [128, 4], F32); gkvc = sb("gkvc", [128, 2], F32)
    kmask = sb("kmask_s", [128, NT], F32)
    ss = sb("ss", [128, NT], F32); sd = sb("sd", [128, NT], F32); rstd = sb("rstd", [128, NT], F32)
    rmc = sb("rmc_s", [128, 4, 64], BF16)
    shiftm = sb("shiftm", [64, 128], BF16)
    identf = sb("identf", [128, 128], F32)
    assert cur[0] <= 25728, cur[0]
    TMP = 25728
    cur[0] = TMP
    tmpf = [sb("tmpf%d" % i, [128, 512], F32) for i in range(3)]
    sq = [sb("sq%d" % i, [128, 512], BF16) for i in range(3)]
    sdb = sb("sdb", [128, 512], F32)
    rb = sb("rb", [128, 512], F32)
    ctt = sb("ctt", [128, 512], F32); stt = sb("stt", [128, 512], F32)
    r2 = sb("r2", [128, 512], F32)
    krot_tmp = sb("krot_tmp", [128, 3072], BF16)
    assert cur[0] <= 52352, cur[0]
    A0 = 52352
    kvn = sbat("kvn", [128, 2, NSLOT], BF16, A0)
    qlatn = sbat("qlatn", [128, 3, 2048], BF16, A0 + 34816)
    NR = A0 + 34816 + 12288
    KN = sbat("KN", [128, 4, 2816], BF16, NR)
    VN = sbat("VN", [128, 22, 8, 65], BF16, NR + 22528)
    QN = sbat("QN", [128, 4, 2048], BF16, NR + 22528 + 22880)
    NR_END = NR + 22528 + 22880 + 16384
    KH = [sbat("KH%d" % i, [128, NSLOT], BF16, NR + i * 17408) for i in range(2)]
    VH = [sbat("VH%d" % i, [128, NT, 65], BF16, NR + 34816 + i * 8864) for i in range(2)]
    QH = [sbat("QH%d" % i, [128, 2048], BF16, NR + 34816 + 17728 + i * 4096) for i in range(2)]
    assert NR + 34816 + 17728 + 8192 <= NR_END
    ATR = NR_END
    AT = sbat("AT", [128, 8, 2048], BF16, ATR)
    TAIL = ATR + 32768
    assert TAIL == 194016, TAIL
    cur[0] = TAIL
    PT2 = [sb("PT%d" % i, [128, 1024], BF16) for i in range(4)]
    osb = [sbat("osb%d" % i, [128, 512], F32, TMP + i * 2048) for i in range(2)]
    dgs = [sbat("dg0", [128, 4, 128], F32, TMP + 4096), sbat("dg1", [128, 4, 128], F32, TMP + 16384)]
    rcols = [sbat("rcol%d" % i, [128, 4], F32, TMP + 18432 + i * 32) for i in range(2)]
    onbs = [sbat("onb%d" % i, [64, 512], BF16, TMP + 6144 + i * 1024) for i in range(2)]
    ATT_W = cur[0]
    class Half:
        def __init__(self, t, off):
            self.t, self.off = t, off
        def __getitem__(self, key):
            r, c = key
            a = (c.start or 0) + self.off
            b_ = (c.stop if c.stop is not None else 512) + self.off
            return self.t[r, a:b_]
    PP = [nc.alloc_psum_tensor("pp%d" % i, [128, 1024], F32) for i in range(2)]
    PS = [nc.alloc_psum_tensor("ps%d" % i, [128, 512], F32) for i in range(4)]
    PS += [Half(PP[0], 0), Half(PP[0], 512), Half(PP[1], 0)]
    pTt = PP[1][:, 512:1024].bitcast(BF16)
    rr = [0]
    def psn(lo=0, hi=8):
        i = lo + rr[0] % (hi - lo); rr[0] += 1
        return i
    gscr = nc.dram_tensor("gscr", [2, D], F32).ap()

    T.op("pool", lambda e: e.memset(ident[:], 0.0), writes=["ident"])
    T.op("pool", lambda e: e.affine_select(out=ident[:], in_=ident[:], pattern=[[-1, 128]], compare_op=ALU.not_equal,
                                           fill=1.0, base=0, channel_multiplier=1), reads=["ident"], writes=["ident"])
    T.op("pool", lambda e: e.memset(identf[:], 0.0), writes=["identf"])
    T.op("pool", lambda e: e.affine_select(out=identf[:], in_=identf[:], pattern=[[-1, 128]], compare_op=ALU.not_equal,
                                           fill=1.0, base=0, channel_multiplier=1), reads=["identf"], writes=["identf"])
    T.op("pool", lambda e: e.memset(shiftm[:], 0.0), writes=["shiftm"])
    T.op("pool", lambda e: e.affine_select(out=shiftm[:], in_=shiftm[:], pattern=[[-1, 128]], compare_op=ALU.not_equal,
                                           fill=1.0, base=64, channel_multiplier=1), reads=["shiftm"], writes=["shiftm"])
    T.op("pool", lambda e: e.memset(ones_bf[:], 1.0), writes=["ones_bf"])
    T.op("pool", lambda e: e.memset(onesf[:], 1.0), writes=["onesf"])
    T.op("pool", lambda e: e.memset(epsc[:], EPS), writes=["epsc"])
    T.op("pool", lambda e: e.memset(ss[:], 0.0), writes=["ss_all"])
    T.dma("sp", kmask[:], kmask_d[:, :], writes=["kmask"])
    T.dma("pool", rmc[:], rmc_d[:, :, :], writes=["rmc"])
    with nc.allow_non_contiguous_dma(reason="tiny column loads"):
        T.dma("sp", gqc[:, 0:3], gq.rearrange("(c p) -> p c", p=128), writes=["gqc"])
        T.dma("sp", gkvc[:, 0:2], gkv.rearrange("(c p) -> p c", p=128), writes=["gkvc"])

    PTR = 7
    pT = pTt

    def rms_feat(ps_list, nfeat, gcol, gkey, dst_fn, dkey):
        n = len(ps_list)
        for c, pi in enumerate(ps_list):
            T.op("act", lambda e, c=c, pi=pi: e.activation(out=tmpf[c][:], in_=PS[pi][:, :], func=AF.Copy), reads=["ps%d" % pi], writes=["tmpf%d" % c])
            T.op("act", lambda e, c=c: e.activation(out=sq[c][:], in_=tmpf[c][:], func=AF.Square), reads=["tmpf%d" % c], writes=["sq%d" % c])
        pq = psn(0, 7)
        T.group("pe", [lambda e, c=c, pq=pq: e.matmul(PS[pq][:, :], lhsT=ones_bf[:], rhs=sq[c][:], start=(c == 0), stop=(c == n - 1)) for c in range(n)],
                reads=["ones_bf"] + ["sq%d" % c for c in range(n)], writes=["ps%d" % pq])
        T.op("act", lambda e, pq=pq: e.activation(out=sdb[:], in_=PS[pq][:, :], func=AF.Sqrt, bias=epsc[:], scale=1.0 / nfeat),
             reads=["ps%d" % pq, "epsc"], writes=["sdb"])
        T.op("dve", lambda e: e.reciprocal(out=rb[:], in_=sdb[:]), reads=["sdb"], writes=["rb"])
        for c in range(n):
            T.op("dve", lambda e, c=c: e.scalar_tensor_tensor(out=dst_fn(c), in0=tmpf[c][:], scalar=gcol[:, c:c + 1], in1=rb[:], op0=ALU.mult, op1=ALU.mult),
                 reads=["tmpf%d" % c, "rb", gkey], writes=[dkey])

    def norm_a(t, xb, xk, nb, nk, rkey):
        T.op("act", lambda e: e.activation(out=nb[:], in_=xb, func=AF.Square, accum_out=ss[:, t:t + 1]), reads=[xk], writes=[nk, "ss%d" % t])
        T.op("act", lambda e: e.activation(out=sd[:, t:t + 1], in_=ss[:, t:t + 1], func=AF.Sqrt, bias=epsc[:], scale=1.0 / D),
             reads=["ss%d" % t, "epsc"], writes=["sd%d" % t])
        T.op("dve", lambda e: e.reciprocal(out=rstd[:, t:t + 1], in_=sd[:, t:t + 1]), reads=["sd%d" % t], writes=[rkey])
        T.op("act", lambda e: e.activation(out=nb[:], in_=xb, func=AF.Identity, scale=rstd[:, t:t + 1]), reads=[xk, rkey], writes=[nk])

    def norm_b(nb, nk, dst_fn, dkeys, cb):
        T.group("pe", [lambda e, kc=kc: e.transpose(out=pT[:, kc * 128:(kc + 1) * 128], in_=nb[:, kc * 128:(kc + 1) * 128], identity=ident[:])
                       for kc in range(8)], reads=[nk, "ident"], writes=["ps%d" % PTR])
        for kc in range(8):
            dst = dst_fn(kc); src = pT[:, kc * 128:(kc + 1) * 128]
            T.op("dve", lambda e, dst=dst, src=src, kc=kc: e.tensor_scalar(out=dst, in0=src, scalar1=modc[:, cb + kc:cb + kc + 1],
                                                                     scalar2=modc[:, cb + 8 + kc:cb + 9 + kc], op0=ALU.mult, op1=ALU.add),
                 reads=["ps%d" % PTR, "modc"], writes=[dkeys[kc]])

    def norm_transpose(src_ap, t, xb, xk, nb, nk, dst_fn, dkeys, cb, rkey):
        norm_a(t, xb, xk, nb, nk, rkey)
        norm_b(nb, nk, dst_fn, dkeys, cb)

    def phaseA(s_list, base, full, krot_dst, xn_base=None, state=None, load_only=False):
        if state is not None:
            return phaseA_run(s_list, full, krot_dst, state)
        o = [base]
        def wa(name, shape, dt):
            nbytes = (int(np.prod(shape[1:])) * (2 if dt == BF16 else 4) + 31) // 32 * 32
            t_ = sbat(name, shape, dt, o[0]); o[0] += nbytes
            return t_
        sfx = "f" if full else "p"
        ncols = 2208 if full else 256
        win_b = wa("win_b" + sfx, [128, 8, ncols], BF16)
        wkr_b = wa("wkr_b" + sfx, [128, 8, 96], BF16)
        wkrs_b = wa("wkrs_b" + sfx, [128, 8, 96], BF16)
        xt = [wa("xt%d%s" % (i, sfx), [128, D], F32) for i in range(2)]
        if xn_base is not None:
            xt += [sbat("xt%d%s" % (2 + i, sfx), [128, D], F32, xn_base + 4096 + i * 4096) for i in range(2)]
        if xn_base is None:
            xn = [wa("xn%d%s" % (i, sfx), [128, D], BF16) for i in range(2)]
        else:
            xn = [sbat("xn%d%s" % (i, sfx), [128, D], BF16, xn_base + i * 2048) for i in range(2)]
        hxT = [wa("hxT%d%s" % (i, sfx), [128, 8, 512], BF16) for i in range(2)]
        assert o[0] <= 229344, o[0]
        wkey = "win_b"
        if full:
            for kc in range(8):
                T.dma("pool", win_b[:, kc, :], w_in[kc * 128:(kc + 1) * 128, :], writes=[wkey])
            cko = C_KVLAT
        else:
            T.dma("pool", win_b[:], w_in[:, C_KVLAT:C_KVLAT + 256].rearrange("(kc p) c -> p kc c", p=128), writes=[wkey])
            cko = 0
        T.op("pool", lambda e: e.memset(wkr_b[:], 0.0), writes=["wkr_b"])
        T.op("pool", lambda e: e.memset(wkrs_b[:], 0.0), writes=["wkrs_b"])
        T.dma("pool", wkr_b[:, :, 64:96], w_in[:, C_KR:C_KR + 32].rearrange("(kc p) c -> p kc c", p=128), writes=["wkr_b"])
        T.dma("pool", wkrs_b[:, :, 64:96], w_krs.rearrange("(kc p) c -> p kc c", p=128), writes=["wkrs_b"])
        T.op("pool", lambda e: e.tensor_scalar(out=wkrs_b[:, :, 64:80], in0=wkrs_b[:, :, 64:80], scalar1=-1.0, scalar2=None, op0=ALU.mult),
             reads=["wkrs_b"], writes=["wkrs_b"])
        state_ = dict(win_b=win_b, wkr_b=wkr_b, wkrs_b=wkrs_b, xt=xt, xn=xn, hxT=hxT, wkey=wkey, cko=cko)
        if load_only:
            return state_
        return phaseA_run(s_list, full, krot_dst, state_)

    def phaseA_run(s_list, full, krot_dst, st_):
        win_b, wkr_b, wkrs_b, xt, xn, hxT, wkey, cko = (st_[k] for k in ("win_b", "wkr_b", "wkrs_b", "xt", "xn", "hxT", "wkey", "cko"))
        if stop_after == "paw":
            return "stop"
        def tinfo(s, tt):
            t = 4 * s + tt
            nx = len(xt)
            return t, xt[t % nx], "xt%d" % (t % nx), xn[t % 2], "xn%d" % (t % 2)
        def nt_dma(s, tt):
            t, xb, xk, nb, nk = tinfo(s, tt)
            T.dma("sp", xb[:], xs[t * 128:(t + 1) * 128, :], writes=[xk])
        def nt_a(s, tt):
            t, xb, xk, nb, nk = tinfo(s, tt)
            norm_a(t, xb[:], xk, nb, nk, "rstd%d" % t)
        def nt_b(s, tt):
            t, xb, xk, nb, nk = tinfo(s, tt)
            hb = hxT[s % 2]; hk = "hxT%d" % (s % 2)
            hks = [hk + "_%d" % kc for kc in range(8)]
            cb = 32 if t in (20, 21) else 0
            norm_b(nb, nk, lambda kc, tt=tt, hb=hb: hb[:, kc, tt * 128:(tt + 1) * 128], hks, cb)

        def mm_parts(s):
            hb = hxT[s % 2]; hk = "hxT%d" % (s % 2)
            hks = [hk + "_%d" % kc for kc in range(8)]
            sl = slice(s * 512, (s + 1) * 512)
            def p0():
                pl = []
                for c in range(2):
                    pi = psn(0, 7); pl.append(pi)
                    T.group("pe", [lambda e, kc=kc, pi=pi, c=c: e.matmul(PS[pi][:, :], lhsT=win_b[:, kc, cko + c * 128:cko + (c + 1) * 128], rhs=hb[:, kc, :],
                                                                     start=(kc == 0), stop=(kc == 7)) for kc in range(8)],
                            reads=hks + [wkey], writes=["ps%d" % pi])
                rms_feat(pl, 256, gkvc, "gkvc", lambda c: kvn[:, c, sl], "kvn")
                pk = psn(0, 7); pks = psn(0, 7)
                T.group("pe", [lambda e, kc=kc: e.matmul(PS[pk][0:96, :], lhsT=wkr_b[:, kc, :], rhs=hb[:, kc, :], start=(kc == 0), stop=(kc == 7)) for kc in range(8)],
                        reads=hks + ["wkr_b"], writes=["ps%d" % pk])
                T.group("pe", [lambda e, kc=kc: e.matmul(PS[pks][0:96, :], lhsT=wkrs_b[:, kc, :], rhs=hb[:, kc, :], start=(kc == 0), stop=(kc == 7)) for kc in range(8)],
                        reads=hks + ["wkrs_b"], writes=["ps%d" % pks])
                T.dma("sp", ctt[64:96, :], ctab[:, sl], writes=["ctt"])
                T.dma("sp", stt[64:96, :], stab[:, sl], writes=["stt"])
                T.op("dve", lambda e: e.tensor_tensor(out=tmpf[2][64:96, :], in0=PS[pk][64:96, :], in1=ctt[64:96, :], op=ALU.mult), reads=["ps%d" % pk, "ctt"], writes=["tmpf2"])
                T.op("dve", lambda e: e.tensor_tensor(out=r2[64:96, :], in0=PS[pks][64:96, :], in1=stt[64:96, :], op=ALU.mult), reads=["ps%d" % pks, "stt"], writes=["r2"])
                for dst, dk in krot_dst(s):
                    T.op("pool", lambda e, dst=dst: e.tensor_tensor(out=dst, in0=tmpf[2][64:96, :], in1=r2[64:96, :], op=ALU.add), reads=["tmpf2", "r2"], writes=[dk])
            def p1():
                if not (full and s < 6):
                    return
                nsl = slice(s * 512, (s + 1) * 512) if s < 5 else slice(2560, 2816)
                ncol = slice(0, 512) if s < 5 else slice(0, 256)
                for j in range(4):
                    pi = psn(0, 7)
                    T.group("pe", [lambda e, kc=kc, pi=pi, j=j: e.matmul(PS[pi][:, :], lhsT=win_b[:, kc, C_NK + j * 128:C_NK + (j + 1) * 128], rhs=hb[:, kc, :],
                                                                     start=(kc == 0), stop=(kc == 7)) for kc in range(8)],
                            reads=hks + [wkey], writes=["ps%d" % pi])
                    if j % 2 == 0:
                        T.op("act", lambda e, pi=pi, j=j: e.activation(out=KN[:, j, nsl], in_=PS[pi][:, ncol], func=AF.Copy), reads=["ps%d" % pi], writes=["KN"])
                    else:
                        T.op("dve", lambda e, pi=pi, j=j: e.tensor_copy(out=KN[:, j, nsl], in_=PS[pi][:, ncol]), reads=["ps%d" % pi], writes=["KN"])
            def p2():
                if not (full and s < 6):
                    return
                for tt in range(4 if s < 5 else 2):
                    pi = psn(0, 7); t = 4 * s + tt
                    T.group("pe", [lambda e, kc=kc, pi=pi, tt=tt: e.matmul(PS[pi][:, :], lhsT=hb[:, kc, tt * 128:(tt + 1) * 128], rhs=win_b[:, kc, C_NV:C_NV + 512],
                                                                       start=(kc == 0), stop=(kc == 7)) for kc in range(8)],
                            reads=hks + [wkey], writes=["ps%d" % pi])
                    src = PS[pi][:, :].rearrange("p (h d) -> p h d", h=8)
                    if tt % 2 == 0:
                        T.op("dve", lambda e, src=src, t=t: e.tensor_copy(out=VN[:, t, :, 0:64], in_=src), reads=["ps%d" % pi], writes=["VN"])
                    else:
                        T.op("act", lambda e, src=src, t=t: e.activation(out=VN[:, t, :, 0:64], in_=src, func=AF.Copy), reads=["ps%d" % pi], writes=["VN"])
            def p3():
                if not (full and s < 4):
                    return
                pl = []
                for c in range(3):
                    pi = psn(0, 7); pl.append(pi)
                    T.group("pe", [lambda e, kc=kc, pi=pi, c=c: e.matmul(PS[pi][:, :], lhsT=win_b[:, kc, c * 128:(c + 1) * 128], rhs=hb[:, kc, :],
                                                                     start=(kc == 0), stop=(kc == 7)) for kc in range(8)],
                            reads=hks + [wkey], writes=["ps%d" % pi])
                rms_feat(pl, 384, gqc, "gqc", lambda c: qlatn[:, c, sl], "qlatn")
                for j in range(4):
                    pi = psn(0, 7)
                    T.group("pe", [lambda e, kc=kc, pi=pi, j=j: e.matmul(PS[pi][:, :], lhsT=win_b[:, kc, 384 + j * 128:384 + (j + 1) * 128], rhs=hb[:, kc, :],
                                                                     start=(kc == 0), stop=(kc == 7)) for kc in range(8)],
                            reads=hks + [wkey], writes=["ps%d" % pi])
                    T.op("act", lambda e, pi=pi, j=j: e.activation(out=QN[:, j, sl], in_=PS[pi][:, :], func=AF.Copy, scale=0.125), reads=["ps%d" % pi], writes=["QN"])
            return [p0, p1, p2, p3]

        s_list = list(s_list)
        seq = [(s, tt) for s in s_list for tt in range(4)]
        depth = len(xt) - 1
        for k0 in range(min(depth, len(seq))):
            nt_dma(*seq[k0])
        nt_a(*seq[0])
        prev_parts = None
        for k, (s, tt) in enumerate(seq):
            if k + depth < len(seq):
                nt_dma(*seq[k + depth])
            if k + 1 < len(seq):
                nt_a(*seq[k + 1])
            nt_b(s, tt)
            if prev_parts is not None:
                prev_parts[tt]()
            if tt == 3:
                prev_parts = mm_parts(s)
        for p_ in prev_parts:
            p_()

    PA1 = phaseA(None, ATR, True, None, load_only=True)
    ccol = sbat("ccol", [128, 2, 8], F32, A0)
    scol = sbat("scol", [128, 2, 8], BF16, A0 + 64)
    wmb = [sbat("wmb%d" % i, [128, 8, 512], BF16, A0 + 128 + i * 8192) for i in range(2)]
    brow = sbat("brow", [1, 512], F32, A0 + 128 + 16384)
    grow = sbat("grow", [1, 512], F32, A0 + 128 + 16384 + 2048)
    mrow = [sbat("mrow%d" % i, [1, 512], F32, A0 + 128 + 16384 + 4096 + i * 2048) for i in range(2)]
    with nc.allow_non_contiguous_dma(reason="tiny column loads"):
        T.dma("sp", ccol[:], cvec.rearrange("r (kc p) -> p r kc", p=128), writes=["ccol"])
    T.op("act", lambda e: e.activation(out=scol[:], in_=ccol[:], func=AF.Silu), reads=["ccol"], writes=["scol"])
    PCOL = 6
    for j in range(12):
        v, hf = j // 2, j % 2
        wb = wmb[j % 2]; wk = "wmb%d" % (j % 2)
        T.dma("pool", wb[:], w_mod[:, j * 512:(j + 1) * 512].rearrange("(kc p) c -> p kc c", p=128), writes=[wk])
        T.dma("sp", brow[:], b_mod[None, j * 512:(j + 1) * 512], writes=["brow"])
        if v in (1, 4):
            gsrc = g_attn if v == 1 else g_ffn
            T.dma("sp", grow[:], gsrc[None, hf * 512:(hf + 1) * 512], writes=["grow"])
        for r in range(2):
            if r == 1 and v > 1:
                continue
            pi = psn(0, 6)
            T.group("pe", [lambda e, kc=kc, pi=pi, r=r, wb=wb: e.matmul(PS[pi][0:1, :], lhsT=scol[:, r, kc:kc + 1], rhs=wb[:, kc, :],
                                                                    start=(kc == 0), stop=(kc == 7)) for kc in range(8)],
                    reads=["scol", wk], writes=["ps%d" % pi])
            mr = mrow[r]; mk = "mrow%d" % r
            T.op("dve", lambda e, pi=pi, mr=mr: e.tensor_tensor(out=mr[:], in0=PS[pi][0:1, :], in1=brow[:], op=ALU.add),
                 reads=["ps%d" % pi, "brow"], writes=[mk])
            if v in (1, 4):
                T.op("dve", lambda e, mr=mr: e.scalar_tensor_tensor(out=mr[:], in0=mr[:], scalar=1.0, in1=grow[:], op0=ALU.add, op1=ALU.mult),
                     reads=[mk, "grow"], writes=[mk])
            if v in (2, 5):
                gi = 0 if v == 2 else 1
                T.dma("sp", gscr[gi:gi + 1, hf * 512:(hf + 1) * 512], mr[:], reads=[mk], writes=["gscr_r"], key="gscr")
            else:
                if r == 0:
                    base = {1: 0, 0: 8, 4: 16, 3: 24}[v]
                else:
                    base = {1: 32, 0: 40}[v]
                T.group("pe", [lambda e, i=i, mr=mr, base=base, hf=hf: e.matmul(PS[PCOL][:, base + hf * 4 + i: base + hf * 4 + i + 1],
                                                                             lhsT=mr[0:1, i * 128:(i + 1) * 128], rhs=onesf[0:1, 0:1],
                                                                             start=True, stop=True) for i in range(4)],
                        reads=[mk, "onesf"], writes=["pscol"])
    T.op("dve", lambda e: e.tensor_copy(out=modc[:, 0:48], in_=PS[PCOL][:, 0:48]), reads=["pscol"], writes=["modc"])
    if stop_after == "adaln":
        dbg_out["modc"] = (modc, [128, 64], F32)
        return finish(nc, T, dbg_out, out)
    T.barrier()

    T.op("pool", lambda e: e.memset(VN[:, :, :, 64:65], 1.0), writes=["VN"])
    rv = phaseA(range(6), ATR, True, lambda s: [(krot_tmp[64:96, s * 512:(s + 1) * 512], "krot_tmp")], state=PA1)
    if rv == "stop":
        return finish(nc, T, dbg_out, out)
    if stop_after == "phaseA":
        dbg_out["kvn"] = (kvn, [128, 2, NSLOT], BF16); dbg_out["qlatn"] = (qlatn, [128, 3, 2048], BF16)
        dbg_out["KN"] = (KN, [128, 4, 2816], BF16); dbg_out["QN"] = (QN, [128, 4, 2048], BF16)
        dbg_out["VN"] = (VN, [128, 22, 8, 65], BF16); dbg_out["krot_tmp"] = (krot_tmp, [128, 3072], BF16)
        return finish(nc, T, dbg_out, out)
    T.barrier()

    nrm = [0]
    pending = []
    def run_pending(i, flush=False):
        for item in [x for x in pending if flush or x[0] <= i]:
            pending.remove(item)
            item[1]()
    def normalize_out(po, h_at, qsl, i, scr=3, off=0):
        par = nrm[0] % 2; nrm[0] += 1
        ob = osb[par]; ok = "osb%d" % par; onb = onbs[par]; onk = "onb%d" % par
        dg = dgs[par]; dk = "dg%d" % par; rcol = rcols[par]; rk = "rcol%d" % par
        pk = "ps%d" % scr
        T.op("dve", lambda e: e.tensor_copy(out=ob[0:65, :], in_=PS[po][0:65, :]), reads=["ps%d" % po], writes=[ok])
        def st3():
            T.group("pe", [lambda e: e.matmul(PS[scr][:, :], lhsT=shiftm[:], rhs=onb[:], start=True, stop=True)], reads=[onk, "shiftm"], writes=[pk])
            T.op("dve", lambda e: e.tensor_copy(out=AT[64:128, h_at // 2, qsl], in_=PS[scr][64:128, :]), reads=[pk], writes=["AT"])
        def st2():
            T.group("pe", [lambda e, j=j: e.matmul(PS[scr][0:64, j * 128:(j + 1) * 128], lhsT=onesf[:, 0:64], rhs=dg[:, j, :], start=True, stop=True) for j in range(4)],
                    reads=[dk, "onesf"], writes=[pk])
            if h_at % 2 == 0:
                T.op("dve", lambda e: e.tensor_tensor(out=AT[0:64, h_at // 2, qsl], in0=ob[0:64, :], in1=PS[scr][0:64, :], op=ALU.mult),
                     reads=[ok, pk], writes=["AT"])
            else:
                T.op("dve", lambda e: e.tensor_tensor(out=onb[:], in0=ob[0:64, :], in1=PS[scr][0:64, :], op=ALU.mult),
                     reads=[ok, pk], writes=[onk])
                pending.append([i + 7 + off, st3])
        def st1():
            T.group("pe", [lambda e, j=j: e.matmul(PS[scr][:, j:j + 1], lhsT=ob[64:65, j * 128:(j + 1) * 128], rhs=onesf[64:65, 0:1], start=True, stop=True) for j in range(4)],
                    reads=[ok, "onesf"], writes=[pk])
            T.op("dve", lambda e: e.reciprocal(out=rcol[:, 0:4], in_=PS[scr][:, 0:4]), reads=[pk], writes=[rk])
            for j in range(4):
                T.op("dve", lambda e, j=j: e.tensor_scalar(out=dg[:, j, :], in0=identf[:], scalar1=rcol[:, j:j + 1], scalar2=None, op0=ALU.mult),
                     reads=["identf", rk], writes=[dk])
            pending.append([i + 4 + off, st2])
        pending.append([i + 1 + off, st1])

    cur[0] = ATT_W
    nabt = [sb("nabt%d" % i, [128, 8, 512], BF16) for i in range(2)]
    assert cur[0] <= 229344
    stg = [sbat("stg%d" % i, [128, 1024], F32, TMP + 8192 + i * 4096) for i in range(2)]
    combt = [sbat("combt%d" % i, [128, 8, 512], BF16, ATR + i * 8192) for i in range(2)]
    na_steps = [(h, g, p) for h in range(8) for g in range(4) for p in range(5)]
    pti = [0]
    na_pt = {}
    def na_load(h):
        T.dma("pool", nabt[h % 2][:], nab[h].rearrange("c p f -> p c f"), writes=["nabt%d" % (h % 2)])
    def na_comb(h, g):
        k = (h * 4 + g) % 2
        T.op("pool", lambda e: e.tensor_tensor(out=combt[k][:].rearrange("p c (i q) -> p c i q", q=64),
                                               in0=nabt[h % 2][:].rearrange("p c (i q) -> p c i q", q=64),
                                               in1=rmc[:, g, :].rearrange("p (c i) -> p c i", i=8).unsqueeze(3).to_broadcast([128, 8, 8, 64]), op=ALU.add),
             reads=["nabt%d" % (h % 2), "rmc"], writes=["combt%d" % k])
    qz = [[sb("qz%d_%d" % (par, k_), [128, 512], BF16) for k_ in range(2)] for par in range(2)]
    assert cur[0] <= 229344
    for par in range(2):
        for k_ in range(2):
            T.op("pool", lambda e, par=par, k_=k_: e.memset(qz[par][k_][:], 0.0), writes=["qz%d_%d" % (par, k_)])
    def na_qz(h, g):
        par = h % 2; k_ = (h * 4 + g) // 1 % 2
        pb_ = par * 64
        T.op("pool", lambda e: e.tensor_copy(out=qz[par][k_][pb_:pb_ + 64, :], in_=QN[pb_:pb_ + 64, h // 2, g * 512:(g + 1) * 512]),
             reads=["QN"], writes=["qz%d_%d" % (par, k_)])
    na_load(0); na_load(1); na_comb(0, 0); na_qz(0, 0)
    def na_qk(i):
        h, g, p = na_steps[i]
        j = h // 2; pb = (h % 2) * 64
        if p == 0:
            nh, ng = (h, g + 1) if g < 3 else (h + 1, 0)
            if nh < 8:
                if ng == 0 and nh + 1 < 8 and False:
                    pass
                na_comb(nh, ng)
                na_qz(nh, ng)
        if p == 4 and g == 3 and h + 2 < 8:
            na_load(h + 2)
        qsl = slice(g * 512, (g + 1) * 512)
        pp = i % 2
        for half in range(2):
            c = 2 * p + half
            ks = na_row_slot(8 * g + 2 * c) if c < 8 else 2560 + (c - 8) * 128
            dst = PP[pp][:, half * 512:(half + 1) * 512]
            qzb = qz[h % 2][(h * 4 + g) % 2]
            T.group("pe", [lambda e, ks=ks, dst=dst: e.matmul(dst, lhsT=KN[:, j, ks:ks + 128], rhs=qzb[:, :], start=True, stop=True)],
                    reads=["KN", "qz%d_%d" % (h % 2, (h * 4 + g) % 2)], writes=["ps%d" % (4 + 2 * pp + half)])
        k = pti[0] % 4; pti[0] += 1
        na_pt[i] = k
        pk_ = ["ps%d" % (4 + 2 * pp), "ps%d" % (5 + 2 * pp)]
        if p < 4:
            ck = (h * 4 + g) % 2
            sg_ = stg[i % 2]; sgk = "stg%d" % (i % 2)
            T.op("dve", lambda e: e.tensor_tensor(out=sg_[:, :], in0=PP[pp][:, :], in1=combt[ck][:, 2 * p:2 * p + 2, :].rearrange("p c f -> p (c f)"), op=ALU.add),
                 reads=pk_ + ["combt%d" % ck], writes=[sgk])
            T.op("act", lambda e: e.activation(out=PT2[k][:, :], in_=sg_[:, :], func=AF.Exp), reads=[sgk], writes=["PT%d" % k])
        else:
            T.op("act", lambda e: e.activation(out=PT2[k][:, :], in_=PP[pp][:, :], func=AF.Exp), reads=pk_, writes=["PT%d" % k])
    def na_pv(i):
        h, g, p = na_steps[i]
        k = na_pt.pop(i)
        po = (h * 4 + g) % 2
        fns = []
        for half in range(2):
            c = 2 * p + half
            ks = na_row_slot(8 * g + 2 * c) if c < 8 else 2560 + (c - 8) * 128
            fns.append(lambda e, ks=ks, half=half, c=c: e.matmul(PS[po][0:65, :], lhsT=VN[:, ks // 128, h, 0:65], rhs=PT2[k][:, half * 512:(half + 1) * 512],
                                                               start=(c == 0), stop=(c == 9)))
        T.group("pe", fns, reads=["VN", "PT%d" % k], writes=["ps%d" % po])
        if p == 4:
            normalize_out(po, 8 + h, slice(g * 512, (g + 1) * 512), i)
    LA = 3
    for i in range(len(na_steps) + LA):
        if i < len(na_steps):
            na_qk(i)
        if i >= LA:
            na_pv(i - LA)
        run_pending(i - LA)
    for _ in range(8):
        run_pending(0, flush=True)
    if stop_after == "na":
        dbg_out["AT"] = (AT, [128, 8, 2048], BF16)
        return finish(nc, T, dbg_out, out)
    T.barrier()

    for i in range(2):
        T.op("pool", lambda e, i=i: e.tensor_copy(out=KH[i][64:96, 0:3072], in_=krot_tmp[64:96, :]), reads=["krot_tmp"], writes=["KHr%d" % i])
    phaseA(range(6, NS), TAIL, False, lambda s: [(KH[i][64:96, s * 512:(s + 1) * 512], "KHr%d" % i) for i in range(2)], xn_base=ATR)
    T.barrier()

    cur[0] = ATT_W
    wuq_b = sb("wuq_b", [128, 3, 768], BF16)
    wuqs_b = sb("wuqs_b", [128, 3, 8, 96], BF16)
    wukv_b = sb("wukv_b", [128, 2, 1024], BF16)
    ctq = sbat("ctq", [128, 512], F32, TMP + 8192); stq = sbat("stq", [128, 512], F32, TMP + 10240)
    r1q = sbat("r1q", [128, 512], F32, TMP + 12288); r2q = sbat("r2q", [128, 512], F32, TMP + 14336)
    assert cur[0] <= 229344, cur[0]
    T.dma("pool", wuq_b[:], w_uq.rearrange("(kc p) c -> p kc c", p=128), writes=["wuq_b"])
    T.op("pool", lambda e: e.memset(wuqs_b[:], 0.0), writes=["wuqs_b"])
    for kc in range(3):
        T.dma("pool", wuqs_b[:, kc, :, 64:96], w_uqs[kc * 128:(kc + 1) * 128, :, :], writes=["wuqs_b"])
    T.op("pool", lambda e: e.tensor_scalar(out=wuqs_b[:, :, :, 64:80], in0=wuqs_b[:, :, :, 64:80], scalar1=-1.0, scalar2=None, op0=ALU.mult),
         reads=["wuqs_b"], writes=["wuqs_b"])
    T.dma("pool", wukv_b[:], w_ukv.rearrange("(kc p) c -> p kc c", p=128), writes=["wukv_b"])
    for i in range(2):
        T.op("pool", lambda e, i=i: e.memset(VH[i][:, :, 64:65], 1.0), writes=["VH%d" % i])
    SCALE = 96 ** -0.5

    def prep_units(h):
        b = h % 2
        units = []
        pbk = [0]
        def nb_():
            pbk[0] += 1
            return idle_set[0][pbk[0] % 2]
        for s in range(NS):
            def ku(s=s):
                PREP = nb_()
                T.group("pe", [lambda e, kc=kc: e.matmul(PS[PREP][0:64, :], lhsT=wukv_b[:, kc, h * 128:h * 128 + 64], rhs=kvn[:, kc, s * 512:(s + 1) * 512],
                                                         start=(kc == 0), stop=(kc == 1)) for kc in range(2)], reads=["kvn", "wukv_b"], writes=["ps%d" % PREP])
                T.op("dve", lambda e: e.tensor_copy(out=KH[b][0:64, s * 512:(s + 1) * 512], in_=PS[PREP][0:64, :]), reads=["ps%d" % PREP], writes=["KH%d" % b])
            units.append(ku)
            def vu(s=s):
                PREP = nb_()
                fns = []
                for tt in range(4):
                    for kc in range(2):
                        fns.append(lambda e, tt=tt, kc=kc: e.matmul(PS[PREP][:, tt * 64:(tt + 1) * 64], lhsT=kvn[:, kc, (4 * s + tt) * 128:(4 * s + tt + 1) * 128],
                                                                    rhs=wukv_b[:, kc, h * 128 + 64:h * 128 + 128], start=(kc == 0), stop=(kc == 1)))
                T.group("pe", fns, reads=["kvn", "wukv_b"], writes=["ps%d" % PREP])
                T.op("dve", lambda e: e.tensor_copy(out=VH[b][:, 4 * s:4 * s + 4, 0:64], in_=PS[PREP][:, 0:256].rearrange("p (t d) -> p t d", t=4)),
                     reads=["ps%d" % PREP], writes=["VH%d" % b])
            units.append(vu)
        for qc in range(4):
            def qu(qc=qc):
                PA_, PB_ = idle_set[0]
                qsl = slice(qc * 512, (qc + 1) * 512)
                T.dma("sp", ctq[64:96, :], ctab[:, qsl], writes=["ctq"])
                T.dma("sp", stq[64:96, :], stab[:, qsl], writes=["stq"])
                T.group("pe", [lambda e, kc=kc: e.matmul(PS[PA_][0:96, :], lhsT=wuq_b[:, kc, h * 96:(h + 1) * 96], rhs=qlatn[:, kc, qsl],
                                                         start=(kc == 0), stop=(kc == 2)) for kc in range(3)], reads=["qlatn", "wuq_b"], writes=["ps%d" % PA_])
                T.group("pe", [lambda e, kc=kc: e.matmul(PS[PB_][0:96, :], lhsT=wuqs_b[:, kc, h, :], rhs=qlatn[:, kc, qsl],
                                                         start=(kc == 0), stop=(kc == 2)) for kc in range(3)], reads=["qlatn", "wuqs_b"], writes=["ps%d" % PB_])
                T.op("dve", lambda e: e.tensor_copy(out=QH[b][0:64, qsl], in_=PS[PA_][0:64, :]), reads=["ps%d" % PA_], writes=["QH%d" % b])
                T.op("dve", lambda e: e.tensor_tensor(out=r1q[64:96, :], in0=PS[PA_][64:96, :], in1=ctq[64:96, :], op=ALU.mult), reads=["ps%d" % PA_, "ctq"], writes=["r1q"])
                T.op("dve", lambda e: e.tensor_tensor(out=r2q[64:96, :], in0=PS[PB_][64:96, :], in1=stq[64:96, :], op=ALU.mult), reads=["ps%d" % PB_, "stq"], writes=["r2q"])
                T.op("pool", lambda e: e.tensor_tensor(out=QH[b][64:96, qsl], in0=r1q[64:96, :], in1=r2q[64:96, :], op=ALU.add), reads=["r1q", "r2q"], writes=["QH%d" % b])
            units.append(qu)
        return units

    idle_set = [(2, 3)]
    for u in prep_units(0):
        u()
    gstep = [0]
    for h in range(8):
        b = h % 2
        nxt = prep_units(h + 1) if h < 7 else []
        ui = [0]
        steps = [(qp, kc) for qp in range(2) for kc in range(NT)]
        n = len(steps)
        ptm = {}
        def m_qk(i):
            qp, kc = steps[i]; pp = i % 2
            for half in range(2):
                qc = 2 * qp + half
                dst = PP[pp][:, half * 512:(half + 1) * 512]
                T.group("pe", [lambda e, dst=dst, kc=kc, qc=qc: e.matmul(dst, lhsT=KH[b][0:96, kc * 128:(kc + 1) * 128], rhs=QH[b][0:96, qc * 512:(qc + 1) * 512],
                                                                     start=True, stop=True)], reads=["KH%d" % b, "KHr%d" % b, "QH%d" % b], writes=["ps%d" % (4 + 2 * pp + half)])
            k = pti[0] % 3; pti[0] += 1
            ptm[i] = k
            T.op("act", lambda e: e.activation(out=PT2[k][:, :], in_=PP[pp][:, :], func=AF.Exp, bias=kmask[:, kc:kc + 1], scale=SCALE),
                 reads=["ps%d" % (4 + 2 * pp), "ps%d" % (5 + 2 * pp), "kmask"], writes=["PT%d" % k])
        def m_pv(i):
            qp, kc = steps[i]
            k = ptm.pop(i)
            ob_ = (0, 1) if qp == 0 else (2, 3)
            if kc == 0:
                idle_set[0] = (2, 3) if qp == 0 else (0, 1)
            for half in range(2):
                T.group("pe", [lambda e, half=half: e.matmul(PS[ob_[half]][0:65, :], lhsT=VH[b][:, kc, 0:65], rhs=PT2[k][:, half * 512:(half + 1) * 512],
                                                           start=(kc == 0), stop=(kc == NT - 1))], reads=["VH%d" % b, "PT%d" % k], writes=["ps%d" % ob_[half]])
            if kc == NT - 1:
                for half in range(2):
                    qc = 2 * qp + half
                    normalize_out(ob_[half], h, slice(qc * 512, (qc + 1) * 512), gstep[0], scr=ob_[half], off=4 * half)
            if nxt:
                want = min(len(nxt), (len(nxt) * (i + 1)) // (n - 16))
                while ui[0] < want:
                    nxt[ui[0]](); ui[0] += 1
        for i in range(n + 2):
            if i < n:
                m_qk(i)
            if i >= 2:
                gstep[0] += 1
                m_pv(i - 2)
                run_pending(gstep[0])
    for _ in range(8):
        run_pending(0, flush=True)
    if stop_after == "mla":
        dbg_out["AT"] = (AT, [128, 8, 2048], BF16)
        return finish(nc, T, dbg_out, out)
    T.barrier()

    T.op("pool", lambda e: e.memset(ss[:], 0.0), writes=["ss_all"])
    T.barrier()
    xnew = sbat("xnew", [128, 16, D], F32, A0)
    wout_b = sbat("wout_b", [128, 8, D], BF16, 117888)
    g1b = sbat("g1b", [128, D], F32, 134272)
    hx2T = sbat("hx2T", [128, 8, 2048], BF16, TAIL)
    xr = [sbat("xr%d" % i, [128, D], F32, TMP + i * 4096) for i in range(4)]
    xn2 = [sbat("xn2_%d" % i, [128, D], BF16, TMP + 16384 + i * 2048) for i in range(2)]
    T.dma("pool", wout_b[:], w_out.rearrange("(kc p) c -> p kc c", p=128), writes=["wout_b"])
    T.dma("sp", g1b[:], gscr[0:1, :].to_broadcast([128, D]), reads=["gscr_r"], writes=["g1b"], key="g1b")
    T.op("pool", lambda e: e.tensor_tensor(out=wout_b[:], in0=wout_b[:], in1=g1b[:].unsqueeze(1).to_broadcast([128, 8, D]), op=ALU.mult),
         reads=["wout_b", "g1b"], writes=["wout_b"])
    def wo_dma(t):
        T.dma("sp", xr[t % 4][:], xs[t * 128:(t + 1) * 128, :], writes=["xr%d" % (t % 4)])
    def wo_a(t):
        xb = xr[t % 4]; xk = "xr%d" % (t % 4)
        for hf in range(2):
            pi = psn(0, 7)
            T.group("pe", [lambda e, kc=kc, pi=pi, hf=hf: e.matmul(PS[pi][:, :], lhsT=AT[:, kc, t * 128:(t + 1) * 128], rhs=wout_b[:, kc, hf * 512:(hf + 1) * 512],
                                                               start=(kc == 0), stop=(kc == 7)) for kc in range(8)], reads=["AT", "wout_b"], writes=["ps%d" % pi])
            T.op("dve", lambda e, pi=pi, hf=hf: e.tensor_tensor(out=xnew[:, t, hf * 512:(hf + 1) * 512], in0=PS[pi][:, :], in1=xb[:, hf * 512:(hf + 1) * 512], op=ALU.add),
                 reads=["ps%d" % pi, xk], writes=["xnew%d" % t])
    def wo_b(t):
        norm_a(t, xnew[:, t, :], "xnew%d" % t, xn2[t % 2], "xn2_%d" % (t % 2), "rstd2_%d" % t)
    def wo_c(t):
        norm_b(xn2[t % 2], "xn2_%d" % (t % 2), lambda kc, t=t: hx2T[:, kc, t * 128:(t + 1) * 128], ["hx2T"] * 8, 16)
    for t in range(3):
        wo_dma(t)
    for k in range(16 + 2):
        if k + 3 < 16:
            wo_dma(k + 3)
        if k < 16:
            wo_a(k)
        if 1 <= k < 17:
            wo_b(k - 1)
        if k >= 2:
            wo_c(k - 2)
    if stop_after == "wout":
        dbg_out["xnew"] = (xnew, [128, 16, D], F32)
        return finish(nc, T, dbg_out, out)
    T.barrier()

    WB = 117888
    wexp = [[sbat("wg%d" % i, [128, 8, 512], BF16, WB + i * 24576),
             sbat("wu%d" % i, [128, 8, 512], BF16, WB + i * 24576 + 8192),
             sbat("wd%d" % i, [128, 4, D], BF16, WB + i * 24576 + 16384)] for i in range(2)]
    hidT = [sbat("hidT%d" % i, [128, 4, 512], BF16, WB + 49152 + i * 4096) for i in range(2)]
    g2b = sbat("g2b", [128, D], F32, WB + 57344)
    gfb = sbat("gfb", [128, D], F32, WB + 61440)
    assert WB + 65536 <= TAIL
    cur[0] = TMP
    wr_b = sb("wr_b", [128, 8, 20], BF16)
    brb = sb("brb", [128, 20], F32)
    lg = sb("lg", [128, 16, 20], F32)
    comb = sb("comb", [128, 16, 16], F32)
    rt = {n_: sb("rt_" + n_, [128, 16, 4], F32) for n_ in ("GE", "OH", "EL", "MK", "SEL", "EX", "TM")}
    rv = {n_: sb("rv_" + n_, [128, 16], F32) for n_ in ("GM", "SG", "GW", "M1", "M2", "SE", "RS", "WS")}
    sg = sb("sg", [128, 4, 512], BF16)
    ofin = [sb("ofin%d" % i, [128, D], F32) for i in range(2)]
    assert cur[0] <= A0, cur[0]
    T.dma("pool", wr_b[:], w_r.rearrange("(kc p) c -> p kc c", p=128), writes=["wr_b"])
    T.dma("sp", brb[:], b_r[None, :].to_broadcast([128, 20]), writes=["brb"])
    T.dma("sp", g2b[:], gscr[1:2, :].to_broadcast([128, D]), reads=["gscr_r"], writes=["g2b"], key="g2b")
    T.dma("sp", gfb[:], g_fin[None, :].to_broadcast([128, D]), writes=["gfb"])
    pr = psn(0, 4)
    for t in range(16):
        T.group("pe", [lambda e, kc=kc, t=t: e.matmul(PS[pr][:, t * 20:(t + 1) * 20], lhsT=hx2T[:, kc, t * 128:(t + 1) * 128], rhs=wr_b[:, kc, :], start=(kc == 0), stop=(kc == 7))
                       for kc in range(8)], reads=["hx2T", "wr_b"], writes=["ps%d" % pr])
    AXX = mybir.AxisListType.X
    def V(fn, rd, wr):
        T.op("dve", fn, reads=rd, writes=wr)
    def B4(ap):
        return ap.unsqueeze(2).to_broadcast([128, 16, 4])
    GL = lg[:, :, 0:4]
    V(lambda e: e.tensor_tensor(out=lg[:], in0=PS[pr][:, 0:320].rearrange("p (t c) -> p t c", c=20), in1=brb[:].unsqueeze(1).to_broadcast([128, 16, 20]), op=ALU.add),
      ["ps%d" % pr, "brb"], ["lg"])
    V(lambda e: e.reduce_max(out=rv["GM"][:], in_=GL, axis=AXX), ["lg"], ["GM"])
    V(lambda e: e.tensor_tensor(out=rt["GE"][:], in0=GL, in1=B4(rv["GM"][:]), op=ALU.subtract), ["lg", "GM"], ["GE"])
    T.op("act", lambda e: e.activation(out=rt["GE"][:], in_=rt["GE"][:], func=AF.Exp), reads=["GE"], writes=["GE"])
    V(lambda e: e.reduce_sum(out=rv["SG"][:], in_=rt["GE"][:], axis=AXX), ["GE"], ["SG"])
    V(lambda e: e.reciprocal(out=rv["GW"][:], in_=rv["SG"][:]), ["SG"], ["GW"])
    V(lambda e: e.tensor_tensor(out=rt["OH"][:], in0=GL, in1=B4(rv["GM"][:]), op=ALU.is_equal), ["lg", "GM"], ["OH"])
    for g in range(4):
        dst = rt["EL"] if g == 0 else rt["TM"]
        V(lambda e, g=g, dst=dst: e.tensor_tensor(out=dst[:], in0=lg[:, :, 4 + 4 * g:8 + 4 * g], in1=rt["OH"][:, :, g:g + 1].to_broadcast([128, 16, 4]), op=ALU.mult),
          ["lg", "OH"], ["EL" if g == 0 else "TM"])
        if g > 0:
            V(lambda e: e.tensor_tensor(out=rt["EL"][:], in0=rt["EL"][:], in1=rt["TM"][:], op=ALU.add), ["EL", "TM"], ["EL"])
    V(lambda e: e.reduce_max(out=rv["M1"][:], in_=rt["EL"][:], axis=AXX), ["EL"], ["M1"])
    V(lambda e: e.tensor_tensor(out=rt["MK"][:], in0=rt["EL"][:], in1=B4(rv["M1"][:]), op=ALU.is_equal), ["EL", "M1"], ["MK"])
    V(lambda e: e.scalar_tensor_tensor(out=rt["MK"][:], in0=rt["MK"][:], scalar=NEG, in1=rt["EL"][:], op0=ALU.mult, op1=ALU.add), ["MK", "EL"], ["MK"])
    V(lambda e: e.reduce_max(out=rv["M2"][:], in_=rt["MK"][:], axis=AXX), ["MK"], ["M2"])
    V(lambda e: e.tensor_tensor(out=rt["SEL"][:], in0=rt["EL"][:], in1=B4(rv["M2"][:]), op=ALU.is_ge), ["EL", "M2"], ["SEL"])
    V(lambda e: e.tensor_tensor(out=rt["EX"][:], in0=rt["EL"][:], in1=B4(rv["M1"][:]), op=ALU.subtract), ["EL", "M1"], ["EX"])
    T.op("act", lambda e: e.activation(out=rt["EX"][:], in_=rt["EX"][:], func=AF.Exp), reads=["EX"], writes=["EX"])
    V(lambda e: e.tensor_tensor(out=rt["EX"][:], in0=rt["EX"][:], in1=rt["SEL"][:], op=ALU.mult), ["EX", "SEL"], ["EX"])
    V(lambda e: e.reduce_sum(out=rv["SE"][:], in_=rt["EX"][:], axis=AXX), ["EX"], ["SE"])
    V(lambda e: e.reciprocal(out=rv["RS"][:], in_=rv["SE"][:]), ["SE"], ["RS"])
    V(lambda e: e.tensor_tensor(out=rv["WS"][:], in0=rv["RS"][:], in1=rv["GW"][:], op=ALU.mult), ["RS", "GW"], ["WS"])
    V(lambda e: e.tensor_tensor(out=rt["EX"][:], in0=rt["EX"][:], in1=B4(rv["WS"][:]), op=ALU.mult), ["EX", "WS"], ["EX"])
    for g in range(4):
        V(lambda e, g=g: e.tensor_tensor(out=comb[:, :, 4 * g:4 * g + 4], in0=rt["EX"][:], in1=rt["OH"][:, :, g:g + 1].to_broadcast([128, 16, 4]), op=ALU.mult),
          ["EX", "OH"], ["comb"])

    msteps = [(ex, s_) for ex in range(16) for s_ in range(4)]
    def moe_gu(i):
        ex, s_ = msteps[i]
        wg, wu, wd = wexp[ex % 2]; wk = "wexp%d" % (ex % 2)
        if s_ == 0:
            T.dma("pool", wg[:], w_gate[ex].rearrange("(kc p) f -> p kc f", p=128), writes=[wk + "g"])
            T.dma("pool", wu[:], w_up[ex].rearrange("(kc p) f -> p kc f", p=128), writes=[wk + "u"])
            T.dma("pool", wd[:], w_down[ex].rearrange("(fc p) d -> p fc d", p=128), writes=[wk + "d"])
            T.op("pool", lambda e: e.tensor_tensor(out=wd[:], in0=wd[:], in1=g2b[:].unsqueeze(1).to_broadcast([128, 4, D]), op=ALU.mult),
                 reads=[wk + "d", "g2b"], writes=[wk + "d"])
        hT = hidT[i % 2]; hk2 = "hidT%d" % (i % 2)
        tsl = slice(s_ * 512, (s_ + 1) * 512)
        for fc in range(4):
            pg = psn(0, 4); pu = psn(0, 4)
            T.group("pe", [lambda e, kc=kc, pg=pg, fc=fc: e.matmul(PS[pg][:, :], lhsT=wg[:, kc, fc * 128:(fc + 1) * 128], rhs=hx2T[:, kc, tsl], start=(kc == 0), stop=(kc == 7))
                           for kc in range(8)], reads=["hx2T", wk + "g"], writes=["ps%d" % pg])
            T.group("pe", [lambda e, kc=kc, pu=pu, fc=fc: e.matmul(PS[pu][:, :], lhsT=wu[:, kc, fc * 128:(fc + 1) * 128], rhs=hx2T[:, kc, tsl], start=(kc == 0), stop=(kc == 7))
                           for kc in range(8)], reads=["hx2T", wk + "u"], writes=["ps%d" % pu])
            T.op("act", lambda e, pg=pg, fc=fc: e.activation(out=sg[:, fc, :], in_=PS[pg][:, :], func=AF.Silu), reads=["ps%d" % pg], writes=["sg%d" % fc])
            T.op("dve", lambda e, pu=pu, fc=fc, hT=hT: e.tensor_tensor(out=hT[:, fc, :], in0=PS[pu][:, :], in1=sg[:, fc, :], op=ALU.mult),
                 reads=["ps%d" % pu, "sg%d" % fc], writes=[hk2 + "_%d" % fc])
    def moe_down(i):
        ex, s_ = msteps[i]
        wg, wu, wd = wexp[ex % 2]; wk = "wexp%d" % (ex % 2)
        hT = hidT[i % 2]; hk2 = "hidT%d" % (i % 2)
        for tt in range(4):
            t = 4 * s_ + tt
            for hf in range(2):
                py = psn(4, 7)
                T.group("pe", [lambda e, fc=fc, py=py, tt=tt, hf=hf: e.matmul(PS[py][:, :], lhsT=hT[:, fc, tt * 128:(tt + 1) * 128], rhs=wd[:, fc, hf * 512:(hf + 1) * 512],
                                                                         start=(fc == 0), stop=(fc == 3)) for fc in range(4)],
                        reads=[hk2 + "_%d" % fc for fc in range(4)] + [wk + "d"], writes=["ps%d" % py])
                T.op("dve", lambda e, py=py, hf=hf, t=t: e.scalar_tensor_tensor(out=xnew[:, t, hf * 512:(hf + 1) * 512], in0=PS[py][:, :], scalar=comb[:, t, ex:ex + 1],
                                                                         in1=xnew[:, t, hf * 512:(hf + 1) * 512], op0=ALU.mult, op1=ALU.add),
                     reads=["ps%d" % py, "comb", "xnew%d" % t], writes=["xnew%d" % t])
    for i in range(len(msteps) + 1):
        if i < len(msteps):
            moe_gu(i)
        if i >= 1:
            moe_down(i - 1)

    for t in range(16):
        ob = ofin[t % 2]; ok = "ofin%d" % (t % 2)
        T.op("act", lambda e, ob=ob, t=t: e.activation(out=ob[:], in_=xnew[:, t, :], func=AF.Square, accum_out=ss[:, 32 + t:33 + t]), reads=["xnew%d" % t], writes=[ok, "ss3_%d" % t])
        T.op("act", lambda e, t=t: e.activation(out=sd[:, 32 + t:33 + t], in_=ss[:, 32 + t:33 + t], func=AF.Sqrt, bias=epsc[:], scale=1.0 / D), reads=["ss3_%d" % t, "epsc"], writes=["sd3_%d" % t])
        T.op("dve", lambda e, t=t: e.reciprocal(out=rstd[:, 32 + t:33 + t], in_=sd[:, 32 + t:33 + t]), reads=["sd3_%d" % t], writes=["rstd3_%d" % t])
        T.op("dve", lambda e, ob=ob, t=t: e.scalar_tensor_tensor(out=ob[:], in0=xnew[:, t, :], scalar=rstd[:, 32 + t:33 + t], in1=gfb[:], op0=ALU.mult, op1=ALU.mult),
             reads=["xnew%d" % t, "rstd3_%d" % t, "gfb"], writes=[ok])
        T.dma("sp", out[t * 128:(t + 1) * 128, :], ob[:], reads=[ok], key="outst%d" % (t % 2))
    return finish(nc, T, dbg_out, out)


def finish(nc, T, dbg_out, out):
    for name, (tens, shape, dt) in dbg_out.items():
        d = nc.dram_tensor("dbg_" + name, list(shape), dt, kind="ExternalOutput").ap()
        idx = tuple(slice(None) for _ in shape)
        T.dma("sp", d[idx], tens[idx], reads=[name], key="dbg_" + name)
    T.finish()
    return nc


GRID_W = 64
_NC_CACHE = {}


def _rope_tables(rows, cols):
    half = 16
    inv_freq = (10000.0 ** (-np.arange(0, half, 2, dtype=np.float32) / half)).astype(np.float32)
    ang = np.concatenate([rows.astype(np.float32)[:, None] * inv_freq, cols.astype(np.float32)[:, None] * inv_freq], axis=-1)
    return np.cos(ang).astype(np.float32), np.sin(ang).astype(np.float32)


def _core_layout(j):
    R0 = 32 * j
    tok = np.full(NSLOT, -1, np.int64)
    tok[0:2048] = np.arange(R0 * 64, (R0 + 32) * 64)
    used = np.zeros(8192, bool); used[R0 * 64:(R0 + 32) * 64] = True
    for i, r in enumerate(list(range(R0 - 4, R0)) + list(range(R0 + 32, R0 + 36))):
        if 0 <= r < 128:
            tok[2048 + i * 64:2048 + (i + 1) * 64] = np.arange(r * 64, (r + 1) * 64)
            used[r * 64:(r + 1) * 64] = True
    tok[2560:2816] = -2 - np.arange(256)
    others = np.nonzero(~used)[0]
    tok[2816:2816 + len(others)] = others
    return tok


def _host_inputs(inp):
    x = np.asarray(inp["x"], np.float32); ctx = np.asarray(inp["ctx"], np.float32)
    w_in = np.ascontiguousarray(np.asarray(inp["w_in"], np.float32)[0])
    w_uq = np.ascontiguousarray(np.asarray(inp["w_uq"], np.float32)[0])
    perm = np.concatenate([np.arange(16, 32), np.arange(0, 16)])
    w_krs = np.ascontiguousarray(w_in[:, C_KR:C_KR + 32][:, perm])
    w_uqs = np.ascontiguousarray(w_uq.reshape(384, 8, 96)[:, :, 64:96][:, :, perm])
    rel = np.asarray(inp["na_rel_bias"], np.float32)[0]
    a = np.arange(2)[:, None, None, None]; ck = np.arange(64)[None, :, None, None]
    i = np.arange(8)[None, None, :, None]; cq = np.arange(64)[None, None, None, :]
    cstart = np.clip(cq - 8, 0, 48)
    col_in = (ck >= cstart) & (ck < cstart + 16)
    dc = ck - cq + 15
    nab = np.full((8, 8, 2, 64, 8, 64), NEG, np.float32)
    for c in range(8):
        dr = 2 * c + a - i + 3
        ok = (dr >= 0) & (dr <= 14) & col_in
        okb = np.broadcast_to(ok, (2, 64, 8, 64))
        drb = np.broadcast_to(np.clip(dr, 0, 14), (2, 64, 8, 64)); dcb = np.broadcast_to(np.clip(dc, 0, 30), (2, 64, 8, 64))
        for h in range(8):
            g = rel[h][drb, dcb]
            nab[h, c] = np.where(okb, g, np.float32(NEG))
    nab = np.ascontiguousarray(nab.reshape(8, 8, 128, 512))
    rowsel = np.zeros((16, 8, 2, 64), np.float32)
    for c in range(8):
        for aa in range(2):
            rowsel[2 * c + aa, c, aa, :] = 1.0
    rowsel = rowsel.reshape(16, 8, 128)
    common = dict(
        w_mod=np.ascontiguousarray(np.asarray(inp["w_mod"], np.float32)[0]), b_mod=np.ascontiguousarray(np.asarray(inp["b_mod"], np.float32)[0]),
        g_attn=np.ascontiguousarray(np.asarray(inp["norm_attn_g"], np.float32)[0]), g_ffn=np.ascontiguousarray(np.asarray(inp["norm_ffn_g"], np.float32)[0]),
        g_fin=np.asarray(inp["final_norm_g"], np.float32), w_in=w_in, w_krs=w_krs,
        gq=np.ascontiguousarray(np.asarray(inp["q_a_norm_g"], np.float32)[0]), gkv=np.ascontiguousarray(np.asarray(inp["kv_a_norm_g"], np.float32)[0]),
        w_uq=w_uq, w_uqs=w_uqs, w_ukv=np.ascontiguousarray(np.asarray(inp["w_ukv"], np.float32)[0]),
        w_out=np.ascontiguousarray(np.asarray(inp["w_out"], np.float32)[0]),
        w_r=np.ascontiguousarray(np.concatenate([np.asarray(inp["w_router_group"], np.float32)[0], np.asarray(inp["w_router_expert"], np.float32)[0]], axis=1)),
        b_r=np.ascontiguousarray(np.concatenate([np.asarray(inp["b_router_group"], np.float32)[0], np.asarray(inp["b_router_expert"], np.float32)[0]])),
        w_gate=np.ascontiguousarray(np.asarray(inp["w_gate"], np.float32)[0]), w_up=np.ascontiguousarray(np.asarray(inp["w_up"], np.float32)[0]),
        w_down=np.ascontiguousarray(np.asarray(inp["w_down"], np.float32)[0]),
        nab=nab,
    )
    maps = []
    for core in range(8):
        b, j = core // 4, core % 4
        R0 = 32 * j
        tok = _core_layout(j)
        xs = np.zeros((NSLOT, D), np.float32)
        m = tok >= 0
        xs[m] = x[b][tok[m]]
        xs[2560:2816] = ctx[b]
        kmask = np.where(tok == -1, np.float32(NEG), np.float32(0)).astype(np.float32).reshape(NT, 128).T.copy()
        rows = np.where(m, tok // GRID_W, 0); cols = np.where(m, tok % GRID_W, 0)
        cos, sin = _rope_tables(rows, cols)
        cos[~m] = 1.0; sin[~m] = 0.0
        ctab = np.ascontiguousarray(np.concatenate([cos, cos], axis=1).T)
        stab = np.ascontiguousarray(np.concatenate([sin, sin], axis=1).T)
        narm = np.full((16, 4, 8, 64), NEG, np.float32)
        for g in range(4):
            for i_ in range(8):
                r = R0 + 8 * g + i_
                start = min(max(r - 4, 0), 120)
                for lp in range(16):
                    kr = R0 - 4 + 8 * g + lp
                    if start <= kr < start + 8 and 0 <= kr < 128:
                        narm[lp, g, i_, :] = 0.0
        d = dict(common)
        RM = narm[:, :, :, 0]
        rmc = np.zeros((128, 4, 8, 8), np.float32)
        for c_ in range(8):
            for a_ in range(2):
                rmc[a_ * 64:(a_ + 1) * 64, :, c_, :] = RM[2 * c_ + a_][None, :, :]
        d.update(xs=xs, rmc=np.ascontiguousarray(rmc.reshape(128, 4, 64)), cvec=np.ascontiguousarray(np.stack([np.asarray(inp["c"], np.float32)[b], np.asarray(inp["c_ctx"], np.float32)])),
                 kmask=kmask, ctab=ctab, stab=stab)
        maps.append(d)
    return maps


def kernel(**inputs):
    if "nc" not in _NC_CACHE:
        _NC_CACHE["nc"] = build()
    nc = _NC_CACHE["nc"]
    maps = _host_inputs(inputs)
    res = run_bass_kernel_spmd(nc, maps, core_ids=list(range(8)))
    out = np.zeros((2, 8192, D), np.float32)
    for core in range(8):
        b, j = core // 4, core % 4
        out[b, j * 2048:(j + 1) * 2048] = res.results[core]["out"]
    return out
```

```python
import numpy as np
import concourse.bass as bass
import concourse.mybir as mybir
from concourse.bass_utils import run_bass_kernel_spmd

F32, BF16 = mybir.dt.float32, mybir.dt.bfloat16
AF = mybir.ActivationFunctionType
ALU = mybir.AluOpType

D = 1024
NT = 68
NT_MLA = 66
NS = 17
NSLOT = NT * 128
QC0, KV0 = 0, 896
C_KVLAT, C_KR, C_NK, C_NV = 896, 1152, 1184, 1696
EPS = 1e-6
import os
EVAC_MODE = int(os.environ.get('EVAC_MODE', '2'))
NEG = -1e30


class Trk:
    def __init__(self, nc):
        self.nc = nc
        self.eng = {}
        for name, h in (("pe", nc.tensor), ("act", nc.scalar), ("dve", nc.vector), ("pool", nc.gpsimd), ("sp", nc.sync)):
            self.eng[name] = dict(h=h, sem=nc.alloc_semaphore("s_" + name), cnt=0, seen={})
        self.res = {}
        self.dsem = {}

    def _r(self, k):
        if k not in self.res:
            self.res[k] = dict(w=None, r={})
        return self.res[k]

    def _waits(self, en, reads, writes):
        e = self.eng[en]
        need = {}
        def add(ev):
            if ev is None:
                return
            sem, val, src = ev
            if src == "pe" and en == "pe":
                return
            if need.get(sem.name, (None, 0))[1] < val:
                need[sem.name] = (sem, val)
        for k in reads:
            add(self._r(k)["w"])
        for k in writes:
            r = self._r(k)
            add(r["w"])
            for ev in r["r"].values():
                add(ev)
        for sn, (sem, val) in need.items():
            if e["seen"].get(sn, 0) < val:
                e["h"].wait_ge(sem, val)
                e["seen"][sn] = val

    def _mark(self, ev, reads, writes):
        for k in writes:
            r = self._r(k)
            r["w"] = ev
            r["r"] = {}
        for k in reads:
            r = self._r(k)
            r["r"][ev[0].name + ev[2]] = ev

    def op(self, en, fn, reads=(), writes=()):
        e = self.eng[en]
        self._waits(en, reads, writes)
        ins = fn(e["h"])
        e["cnt"] += 1
        ins.then_inc(e["sem"], 1)
        self._mark((e["sem"], e["cnt"], en), reads, writes)

    def group(self, en, fns, reads=(), writes=()):
        e = self.eng[en]
        self._waits(en, reads, writes)
        ins = None
        for fn in fns:
            ins = fn(e["h"])
        e["cnt"] += 1
        ins.then_inc(e["sem"], 1)
        self._mark((e["sem"], e["cnt"], en), reads, writes)

    def dma(self, qn, out, in_, reads=(), writes=(), key=None):
        e = self.eng[qn]
        self._waits(qn, reads, writes)
        key = key or (writes[0] if writes else reads[0])
        if key not in self.dsem:
            self.dsem[key] = [self.nc.alloc_semaphore("d%d" % len(self.dsem)), 0]
        ds = self.dsem[key]
        e["h"].dma_start(out=out, in_=in_).then_inc(ds[0], 16)
        ds[1] += 16
        self._mark((ds[0], ds[1], "dma"), reads, writes)

    def barrier(self):
        for en, e in self.eng.items():
            for fn, f in self.eng.items():
                if fn != en and f["cnt"] > e["seen"].get(f["sem"].name, 0):
                    e["h"].wait_ge(f["sem"], f["cnt"])
                    e["seen"][f["sem"].name] = f["cnt"]
            for k, (sem, tot) in self.dsem.items():
                if tot > e["seen"].get(sem.name, 0):
                    e["h"].wait_ge(sem, tot)
                    e["seen"][sem.name] = tot

    def finish(self):
        e = self.eng["sp"]
        for fn, f in self.eng.items():
            if fn != "sp" and f["cnt"] > e["seen"].get(f["sem"].name, 0):
                e["h"].wait_ge(f["sem"], f["cnt"])
        for k, (sem, tot) in self.dsem.items():
            if tot > e["seen"].get(sem.name, 0):
                e["h"].wait_ge(sem, tot)


def na_row_slot(l):
    if 4 <= l < 36:
        return (l - 4) * 64
    if l < 4:
        return 2048 + l * 64
    return 2304 + (l - 36) * 64


def build(stop_after=None, dbg=None):
    nc = bass.Bass("TRN2", target_bir_lowering=False)
    T = Trk(nc)

    def din(name, shape, dt=F32):
        return nc.dram_tensor(name, list(shape), dt, kind="ExternalInput").ap()

    xs = din("xs", [NSLOT, D])
    cvec = din("cvec", [2, D])
    w_mod = din("w_mod", [D, 6 * D]); b_mod = din("b_mod", [6 * D])
    g_attn = din("g_attn", [D]); g_ffn = din("g_ffn", [D]); g_fin = din("g_fin", [D])
    w_in = din("w_in", [D, 2208]); w_krs = din("w_krs", [D, 32])
    gq = din("gq", [384]); gkv = din("gkv", [256])
    w_uq = din("w_uq", [384, 768]); w_uqs = din("w_uqs", [384, 8, 32]); w_ukv = din("w_ukv", [256, 1024])
    w_out = din("w_out", [D, D])
    w_r = din("w_r", [D, 20]); b_r = din("b_r", [20])
    w_gate = din("w_gate", [16, D, 512]); w_up = din("w_up", [16, D, 512]); w_down = din("w_down", [16, 512, D])
    kmask_d = din("kmask", [128, NT])
    ctab = din("ctab", [32, NSLOT]); stab = din("stab", [32, NSLOT])
    nab = din("nab", [8, 8, 128, 512]); rmc_d = din("rmc", [128, 4, 64])
    out = nc.dram_tensor("out", [2048, D], F32, kind="ExternalOutput").ap()
    dbg_out = {}

    def sbat(name, shape, dt, at):
        nbytes = int(np.prod(shape[1:])) * (2 if dt == BF16 else 4)
        assert at % 32 == 0 and at + nbytes <= 229344, (name, at, nbytes)
        return nc.alloc_sbuf_tensor_at(name, list(shape), dt, offset=at)
    cur = [16512]
    def sb(name, shape, dt):
        nbytes = (int(np.prod(shape[1:])) * (2 if dt == BF16 else 4) + 31) // 32 * 32
        off = cur[0]; cur[0] += nbytes
        return sbat(name, shape, dt, off)
    ident = sb("ident", [128, 128], BF16)
    ones_bf = sb("ones_bf", [128, 128], BF16)
    onesf = sb("onesf", [128, 128], F32)
    epsc = sb("epsc", [128, 1], F32)
    modc = sb("modc", [128, 64], F32)
    gqc = sb("gqc", [128, 4], F32); gkvc = sb("gkvc", [128, 2], F32)
    kmask = sb("kmask_s", [128, NT], F32)
    ss = sb("ss", [128, NT], F32); sd = sb("sd", [128, NT], F32); rstd = sb("rstd", [128, NT], F32)
    rmc = sb("rmc_s", [128, 4, 64], BF16)
    shiftm = sb("shiftm", [64, 128], BF16)
    assert cur[0] <= 25728, cur[0]
    TMP = 25728
    cur[0] = TMP
    tmpf = [sb("tmpf%d" % i, [128, 512], F32) for i in range(3)]
    sq = [sb("sq%d" % i, [128, 512], BF16) for i in range(3)]
    sdb = sb("sdb", [128, 512], F32)
    rb = sb("rb", [128, 512], F32)
    ctt = sb("ctt", [128, 512], F32); stt = sb("stt", [128, 512], F32)
    r2 = sb("r2", [128, 512], F32)
    krot_tmp = sb("krot_tmp", [128, 3072], BF16)
    assert cur[0] <= 52352, cur[0]
    A0 = 52352
    kvn = sbat("kvn", [128, 2, NSLOT], BF16, A0)
    qlatn = sbat("qlatn", [128, 3, 2048], BF16, A0 + 34816)
    NR = A0 + 34816 + 12288
    KN = sbat("KN", [128, 4, 2816], BF16, NR)
    VN = sbat("VN", [128, 22, 8, 65], BF16, NR + 22528)
    QN = sbat("QN", [128, 4, 2048], BF16, NR + 22528 + 22880)
    NR_END = NR + 22528 + 22880 + 16384
    KH = [sbat("KH%d" % i, [128, NSLOT], BF16, NR + i * 17408) for i in range(2)]
    VH = [sbat("VH%d" % i, [128, NT, 65], BF16, NR + 34816 + i * 8864) for i in range(2)]
    QH = [sbat("QH%d" % i, [128, 2048], BF16, NR + 34816 + 17728 + i * 4096) for i in range(2)]
    assert NR + 34816 + 17728 + 8192 <= NR_END
    ATR = NR_END
    AT = sbat("AT", [128, 8, 2048], BF16, ATR)
    TAIL = ATR + 32768
    assert TAIL == 194016, TAIL
    cur[0] = TAIL
    PT2 = [sb("PT%d" % i, [128, 1024], BF16) for i in range(4)]
    osb = [sbat("osb%d" % i, [128, 512], F32, TMP + i * 2048) for i in range(2)]
    rc = sbat("rc", [128, 512], F32, TMP + 4096)
    onbs = [sbat("onb%d" % i, [64, 512], BF16, TMP + 6144 + i * 1024) for i in range(2)]
    ATT_W = cur[0]
    class Half:
        def __init__(self, t, off):
            self.t, self.off = t, off
        def __getitem__(self, key):
            r, c = key
            a = (c.start or 0) + self.off
            b_ = (c.stop if c.stop is not None else 512) + self.off
            return self.t[r, a:b_]
    PP = [nc.alloc_psum_tensor("pp%d" % i, [128, 1024], F32) for i in range(2)]
    PS = [nc.alloc_psum_tensor("ps%d" % i, [128, 512], F32) for i in range(4)]
    PS += [Half(PP[0], 0), Half(PP[0], 512), Half(PP[1], 0)]
    pTt = PP[1][:, 512:1024].bitcast(BF16)
    rr = [0]
    def psn(lo=0, hi=8):
        i = lo + rr[0] % (hi - lo); rr[0] += 1
        return i
    gscr = nc.dram_tensor("gscr", [2, D], F32).ap()

    T.op("pool", lambda e: e.memset(ident[:], 0.0), writes=["ident"])
    T.op("pool", lambda e: e.affine_select(out=ident[:], in_=ident[:], pattern=[[-1, 128]], compare_op=ALU.not_equal,
                                           fill=1.0, base=0, channel_multiplier=1), reads=["ident"], writes=["ident"])
    T.op("pool", lambda e: e.memset(shiftm[:], 0.0), writes=["shiftm"])
    T.op("pool", lambda e: e.affine_select(out=shiftm[:], in_=shiftm[:], pattern=[[-1, 128]], compare_op=ALU.not_equal,
                                           fill=1.0, base=64, channel_multiplier=1), reads=["shiftm"], writes=["shiftm"])
    T.op("pool", lambda e: e.memset(ones_bf[:], 1.0), writes=["ones_bf"])
    T.op("pool", lambda e: e.memset(onesf[:], 1.0), writes=["onesf"])
    T.op("pool", lambda e: e.memset(epsc[:], EPS), writes=["epsc"])
    T.op("pool", lambda e: e.memset(ss[:], 0.0), writes=["ss_all"])
    T.dma("sp", kmask[:], kmask_d[:, :], writes=["kmask"])
    T.dma("pool", rmc[:], rmc_d[:, :, :], writes=["rmc"])
    with nc.allow_non_contiguous_dma(reason="tiny column loads"):
        T.dma("sp", gqc[:, 0:3], gq.rearrange("(c p) -> p c", p=128), writes=["gqc"])
        T.dma("sp", gkvc[:, 0:2], gkv.rearrange("(c p) -> p c", p=128), writes=["gkvc"])

    PTR = 7
    pT = pTt

    def rms_feat(ps_list, nfeat, gcol, gkey, dst_fn, dkey):
        n = len(ps_list)
        for c, pi in enumerate(ps_list):
            T.op("act", lambda e, c=c, pi=pi: e.activation(out=tmpf[c][:], in_=PS[pi][:, :], func=AF.Copy), reads=["ps%d" % pi], writes=["tmpf%d" % c])
            T.op("act", lambda e, c=c: e.activation(out=sq[c][:], in_=tmpf[c][:], func=AF.Square), reads=["tmpf%d" % c], writes=["sq%d" % c])
        pq = psn(0, 7)
        T.group("pe", [lambda e, c=c, pq=pq: e.matmul(PS[pq][:, :], lhsT=ones_bf[:], rhs=sq[c][:], start=(c == 0), stop=(c == n - 1)) for c in range(n)],
                reads=["ones_bf"] + ["sq%d" % c for c in range(n)], writes=["ps%d" % pq])
        T.op("act", lambda e, pq=pq: e.activation(out=sdb[:], in_=PS[pq][:, :], func=AF.Sqrt, bias=epsc[:], scale=1.0 / nfeat),
             reads=["ps%d" % pq, "epsc"], writes=["sdb"])
        T.op("dve", lambda e: e.reciprocal(out=rb[:], in_=sdb[:]), reads=["sdb"], writes=["rb"])
        for c in range(n):
            T.op("dve", lambda e, c=c: e.scalar_tensor_tensor(out=dst_fn(c), in0=tmpf[c][:], scalar=gcol[:, c:c + 1], in1=rb[:], op0=ALU.mult, op1=ALU.mult),
                 reads=["tmpf%d" % c, "rb", gkey], writes=[dkey])

    def norm_a(t, xb, xk, nb, nk, rkey):
        T.op("act", lambda e: e.activation(out=nb[:], in_=xb, func=AF.Square, accum_out=ss[:, t:t + 1]), reads=[xk], writes=[nk, "ss%d" % t])
        T.op("act", lambda e: e.activation(out=sd[:, t:t + 1], in_=ss[:, t:t + 1], func=AF.Sqrt, bias=epsc[:], scale=1.0 / D),
             reads=["ss%d" % t, "epsc"], writes=["sd%d" % t])
        T.op("dve", lambda e: e.reciprocal(out=rstd[:, t:t + 1], in_=sd[:, t:t + 1]), reads=["sd%d" % t], writes=[rkey])
        T.op("act", lambda e: e.activation(out=nb[:], in_=xb, func=AF.Identity, scale=rstd[:, t:t + 1]), reads=[xk, rkey], writes=[nk])

    def norm_b(nb, nk, dst_fn, dkeys, cb):
        T.group("pe", [lambda e, kc=kc: e.transpose(out=pT[:, kc * 128:(kc + 1) * 128], in_=nb[:, kc * 128:(kc + 1) * 128], identity=ident[:])
                       for kc in range(8)], reads=[nk, "ident"], writes=["ps%d" % PTR])
        for kc in range(8):
            dst = dst_fn(kc); src = pT[:, kc * 128:(kc + 1) * 128]
            T.op("dve", lambda e, dst=dst, src=src, kc=kc: e.tensor_scalar(out=dst, in0=src, scalar1=modc[:, cb + kc:cb + kc + 1],
                                                                     scalar2=modc[:, cb + 8 + kc:cb + 9 + kc], op0=ALU.mult, op1=ALU.add),
                 reads=["ps%d" % PTR, "modc"], writes=[dkeys[kc]])

    def norm_transpose(src_ap, t, xb, xk, nb, nk, dst_fn, dkeys, cb, rkey):
        norm_a(t, xb, xk, nb, nk, rkey)
        norm_b(nb, nk, dst_fn, dkeys, cb)

    def phaseA(s_list, base, full, krot_dst, xn_base=None, state=None, load_only=False):
        if state is not None:
            return phaseA_run(s_list, full, krot_dst, state)
        o = [base]
        def wa(name, shape, dt):
            nbytes = (int(np.prod(shape[1:])) * (2 if dt == BF16 else 4) + 31) // 32 * 32
            t_ = sbat(name, shape, dt, o[0]); o[0] += nbytes
            return t_
        sfx = "f" if full else "p"
        ncols = 2208 if full else 256
        win_b = wa("win_b" + sfx, [128, 8, ncols], BF16)
        wkr_b = wa("wkr_b" + sfx, [128, 8, 96], BF16)
        wkrs_b = wa("wkrs_b" + sfx, [128, 8, 96], BF16)
        xt = [wa("xt%d%s" % (i, sfx), [128, D], F32) for i in range(2)]
        if xn_base is not None:
            xt += [sbat("xt%d%s" % (2 + i, sfx), [128, D], F32, xn_base + 4096 + i * 4096) for i in range(2)]
        if xn_base is None:
            xn = [wa("xn%d%s" % (i, sfx), [128, D], BF16) for i in range(2)]
        else:
            xn = [sbat("xn%d%s" % (i, sfx), [128, D], BF16, xn_base + i * 2048) for i in range(2)]
        hxT = [wa("hxT%d%s" % (i, sfx), [128, 8, 512], BF16) for i in range(2)]
        assert o[0] <= 229344, o[0]
        wkey = "win_b"
        if full:
            for kc in range(8):
                T.dma("pool", win_b[:, kc, :], w_in[kc * 128:(kc + 1) * 128, :], writes=[wkey])
            cko = C_KVLAT
        else:
            T.dma("pool", win_b[:], w_in[:, C_KVLAT:C_KVLAT + 256].rearrange("(kc p) c -> p kc c", p=128), writes=[wkey])
            cko = 0
        T.op("pool", lambda e: e.memset(wkr_b[:], 0.0), writes=["wkr_b"])
        T.op("pool", lambda e: e.memset(wkrs_b[:], 0.0), writes=["wkrs_b"])
        T.dma("pool", wkr_b[:, :, 64:96], w_in[:, C_KR:C_KR + 32].rearrange("(kc p) c -> p kc c", p=128), writes=["wkr_b"])
        T.dma("pool", wkrs_b[:, :, 64:96], w_krs.rearrange("(kc p) c -> p kc c", p=128), writes=["wkrs_b"])
        T.op("pool", lambda e: e.tensor_scalar(out=wkrs_b[:, :, 64:80], in0=wkrs_b[:, :, 64:80], scalar1=-1.0, scalar2=None, op0=ALU.mult),
             reads=["wkrs_b"], writes=["wkrs_b"])
        state_ = dict(win_b=win_b, wkr_b=wkr_b, wkrs_b=wkrs_b, xt=xt, xn=xn, hxT=hxT, wkey=wkey, cko=cko)
        if load_only:
            return state_
        return phaseA_run(s_list, full, krot_dst, state_)

    def phaseA_run(s_list, full, krot_dst, st_):
        win_b, wkr_b, wkrs_b, xt, xn, hxT, wkey, cko = (st_[k] for k in ("win_b", "wkr_b", "wkrs_b", "xt", "xn", "hxT", "wkey", "cko"))
        if stop_after == "paw":
            return "stop"
        def tinfo(s, tt):
            t = 4 * s + tt
            nx = len(xt)
            return t, xt[t % nx], "xt%d" % (t % nx), xn[t % 2], "xn%d" % (t % 2)
        def nt_dma(s, tt):
            t, xb, xk, nb, nk = tinfo(s, tt)
            T.dma("sp", xb[:], xs[t * 128:(t + 1) * 128, :], writes=[xk])
        def nt_a(s, tt):
            t, xb, xk, nb, nk = tinfo(s, tt)
            norm_a(t, xb[:], xk, nb, nk, "rstd%d" % t)
        def nt_b(s, tt):
            t, xb, xk, nb, nk = tinfo(s, tt)
            hb = hxT[s % 2]; hk = "hxT%d" % (s % 2)
            hks = [hk + "_%d" % kc for kc in range(8)]
            cb = 32 if t in (20, 21) else 0
            norm_b(nb, nk, lambda kc, tt=tt, hb=hb: hb[:, kc, tt * 128:(tt + 1) * 128], hks, cb)

        def mm_parts(s):
            hb = hxT[s % 2]; hk = "hxT%d" % (s % 2)
            hks = [hk + "_%d" % kc for kc in range(8)]
            sl = slice(s * 512, (s + 1) * 512)
            def p0():
                pl = []
                for c in range(2):
                    pi = psn(0, 7); pl.append(pi)
                    T.group("pe", [lambda e, kc=kc, pi=pi, c=c: e.matmul(PS[pi][:, :], lhsT=win_b[:, kc, cko + c * 128:cko + (c + 1) * 128], rhs=hb[:, kc, :],
                                                                     start=(kc == 0), stop=(kc == 7)) for kc in range(8)],
                            reads=hks + [wkey], writes=["ps%d" % pi])
                rms_feat(pl, 256, gkvc, "gkvc", lambda c: kvn[:, c, sl], "kvn")
                pk = psn(0, 7); pks = psn(0, 7)
                T.group("pe", [lambda e, kc=kc: e.matmul(PS[pk][0:96, :], lhsT=wkr_b[:, kc, :], rhs=hb[:, kc, :], start=(kc == 0), stop=(kc == 7)) for kc in range(8)],
                        reads=hks + ["wkr_b"], writes=["ps%d" % pk])
                T.group("pe", [lambda e, kc=kc: e.matmul(PS[pks][0:96, :], lhsT=wkrs_b[:, kc, :], rhs=hb[:, kc, :], start=(kc == 0), stop=(kc == 7)) for kc in range(8)],
                        reads=hks + ["wkrs_b"], writes=["ps%d" % pks])
                T.dma("sp", ctt[64:96, :], ctab[:, sl], writes=["ctt"])
                T.dma("sp", stt[64:96, :], stab[:, sl], writes=["stt"])
                T.op("dve", lambda e: e.tensor_tensor(out=tmpf[2][64:96, :], in0=PS[pk][64:96, :], in1=ctt[64:96, :], op=ALU.mult), reads=["ps%d" % pk, "ctt"], writes=["tmpf2"])
                T.op("dve", lambda e: e.tensor_tensor(out=r2[64:96, :], in0=PS[pks][64:96, :], in1=stt[64:96, :], op=ALU.mult), reads=["ps%d" % pks, "stt"], writes=["r2"])
                for dst, dk in krot_dst(s):
                    T.op("pool", lambda e, dst=dst: e.tensor_tensor(out=dst, in0=tmpf[2][64:96, :], in1=r2[64:96, :], op=ALU.add), reads=["tmpf2", "r2"], writes=[dk])
            def p1():
                if not (full and s < 6):
                    return
                nsl = slice(s * 512, (s + 1) * 512) if s < 5 else slice(2560, 2816)
                ncol = slice(0, 512) if s < 5 else slice(0, 256)
                for j in range(4):
                    pi = psn(0, 7)
                    T.group("pe", [lambda e, kc=kc, pi=pi, j=j: e.matmul(PS[pi][:, :], lhsT=win_b[:, kc, C_NK + j * 128:C_NK + (j + 1) * 128], rhs=hb[:, kc, :],
                                                                     start=(kc == 0), stop=(kc == 7)) for kc in range(8)],
                            reads=hks + [wkey], writes=["ps%d" % pi])
                    if j % 2 == 0:
                        T.op("act", lambda e, pi=pi, j=j: e.activation(out=KN[:, j, nsl], in_=PS[pi][:, ncol], func=AF.Copy), reads=["ps%d" % pi], writes=["KN"])
                    else:
                        T.op("dve", lambda e, pi=pi, j=j: e.tensor_copy(out=KN[:, j, nsl], in_=PS[pi][:, ncol]), reads=["ps%d" % pi], writes=["KN"])
            def p2():
                if not (full and s < 6):
                    return
                for tt in range(4 if s < 5 else 2):
                    pi = psn(0, 7); t = 4 * s + tt
                    T.group("pe", [lambda e, kc=kc, pi=pi, tt=tt: e.matmul(PS[pi][:, :], lhsT=hb[:, kc, tt * 128:(tt + 1) * 128], rhs=win_b[:, kc, C_NV:C_NV + 512],
                                                                       start=(kc == 0), stop=(kc == 7)) for kc in range(8)],
                            reads=hks + [wkey], writes=["ps%d" % pi])
                    src = PS[pi][:, :].rearrange("p (h d) -> p h d", h=8)
                    if tt % 2 == 0:
                        T.op("dve", lambda e, src=src, t=t: e.tensor_copy(out=VN[:, t, :, 0:64], in_=src), reads=["ps%d" % pi], writes=["VN"])
                    else:
                        T.op("act", lambda e, src=src, t=t: e.activation(out=VN[:, t, :, 0:64], in_=src, func=AF.Copy), reads=["ps%d" % pi], writes=["VN"])
            def p3():
                if not (full and s < 4):
                    return
                pl = []
                for c in range(3):
                    pi = psn(0, 7); pl.append(pi)
                    T.group("pe", [lambda e, kc=kc, pi=pi, c=c: e.matmul(PS[pi][:, :], lhsT=win_b[:, kc, c * 128:(c + 1) * 128], rhs=hb[:, kc, :],
                                                                     start=(kc == 0), stop=(kc == 7)) for kc in range(8)],
                            reads=hks + [wkey], writes=["ps%d" % pi])
                rms_feat(pl, 384, gqc, "gqc", lambda c: qlatn[:, c, sl], "qlatn")
                for j in range(4):
                    pi = psn(0, 7)
                    T.group("pe", [lambda e, kc=kc, pi=pi, j=j: e.matmul(PS[pi][:, :], lhsT=win_b[:, kc, 384 + j * 128:384 + (j + 1) * 128], rhs=hb[:, kc, :],
                                                                     start=(kc == 0), stop=(kc == 7)) for kc in range(8)],
                            reads=hks + [wkey], writes=["ps%d" % pi])
                    T.op("act", lambda e, pi=pi, j=j: e.activation(out=QN[:, j, sl], in_=PS[pi][:, :], func=AF.Copy, scale=0.125), reads=["ps%d" % pi], writes=["QN"])
            return [p0, p1, p2, p3]

        s_list = list(s_list)
        seq = [(s, tt) for s in s_list for tt in range(4)]
        depth = len(xt) - 1
        for k0 in range(min(depth, len(seq))):
            nt_dma(*seq[k0])
        nt_a(*seq[0])
        prev_parts = None
        for k, (s, tt) in enumerate(seq):
            if k + depth < len(seq):
                nt_dma(*seq[k + depth])
            if k + 1 < len(seq):
                nt_a(*seq[k + 1])
            nt_b(s, tt)
            if prev_parts is not None:
                prev_parts[tt]()
            if tt == 3:
                prev_parts = mm_parts(s)
        for p_ in prev_parts:
            p_()

    PA1 = phaseA(None, ATR, True, None, load_only=True)
    ccol = sbat("ccol", [128, 2, 8], F32, A0)
    scol = sbat("scol", [128, 2, 8], BF16, A0 + 64)
    wmb = [sbat("wmb%d" % i, [128, 8, 512], BF16, A0 + 128 + i * 8192) for i in range(2)]
    brow = sbat("brow", [1, 512], F32, A0 + 128 + 16384)
    grow = sbat("grow", [1, 512], F32, A0 + 128 + 16384 + 2048)
    mrow = [sbat("mrow%d" % i, [1, 512], F32, A0 + 128 + 16384 + 4096 + i * 2048) for i in range(2)]
    with nc.allow_non_contiguous_dma(reason="tiny column loads"):
        T.dma("sp", ccol[:], cvec.rearrange("r (kc p) -> p r kc", p=128), writes=["ccol"])
    T.op("act", lambda e: e.activation(out=scol[:], in_=ccol[:], func=AF.Silu), reads=["ccol"], writes=["scol"])
    PCOL = 6
    for j in range(12):
        v, hf = j // 2, j % 2
        wb = wmb[j % 2]; wk = "wmb%d" % (j % 2)
        T.dma("pool", wb[:], w_mod[:, j * 512:(j + 1) * 512].rearrange("(kc p) c -> p kc c", p=128), writes=[wk])
        T.dma("sp", brow[:], b_mod[None, j * 512:(j + 1) * 512], writes=["brow"])
        if v in (1, 4):
            gsrc = g_attn if v == 1 else g_ffn
            T.dma("sp", grow[:], gsrc[None, hf * 512:(hf + 1) * 512], writes=["grow"])
        for r in range(2):
            if r == 1 and v > 1:
                continue
            pi = psn(0, 6)
            T.group("pe", [lambda e, kc=kc, pi=pi, r=r, wb=wb: e.matmul(PS[pi][0:1, :], lhsT=scol[:, r, kc:kc + 1], rhs=wb[:, kc, :],
                                                                    start=(kc == 0), stop=(kc == 7)) for kc in range(8)],
                    reads=["scol", wk], writes=["ps%d" % pi])
            mr = mrow[r]; mk = "mrow%d" % r
            T.op("dve", lambda e, pi=pi, mr=mr: e.tensor_tensor(out=mr[:], in0=PS[pi][0:1, :], in1=brow[:], op=ALU.add),
                 reads=["ps%d" % pi, "brow"], writes=[mk])
            if v in (1, 4):
                T.op("dve", lambda e, mr=mr: e.scalar_tensor_tensor(out=mr[:], in0=mr[:], scalar=1.0, in1=grow[:], op0=ALU.add, op1=ALU.mult),
                     reads=[mk, "grow"], writes=[mk])
            if v in (2, 5):
                gi = 0 if v == 2 else 1
                T.dma("sp", gscr[gi:gi + 1, hf * 512:(hf + 1) * 512], mr[:], reads=[mk], writes=["gscr_r"], key="gscr")
            else:
                if r == 0:
                    base = {1: 0, 0: 8, 4: 16, 3: 24}[v]
                else:
                    base = {1: 32, 0: 40}[v]
                T.group("pe", [lambda e, i=i, mr=mr, base=base, hf=hf: e.matmul(PS[PCOL][:, base + hf * 4 + i: base + hf * 4 + i + 1],
                                                                             lhsT=mr[0:1, i * 128:(i + 1) * 128], rhs=onesf[0:1, 0:1],
                                                                             start=True, stop=True) for i in range(4)],
                        reads=[mk, "onesf"], writes=["pscol"])
    T.op("dve", lambda e: e.tensor_copy(out=modc[:, 0:48], in_=PS[PCOL][:, 0:48]), reads=["pscol"], writes=["modc"])
    if stop_after == "adaln":
        dbg_out["modc"] = (modc, [128, 64], F32)
        return finish(nc, T, dbg_out, out)
    T.barrier()

    T.op("pool", lambda e: e.memset(VN[:, :, :, 64:65], 1.0), writes=["VN"])
    rv = phaseA(range(6), ATR, True, lambda s: [(krot_tmp[64:96, s * 512:(s + 1) * 512], "krot_tmp")], state=PA1)
    if rv == "stop":
        return finish(nc, T, dbg_out, out)
    if stop_after == "phaseA":
        dbg_out["kvn"] = (kvn, [128, 2, NSLOT], BF16); dbg_out["qlatn"] = (qlatn, [128, 3, 2048], BF16)
        dbg_out["KN"] = (KN, [128, 4, 2816], BF16); dbg_out["QN"] = (QN, [128, 4, 2048], BF16)
        dbg_out["VN"] = (VN, [128, 22, 8, 65], BF16); dbg_out["krot_tmp"] = (krot_tmp, [128, 3072], BF16)
        return finish(nc, T, dbg_out, out)
    T.barrier()

    nrm = [0]
    pending = []
    def run_pending(i, flush=False):
        for item in [x for x in pending if flush or x[0] <= i]:
            pending.remove(item)
            item[1]()
    def normalize_out(po, h_at, qsl, i, scr=3, off=0):
        ob = osb[nrm[0] % 2]; ok = "osb%d" % (nrm[0] % 2); onb = onbs[nrm[0] % 2]; onk = "onb%d" % (nrm[0] % 2); nrm[0] += 1
        T.op("dve", lambda e: e.tensor_copy(out=ob[0:65, :], in_=PS[po][0:65, :]), reads=["ps%d" % po], writes=[ok])
        for q4 in range(4):
            pending.append([i + 1 + off + q4, lambda q4=q4: T.op("dve", lambda e: e.reciprocal(out=ob[64:65, q4 * 128:(q4 + 1) * 128], in_=ob[64:65, q4 * 128:(q4 + 1) * 128]),
                                                             reads=[ok], writes=[ok])])
        def st3():
            T.group("pe", [lambda e: e.matmul(PS[scr][:, :], lhsT=shiftm[:], rhs=onb[:], start=True, stop=True)], reads=[onk, "shiftm"], writes=["ps%d" % scr])
            T.op("dve", lambda e: e.tensor_copy(out=AT[64:128, h_at // 2, qsl], in_=PS[scr][64:128, :]), reads=["ps%d" % scr], writes=["AT"])
        def st2():
            T.group("pe", [lambda e: e.matmul(PS[scr][0:64, :], lhsT=onesf[64:65, 0:64], rhs=ob[64:65, :], start=True, stop=True)],
                    reads=[ok, "onesf"], writes=["ps%d" % scr])
            if h_at % 2 == 0:
                T.op("dve", lambda e: e.tensor_tensor(out=AT[0:64, h_at // 2, qsl], in0=ob[0:64, :], in1=PS[scr][0:64, :], op=ALU.mult),
                     reads=[ok, "ps%d" % scr], writes=["AT"])
            else:
                T.op("dve", lambda e: e.tensor_tensor(out=onb[:], in0=ob[0:64, :], in1=PS[scr][0:64, :], op=ALU.mult),
                     reads=[ok, "ps%d" % scr], writes=[onk])
                pending.append([i + 9 + off, st3])
        pending.append([i + 6 + off, st2])

    cur[0] = ATT_W
    nabt = [sb("nabt%d" % i, [128, 8, 512], BF16) for i in range(2)]
    assert cur[0] <= 229344
    stg = [sbat("stg%d" % i, [128, 1024], F32, TMP + 8192 + i * 4096) for i in range(2)]
    combt = [sbat("combt%d" % i, [128, 8, 512], BF16, ATR + i * 8192) for i in range(2)]
    na_steps = [(h, g, p) for h in range(8) for g in range(4) for p in range(5)]
    pti = [0]
    na_pt = {}
    def na_load(h):
        T.dma("pool", nabt[h % 2][:], nab[h].rearrange("c p f -> p c f"), writes=["nabt%d" % (h % 2)])
    def na_comb(h, g):
        k = (h * 4 + g) % 2
        T.op("pool", lambda e: e.tensor_tensor(out=combt[k][:].rearrange("p c (i q) -> p c i q", q=64),
                                               in0=nabt[h % 2][:].rearrange("p c (i q) -> p c i q", q=64),
                                               in1=rmc[:, g, :].rearrange("p (c i) -> p c i", i=8).unsqueeze(3).to_broadcast([128, 8, 8, 64]), op=ALU.add),
             reads=["nabt%d" % (h % 2), "rmc"], writes=["combt%d" % k])
    qz = [[sb("qz%d_%d" % (par, k_), [128, 512], BF16) for k_ in range(2)] for par in range(2)]
    assert cur[0] <= 229344
    for par in range(2):
        for k_ in range(2):
            T.op("pool", lambda e, par=par, k_=k_: e.memset(qz[par][k_][:], 0.0), writes=["qz%d_%d" % (par, k_)])
    def na_qz(h, g):
        par = h % 2; k_ = (h * 4 + g) // 1 % 2
        pb_ = par * 64
        T.op("pool", lambda e: e.tensor_copy(out=qz[par][k_][pb_:pb_ + 64, :], in_=QN[pb_:pb_ + 64, h // 2, g * 512:(g + 1) * 512]),
             reads=["QN"], writes=["qz%d_%d" % (par, k_)])
    na_load(0); na_load(1); na_comb(0, 0); na_qz(0, 0)
    def na_qk(i):
        h, g, p = na_steps[i]
        j = h // 2; pb = (h % 2) * 64
        if p == 0:
            nh, ng = (h, g + 1) if g < 3 else (h + 1, 0)
            if nh < 8:
                if ng == 0 and nh + 1 < 8 and False:
                    pass
                na_comb(nh, ng)
                na_qz(nh, ng)
        if p == 4 and g == 3 and h + 2 < 8:
            na_load(h + 2)
        qsl = slice(g * 512, (g + 1) * 512)
        pp = i % 2
        for half in range(2):
            c = 2 * p + half
            ks = na_row_slot(8 * g + 2 * c) if c < 8 else 2560 + (c - 8) * 128
            dst = PP[pp][:, half * 512:(half + 1) * 512]
            qzb = qz[h % 2][(h * 4 + g) % 2]
            T.group("pe", [lambda e, ks=ks, dst=dst: e.matmul(dst, lhsT=KN[:, j, ks:ks + 128], rhs=qzb[:, :], start=True, stop=True)],
                    reads=["KN", "qz%d_%d" % (h % 2, (h * 4 + g) % 2)], writes=["ps%d" % (4 + 2 * pp + half)])
        k = pti[0] % 4; pti[0] += 1
        na_pt[i] = k
        pk_ = ["ps%d" % (4 + 2 * pp), "ps%d" % (5 + 2 * pp)]
        if p < 4:
            ck = (h * 4 + g) % 2
            sg_ = stg[i % 2]; sgk = "stg%d" % (i % 2)
            T.op("dve", lambda e: e.tensor_tensor(out=sg_[:, :], in0=PP[pp][:, :], in1=combt[ck][:, 2 * p:2 * p + 2, :].rearrange("p c f -> p (c f)"), op=ALU.add),
                 reads=pk_ + ["combt%d" % ck], writes=[sgk])
            T.op("act", lambda e: e.activation(out=PT2[k][:, :], in_=sg_[:, :], func=AF.Exp), reads=[sgk], writes=["PT%d" % k])
        else:
            T.op("act", lambda e: e.activation(out=PT2[k][:, :], in_=PP[pp][:, :], func=AF.Exp), reads=pk_, writes=["PT%d" % k])
    def na_pv(i):
        h, g, p = na_steps[i]
        k = na_pt.pop(i)
        po = (h * 4 + g) % 2
        fns = []
        for half in range(2):
            c = 2 * p + half
            ks = na_row_slot(8 * g + 2 * c) if c < 8 else 2560 + (c - 8) * 128
            fns.append(lambda e, ks=ks, half=half, c=c: e.matmul(PS[po][0:65, :], lhsT=VN[:, ks // 128, h, 0:65], rhs=PT2[k][:, half * 512:(half + 1) * 512],
                                                               start=(c == 0), stop=(c == 9)))
        T.group("pe", fns, reads=["VN", "PT%d" % k], writes=["ps%d" % po])
        if p == 4:
            normalize_out(po, 8 + h, slice(g * 512, (g + 1) * 512), i)
    LA = 3
    for i in range(len(na_steps) + LA):
        if i < len(na_steps):
            na_qk(i)
        if i >= LA:
            na_pv(i - LA)
        run_pending(i - LA)
    for _ in range(8):
        run_pending(0, flush=True)
    if stop_after == "na":
        dbg_out["AT"] = (AT, [128, 8, 2048], BF16)
        return finish(nc, T, dbg_out, out)
    T.barrier()

    for i in range(2):
        T.op("pool", lambda e, i=i: e.tensor_copy(out=KH[i][64:96, 0:3072], in_=krot_tmp[64:96, :]), reads=["krot_tmp"], writes=["KHr%d" % i])
    phaseA(range(6, NS), TAIL, False, lambda s: [(KH[i][64:96, s * 512:(s + 1) * 512], "KHr%d" % i) for i in range(2)], xn_base=ATR)
    T.barrier()

    cur[0] = ATT_W
    wuq_b = sb("wuq_b", [128, 3, 768], BF16)
    wuqs_b = sb("wuqs_b", [128, 3, 8, 96], BF16)
    wukv_b = sb("wukv_b", [128, 2, 1024], BF16)
    ctq = sbat("ctq", [128, 512], F32, TMP + 8192); stq = sbat("stq", [128, 512], F32, TMP + 10240)
    r1q = sbat("r1q", [128, 512], F32, TMP + 12288); r2q = sbat("r2q", [128, 512], F32, TMP + 14336)
    assert cur[0] <= 229344, cur[0]
    T.dma("pool", wuq_b[:], w_uq.rearrange("(kc p) c -> p kc c", p=128), writes=["wuq_b"])
    T.op("pool", lambda e: e.memset(wuqs_b[:], 0.0), writes=["wuqs_b"])
    for kc in range(3):
        T.dma("pool", wuqs_b[:, kc, :, 64:96], w_uqs[kc * 128:(kc + 1) * 128, :, :], writes=["wuqs_b"])
    T.op("pool", lambda e: e.tensor_scalar(out=wuqs_b[:, :, :, 64:80], in0=wuqs_b[:, :, :, 64:80], scalar1=-1.0, scalar2=None, op0=ALU.mult),
         reads=["wuqs_b"], writes=["wuqs_b"])
    T.dma("pool", wukv_b[:], w_ukv.rearrange("(kc p) c -> p kc c", p=128), writes=["wukv_b"])
    for i in range(2):
        T.op("pool", lambda e, i=i: e.memset(VH[i][:, :, 64:65], 1.0), writes=["VH%d" % i])
    SCALE = 96 ** -0.5

    def prep_units(h):
        b = h % 2
        units = []
        pbk = [0]
        def nb_():
            pbk[0] += 1
            return idle_set[0][pbk[0] % 2]
        for s in range(NS):
            def ku(s=s):
                PREP = nb_()
                T.group("pe", [lambda e, kc=kc: e.matmul(PS[PREP][0:64, :], lhsT=wukv_b[:, kc, h * 128:h * 128 + 64], rhs=kvn[:, kc, s * 512:(s + 1) * 512],
                                                         start=(kc == 0), stop=(kc == 1)) for kc in range(2)], reads=["kvn", "wukv_b"], writes=["ps%d" % PREP])
                T.op("dve", lambda e: e.tensor_copy(out=KH[b][0:64, s * 512:(s + 1) * 512], in_=PS[PREP][0:64, :]), reads=["ps%d" % PREP], writes=["KH%d" % b])
            units.append(ku)
            def vu(s=s):
                PREP = nb_()
                fns = []
                for tt in range(4):
                    for kc in range(2):
                        fns.append(lambda e, tt=tt, kc=kc: e.matmul(PS[PREP][:, tt * 64:(tt + 1) * 64], lhsT=kvn[:, kc, (4 * s + tt) * 128:(4 * s + tt + 1) * 128],
                                                                    rhs=wukv_b[:, kc, h * 128 + 64:h * 128 + 128], start=(kc == 0), stop=(kc == 1)))
                T.group("pe", fns, reads=["kvn", "wukv_b"], writes=["ps%d" % PREP])
                T.op("dve", lambda e: e.tensor_copy(out=VH[b][:, 4 * s:4 * s + 4, 0:64], in_=PS[PREP][:, 0:256].rearrange("p (t d) -> p t d", t=4)),
                     reads=["ps%d" % PREP], writes=["VH%d" % b])
            units.append(vu)
        for qc in range(4):
            def qu(qc=qc):
                PA_, PB_ = idle_set[0]
                qsl = slice(qc * 512, (qc + 1) * 512)
                T.dma("sp", ctq[64:96, :], ctab[:, qsl], writes=["ctq"])
                T.dma("sp", stq[64:96, :], stab[:, qsl], writes=["stq"])
                T.group("pe", [lambda e, kc=kc: e.matmul(PS[PA_][0:96, :], lhsT=wuq_b[:, kc, h * 96:(h + 1) * 96], rhs=qlatn[:, kc, qsl],
                                                         start=(kc == 0), stop=(kc == 2)) for kc in range(3)], reads=["qlatn", "wuq_b"], writes=["ps%d" % PA_])
                T.group("pe", [lambda e, kc=kc: e.matmul(PS[PB_][0:96, :], lhsT=wuqs_b[:, kc, h, :], rhs=qlatn[:, kc, qsl],
                                                         start=(kc == 0), stop=(kc == 2)) for kc in range(3)], reads=["qlatn", "wuqs_b"], writes=["ps%d" % PB_])
                T.op("dve", lambda e: e.tensor_copy(out=QH[b][0:64, qsl], in_=PS[PA_][0:64, :]), reads=["ps%d" % PA_], writes=["QH%d" % b])
                T.op("dve", lambda e: e.tensor_tensor(out=r1q[64:96, :], in0=PS[PA_][64:96, :], in1=ctq[64:96, :], op=ALU.mult), reads=["ps%d" % PA_, "ctq"], writes=["r1q"])
                T.op("dve", lambda e: e.tensor_tensor(out=r2q[64:96, :], in0=PS[PB_][64:96, :], in1=stq[64:96, :], op=ALU.mult), reads=["ps%d" % PB_, "stq"], writes=["r2q"])
                T.op("pool", lambda e: e.tensor_tensor(out=QH[b][64:96, qsl], in0=r1q[64:96, :], in1=r2q[64:96, :], op=ALU.add), reads=["r1q", "r2q"], writes=["QH%d" % b])
            units.append(qu)
        return units

    idle_set = [(2, 3)]
    for u in prep_units(0):
        u()
    gstep = [0]
    for h in range(8):
        b = h % 2
        nxt = prep_units(h + 1) if h < 7 else []
        ui = [0]
        steps = [(qp, kc) for qp in range(2) for kc in range(NT_MLA)]
        n = len(steps)
        ptm = {}
        def m_qk(i):
            qp, kc = steps[i]; pp = i % 2
            for half in range(2):
                qc = 2 * qp + half
                dst = PP[pp][:, half * 512:(half + 1) * 512]
                T.group("pe", [lambda e, dst=dst, kc=kc, qc=qc: e.matmul(dst, lhsT=KH[b][0:96, kc * 128:(kc + 1) * 128], rhs=QH[b][0:96, qc * 512:(qc + 1) * 512],
                                                                     start=True, stop=True)], reads=["KH%d" % b, "KHr%d" % b, "QH%d" % b], writes=["ps%d" % (4 + 2 * pp + half)])
            k = pti[0] % 3; pti[0] += 1
            ptm[i] = k
            T.op("act", lambda e: e.activation(out=PT2[k][:, :], in_=PP[pp][:, :], func=AF.Exp, bias=kmask[:, kc:kc + 1], scale=SCALE),
                 reads=["ps%d" % (4 + 2 * pp), "ps%d" % (5 + 2 * pp), "kmask"], writes=["PT%d" % k])
        def m_pv(i):
            qp, kc = steps[i]
            k = ptm.pop(i)
            ob_ = (0, 1) if qp == 0 else (2, 3)
            if kc == 0:
                idle_set[0] = (2, 3) if qp == 0 else (0, 1)
            for half in range(2):
                T.group("pe", [lambda e, half=half: e.matmul(PS[ob_[half]][0:65, :], lhsT=VH[b][:, kc, 0:65], rhs=PT2[k][:, half * 512:(half + 1) * 512],
                                                           start=(kc == 0), stop=(kc == NT_MLA - 1))], reads=["VH%d" % b, "PT%d" % k], writes=["ps%d" % ob_[half]])
            if kc == NT_MLA - 1:
                for half in range(2):
                    qc = 2 * qp + half
                    normalize_out(ob_[half], h, slice(qc * 512, (qc + 1) * 512), gstep[0], scr=ob_[half], off=4 * half)
            if nxt:
                want = min(len(nxt), (len(nxt) * (i + 1)) // (n - 16))
                while ui[0] < want:
                    nxt[ui[0]](); ui[0] += 1
        for i in range(n + 2):
            if i < n:
                m_qk(i)
            if i >= 2:
                gstep[0] += 1
                m_pv(i - 2)
                run_pending(gstep[0])
    for _ in range(8):
        run_pending(0, flush=True)
    if stop_after == "mla":
        dbg_out["AT"] = (AT, [128, 8, 2048], BF16)
        return finish(nc, T, dbg_out, out)
    T.barrier()

    T.op("pool", lambda e: e.memset(ss[:], 0.0), writes=["ss_all"])
    T.barrier()
    xnew = sbat("xnew", [128, 16, D], F32, A0)
    wout_b = sbat("wout_b", [128, 8, D], BF16, 117888)
    g1b = sbat("g1b", [128, D], F32, 134272)
    hx2T = sbat("hx2T", [128, 8, 2048], BF16, TAIL)
    xr = [sbat("xr%d" % i, [128, D], F32, TMP + i * 4096) for i in range(4)]
    xn2 = [sbat("xn2_%d" % i, [128, D], BF16, TMP + 16384 + i * 2048) for i in range(2)]
    T.dma("pool", wout_b[:], w_out.rearrange("(kc p) c -> p kc c", p=128), writes=["wout_b"])
    T.dma("sp", g1b[:], gscr[0:1, :].to_broadcast([128, D]), reads=["gscr_r"], writes=["g1b"], key="g1b")
    T.op("pool", lambda e: e.tensor_tensor(out=wout_b[:], in0=wout_b[:], in1=g1b[:].unsqueeze(1).to_broadcast([128, 8, D]), op=ALU.mult),
         reads=["wout_b", "g1b"], writes=["wout_b"])
    def wo_dma(t):
        T.dma("sp", xr[t % 4][:], xs[t * 128:(t + 1) * 128, :], writes=["xr%d" % (t % 4)])
    def wo_a(t):
        xb = xr[t % 4]; xk = "xr%d" % (t % 4)
        for hf in range(2):
            pi = psn(0, 7)
            T.group("pe", [lambda e, kc=kc, pi=pi, hf=hf: e.matmul(PS[pi][:, :], lhsT=AT[:, kc, t * 128:(t + 1) * 128], rhs=wout_b[:, kc, hf * 512:(hf + 1) * 512],
                                                               start=(kc == 0), stop=(kc == 7)) for kc in range(8)], reads=["AT", "wout_b"], writes=["ps%d" % pi])
            T.op("dve", lambda e, pi=pi, hf=hf: e.tensor_tensor(out=xnew[:, t, hf * 512:(hf + 1) * 512], in0=PS[pi][:, :], in1=xb[:, hf * 512:(hf + 1) * 512], op=ALU.add),
                 reads=["ps%d" % pi, xk], writes=["xnew%d" % t])
    def wo_b(t):
        norm_a(t, xnew[:, t, :], "xnew%d" % t, xn2[t % 2], "xn2_%d" % (t % 2), "rstd2_%d" % t)
    def wo_c(t):
        norm_b(xn2[t % 2], "xn2_%d" % (t % 2), lambda kc, t=t: hx2T[:, kc, t * 128:(t + 1) * 128], ["hx2T"] * 8, 16)
    for t in range(3):
        wo_dma(t)
    for k in range(16 + 2):
        if k + 3 < 16:
            wo_dma(k + 3)
        if k < 16:
            wo_a(k)
        if 1 <= k < 17:
            wo_b(k - 1)
        if k >= 2:
            wo_c(k - 2)
    if stop_after == "wout":
        dbg_out["xnew"] = (xnew, [128, 16, D], F32)
        return finish(nc, T, dbg_out, out)
    T.barrier()

    WB = 117888
    wexp = [[sbat("wg%d" % i, [128, 8, 512], BF16, WB + i * 24576),
             sbat("wu%d" % i, [128, 8, 512], BF16, WB + i * 24576 + 8192),
             sbat("wd%d" % i, [128, 4, D], BF16, WB + i * 24576 + 16384)] for i in range(2)]
    hidT = [sbat("hidT%d" % i, [128, 4, 512], BF16, WB + 49152 + i * 4096) for i in range(2)]
    g2b = sbat("g2b", [128, D], F32, WB + 57344)
    gfb = sbat("gfb", [128, D], F32, WB + 61440)
    assert WB + 65536 <= TAIL
    cur[0] = TMP
    wr_b = sb("wr_b", [128, 8, 20], BF16)
    brb = sb("brb", [128, 20], F32)
    lg = sb("lg", [128, 16, 20], F32)
    comb = sb("comb", [128, 16, 16], F32)
    rt = {n_: sb("rt_" + n_, [128, 16, 4], F32) for n_ in ("GE", "OH", "EL", "MK", "SEL", "EX", "TM")}
    rv = {n_: sb("rv_" + n_, [128, 16], F32) for n_ in ("GM", "SG", "GW", "M1", "M2", "SE", "RS", "WS")}
    sg = sb("sg", [128, 4, 512], BF16)
    ofin = [sb("ofin%d" % i, [128, D], F32) for i in range(2)]
    assert cur[0] <= A0, cur[0]
    T.dma("pool", wr_b[:], w_r.rearrange("(kc p) c -> p kc c", p=128), writes=["wr_b"])
    T.dma("sp", brb[:], b_r[None, :].to_broadcast([128, 20]), writes=["brb"])
    T.dma("sp", g2b[:], gscr[1:2, :].to_broadcast([128, D]), reads=["gscr_r"], writes=["g2b"], key="g2b")
    T.dma("sp", gfb[:], g_fin[None, :].to_broadcast([128, D]), writes=["gfb"])
    pr = psn(0, 4)
    for t in range(16):
        T.group("pe", [lambda e, kc=kc, t=t: e.matmul(PS[pr][:, t * 20:(t + 1) * 20], lhsT=hx2T[:, kc, t * 128:(t + 1) * 128], rhs=wr_b[:, kc, :], start=(kc == 0), stop=(kc == 7))
                       for kc in range(8)], reads=["hx2T", "wr_b"], writes=["ps%d" % pr])
    AXX = mybir.AxisListType.X
    def V(fn, rd, wr):
        T.op("dve", fn, reads=rd, writes=wr)
    def B4(ap):
        return ap.unsqueeze(2).to_broadcast([128, 16, 4])
    GL = lg[:, :, 0:4]
    V(lambda e: e.tensor_tensor(out=lg[:], in0=PS[pr][:, 0:320].rearrange("p (t c) -> p t c", c=20), in1=brb[:].unsqueeze(1).to_broadcast([128, 16, 20]), op=ALU.add),
      ["ps%d" % pr, "brb"], ["lg"])
    V(lambda e: e.reduce_max(out=rv["GM"][:], in_=GL, axis=AXX), ["lg"], ["GM"])
    V(lambda e: e.tensor_tensor(out=rt["GE"][:], in0=GL, in1=B4(rv["GM"][:]), op=ALU.subtract), ["lg", "GM"], ["GE"])
    T.op("act", lambda e: e.activation(out=rt["GE"][:], in_=rt["GE"][:], func=AF.Exp), reads=["GE"], writes=["GE"])
    V(lambda e: e.reduce_sum(out=rv["SG"][:], in_=rt["GE"][:], axis=AXX), ["GE"], ["SG"])
    V(lambda e: e.reciprocal(out=rv["GW"][:], in_=rv["SG"][:]), ["SG"], ["GW"])
    V(lambda e: e.tensor_tensor(out=rt["OH"][:], in0=GL, in1=B4(rv["GM"][:]), op=ALU.is_equal), ["lg", "GM"], ["OH"])
    for g in range(4):
        dst = rt["EL"] if g == 0 else rt["TM"]
        V(lambda e, g=g, dst=dst: e.tensor_tensor(out=dst[:], in0=lg[:, :, 4 + 4 * g:8 + 4 * g], in1=rt["OH"][:, :, g:g + 1].to_broadcast([128, 16, 4]), op=ALU.mult),
          ["lg", "OH"], ["EL" if g == 0 else "TM"])
        if g > 0:
            V(lambda e: e.tensor_tensor(out=rt["EL"][:], in0=rt["EL"][:], in1=rt["TM"][:], op=ALU.add), ["EL", "TM"], ["EL"])
    V(lambda e: e.reduce_max(out=rv["M1"][:], in_=rt["EL"][:], axis=AXX), ["EL"], ["M1"])
    V(lambda e: e.tensor_tensor(out=rt["MK"][:], in0=rt["EL"][:], in1=B4(rv["M1"][:]), op=ALU.is_equal), ["EL", "M1"], ["MK"])
    V(lambda e: e.scalar_tensor_tensor(out=rt["MK"][:], in0=rt["MK"][:], scalar=NEG, in1=rt["EL"][:], op0=ALU.mult, op1=ALU.add), ["MK", "EL"], ["MK"])
    V(lambda e: e.reduce_max(out=rv["M2"][:], in_=rt["MK"][:], axis=AXX), ["MK"], ["M2"])
    V(lambda e: e.tensor_tensor(out=rt["SEL"][:], in0=rt["EL"][:], in1=B4(rv["M2"][:]), op=ALU.is_ge), ["EL", "M2"], ["SEL"])
    V(lambda e: e.tensor_tensor(out=rt["EX"][:], in0=rt["EL"][:], in1=B4(rv["M1"][:]), op=ALU.subtract), ["EL", "M1"], ["EX"])
    T.op("act", lambda e: e.activation(out=rt["EX"][:], in_=rt["EX"][:], func=AF.Exp), reads=["EX"], writes=["EX"])
    V(lambda e: e.tensor_tensor(out=rt["EX"][:], in0=rt["EX"][:], in1=rt["SEL"][:], op=ALU.mult), ["EX", "SEL"], ["EX"])
    V(lambda e: e.reduce_sum(out=rv["SE"][:], in_=rt["EX"][:], axis=AXX), ["EX"], ["SE"])
    V(lambda e: e.reciprocal(out=rv["RS"][:], in_=rv["SE"][:]), ["SE"], ["RS"])
    V(lambda e: e.tensor_tensor(out=rv["WS"][:], in0=rv["RS"][:], in1=rv["GW"][:], op=ALU.mult), ["RS", "GW"], ["WS"])
    V(lambda e: e.tensor_tensor(out=rt["EX"][:], in0=rt["EX"][:], in1=B4(rv["WS"][:]), op=ALU.mult), ["EX", "WS"], ["EX"])
    for g in range(4):
        V(lambda e, g=g: e.tensor_tensor(out=comb[:, :, 4 * g:4 * g + 4], in0=rt["EX"][:], in1=rt["OH"][:, :, g:g + 1].to_broadcast([128, 16, 4]), op=ALU.mult),
          ["EX", "OH"], ["comb"])

    msteps = [(ex, s_) for ex in range(16) for s_ in range(4)]
    def moe_gu(i):
        ex, s_ = msteps[i]
        wg, wu, wd = wexp[ex % 2]; wk = "wexp%d" % (ex % 2)
        if s_ == 0:
            T.dma("pool", wg[:], w_gate[ex].rearrange("(kc p) f -> p kc f", p=128), writes=[wk + "g"])
            T.dma("pool", wu[:], w_up[ex].rearrange("(kc p) f -> p kc f", p=128), writes=[wk + "u"])
            T.dma("pool", wd[:], w_down[ex].rearrange("(fc p) d -> p fc d", p=128), writes=[wk + "d"])
            T.op("pool", lambda e: e.tensor_tensor(out=wd[:], in0=wd[:], in1=g2b[:].unsqueeze(1).to_broadcast([128, 4, D]), op=ALU.mult),
                 reads=[wk + "d", "g2b"], writes=[wk + "d"])
        hT = hidT[i % 2]; hk2 = "hidT%d" % (i % 2)
        tsl = slice(s_ * 512, (s_ + 1) * 512)
        for fc in range(4):
            pg = psn(0, 4); pu = psn(0, 4)
            T.group("pe", [lambda e, kc=kc, pg=pg, fc=fc: e.matmul(PS[pg][:, :], lhsT=wg[:, kc, fc * 128:(fc + 1) * 128], rhs=hx2T[:, kc, tsl], start=(kc == 0), stop=(kc == 7))
                           for kc in range(8)], reads=["hx2T", wk + "g"], writes=["ps%d" % pg])
            T.group("pe", [lambda e, kc=kc, pu=pu, fc=fc: e.matmul(PS[pu][:, :], lhsT=wu[:, kc, fc * 128:(fc + 1) * 128], rhs=hx2T[:, kc, tsl], start=(kc == 0), stop=(kc == 7))
                           for kc in range(8)], reads=["hx2T", wk + "u"], writes=["ps%d" % pu])
            T.op("act", lambda e, pg=pg, fc=fc: e.activation(out=sg[:, fc, :], in_=PS[pg][:, :], func=AF.Silu), reads=["ps%d" % pg], writes=["sg%d" % fc])
            T.op("dve", lambda e, pu=pu, fc=fc, hT=hT: e.tensor_tensor(out=hT[:, fc, :], in0=PS[pu][:, :], in1=sg[:, fc, :], op=ALU.mult),
                 reads=["ps%d" % pu, "sg%d" % fc], writes=[hk2 + "_%d" % fc])
    def moe_down(i):
        ex, s_ = msteps[i]
        wg, wu, wd = wexp[ex % 2]; wk = "wexp%d" % (ex % 2)
        hT = hidT[i % 2]; hk2 = "hidT%d" % (i % 2)
        for tt in range(4):
            t = 4 * s_ + tt
            for hf in range(2):
                py = psn(4, 7)
                T.group("pe", [lambda e, fc=fc, py=py, tt=tt, hf=hf: e.matmul(PS[py][:, :], lhsT=hT[:, fc, tt * 128:(tt + 1) * 128], rhs=wd[:, fc, hf * 512:(hf + 1) * 512],
                                                                         start=(fc == 0), stop=(fc == 3)) for fc in range(4)],
                        reads=[hk2 + "_%d" % fc for fc in range(4)] + [wk + "d"], writes=["ps%d" % py])
                T.op("dve", lambda e, py=py, hf=hf, t=t: e.scalar_tensor_tensor(out=xnew[:, t, hf * 512:(hf + 1) * 512], in0=PS[py][:, :], scalar=comb[:, t, ex:ex + 1],
                                                                         in1=xnew[:, t, hf * 512:(hf + 1) * 512], op0=ALU.mult, op1=ALU.add),
                     reads=["ps%d" % py, "comb", "xnew%d" % t], writes=["xnew%d" % t])
    for i in range(len(msteps) + 1):
        if i < len(msteps):
            moe_gu(i)
        if i >= 1:
            moe_down(i - 1)

    for t in range(16):
        ob = ofin[t % 2]; ok = "ofin%d" % (t % 2)
        T.op("act", lambda e, ob=ob, t=t: e.activation(out=ob[:], in_=xnew[:, t, :], func=AF.Square, accum_out=ss[:, 32 + t:33 + t]), reads=["xnew%d" % t], writes=[ok, "ss3_%d" % t])
        T.op("act", lambda e, t=t: e.activation(out=sd[:, 32 + t:33 + t], in_=ss[:, 32 + t:33 + t], func=AF.Sqrt, bias=epsc[:], scale=1.0 / D), reads=["ss3_%d" % t, "epsc"], writes=["sd3_%d" % t])
        T.op("dve", lambda e, t=t: e.reciprocal(out=rstd[:, 32 + t:33 + t], in_=sd[:, 32 + t:33 + t]), reads=["sd3_%d" % t], writes=["rstd3_%d" % t])
        T.op("dve", lambda e, ob=ob, t=t: e.scalar_tensor_tensor(out=ob[:], in0=xnew[:, t, :], scalar=rstd[:, 32 + t:33 + t], in1=gfb[:], op0=ALU.mult, op1=ALU.mult),
             reads=["xnew%d" % t, "rstd3_%d" % t, "gfb"], writes=[ok])
        T.dma("sp", out[t * 128:(t + 1) * 128, :], ob[:], reads=[ok], key="outst%d" % (t % 2))
    return finish(nc, T, dbg_out, out)


def finish(nc, T, dbg_out, out):
    for name, (tens, shape, dt) in dbg_out.items():
        d = nc.dram_tensor("dbg_" + name, list(shape), dt, kind="ExternalOutput").ap()
        idx = tuple(slice(None) for _ in shape)
        T.dma("sp", d[idx], tens[idx], reads=[name], key="dbg_" + name)
    T.finish()
    return nc


GRID_W = 64
_NC_CACHE = {}


def _rope_tables(rows, cols):
    half = 16
    inv_freq = (10000.0 ** (-np.arange(0, half, 2, dtype=np.float32) / half)).astype(np.float32)
    ang = np.concatenate([rows.astype(np.float32)[:, None] * inv_freq, cols.astype(np.float32)[:, None] * inv_freq], axis=-1)
    return np.cos(ang).astype(np.float32), np.sin(ang).astype(np.float32)


def _core_layout(j):
    R0 = 32 * j
    tok = np.full(NSLOT, -1, np.int64)
    tok[0:2048] = np.arange(R0 * 64, (R0 + 32) * 64)
    used = np.zeros(8192, bool); used[R0 * 64:(R0 + 32) * 64] = True
    for i, r in enumerate(list(range(R0 - 4, R0)) + list(range(R0 + 32, R0 + 36))):
        if 0 <= r < 128:
            tok[2048 + i * 64:2048 + (i + 1) * 64] = np.arange(r * 64, (r + 1) * 64)
            used[r * 64:(r + 1) * 64] = True
    tok[2560:2816] = -2 - np.arange(256)
    others = np.nonzero(~used)[0]
    free_halo = np.nonzero(tok[2048:2560] == -1)[0] + 2048
    nfill = len(free_halo)
    if nfill:
        tok[free_halo] = others[len(others) - nfill:]
        others = others[:len(others) - nfill]
    assert len(others) == 5632, len(others)
    tok[2816:2816 + len(others)] = others
    assert (tok[:8448] != -1).all() and (tok[8448:] == -1).all()
    return tok


def _host_inputs(inp):
    x = np.asarray(inp["x"], np.float32); ctx = np.asarray(inp["ctx"], np.float32)
    w_in = np.ascontiguousarray(np.asarray(inp["w_in"], np.float32)[0])
    w_uq = np.ascontiguousarray(np.asarray(inp["w_uq"], np.float32)[0])
    perm = np.concatenate([np.arange(16, 32), np.arange(0, 16)])
    w_krs = np.ascontiguousarray(w_in[:, C_KR:C_KR + 32][:, perm])
    w_uqs = np.ascontiguousarray(w_uq.reshape(384, 8, 96)[:, :, 64:96][:, :, perm])
    rel = np.asarray(inp["na_rel_bias"], np.float32)[0]
    a = np.arange(2)[:, None, None, None]; ck = np.arange(64)[None, :, None, None]
    i = np.arange(8)[None, None, :, None]; cq = np.arange(64)[None, None, None, :]
    cstart = np.clip(cq - 8, 0, 48)
    col_in = (ck >= cstart) & (ck < cstart + 16)
    dc = ck - cq + 15
    nab = np.full((8, 8, 2, 64, 8, 64), NEG, np.float32)
    for c in range(8):
        dr = 2 * c + a - i + 3
        ok = (dr >= 0) & (dr <= 14) & col_in
        okb = np.broadcast_to(ok, (2, 64, 8, 64))
        drb = np.broadcast_to(np.clip(dr, 0, 14), (2, 64, 8, 64)); dcb = np.broadcast_to(np.clip(dc, 0, 30), (2, 64, 8, 64))
        for h in range(8):
            g = rel[h][drb, dcb]
            nab[h, c] = np.where(okb, g, np.float32(NEG))
    nab = np.ascontiguousarray(nab.reshape(8, 8, 128, 512))
    rowsel = np.zeros((16, 8, 2, 64), np.float32)
    for c in range(8):
        for aa in range(2):
            rowsel[2 * c + aa, c, aa, :] = 1.0
    rowsel = rowsel.reshape(16, 8, 128)
    common = dict(
        w_mod=np.ascontiguousarray(np.asarray(inp["w_mod"], np.float32)[0]), b_mod=np.ascontiguousarray(np.asarray(inp["b_mod"], np.float32)[0]),
        g_attn=np.ascontiguousarray(np.asarray(inp["norm_attn_g"], np.float32)[0]), g_ffn=np.ascontiguousarray(np.asarray(inp["norm_ffn_g"], np.float32)[0]),
        g_fin=np.asarray(inp["final_norm_g"], np.float32), w_in=w_in, w_krs=w_krs,
        gq=np.ascontiguousarray(np.asarray(inp["q_a_norm_g"], np.float32)[0]), gkv=np.ascontiguousarray(np.asarray(inp["kv_a_norm_g"], np.float32)[0]),
        w_uq=w_uq, w_uqs=w_uqs, w_ukv=np.ascontiguousarray(np.asarray(inp["w_ukv"], np.float32)[0]),
        w_out=np.ascontiguousarray(np.asarray(inp["w_out"], np.float32)[0]),
        w_r=np.ascontiguousarray(np.concatenate([np.asarray(inp["w_router_group"], np.float32)[0], np.asarray(inp["w_router_expert"], np.float32)[0]], axis=1)),
        b_r=np.ascontiguousarray(np.concatenate([np.asarray(inp["b_router_group"], np.float32)[0], np.asarray(inp["b_router_expert"], np.float32)[0]])),
        w_gate=np.ascontiguousarray(np.asarray(inp["w_gate"], np.float32)[0]), w_up=np.ascontiguousarray(np.asarray(inp["w_up"], np.float32)[0]),
        w_down=np.ascontiguousarray(np.asarray(inp["w_down"], np.float32)[0]),
        nab=nab,
    )
    maps = []
    for core in range(8):
        b, j = core // 4, core % 4
        R0 = 32 * j
        tok = _core_layout(j)
        xs = np.zeros((NSLOT, D), np.float32)
        m = tok >= 0
        xs[m] = x[b][tok[m]]
        xs[2560:2816] = ctx[b]
        kmask = np.where(tok == -1, np.float32(NEG), np.float32(0)).astype(np.float32).reshape(NT, 128).T.copy()
        rows = np.where(m, tok // GRID_W, 0); cols = np.where(m, tok % GRID_W, 0)
        cos, sin = _rope_tables(rows, cols)
        cos[~m] = 1.0; sin[~m] = 0.0
        ctab = np.ascontiguousarray(np.concatenate([cos, cos], axis=1).T)
        stab = np.ascontiguousarray(np.concatenate([sin, sin], axis=1).T)
        narm = np.full((16, 4, 8, 64), NEG, np.float32)
        for g in range(4):
            for i_ in range(8):
                r = R0 + 8 * g + i_
                start = min(max(r - 4, 0), 120)
                for lp in range(16):
                    kr = R0 - 4 + 8 * g + lp
                    if start <= kr < start + 8 and 0 <= kr < 128:
                        narm[lp, g, i_, :] = 0.0
        d = dict(common)
        RM = narm[:, :, :, 0]
        rmc = np.zeros((128, 4, 8, 8), np.float32)
        for c_ in range(8):
            for a_ in range(2):
                rmc[a_ * 64:(a_ + 1) * 64, :, c_, :] = RM[2 * c_ + a_][None, :, :]
        d.update(xs=xs, rmc=np.ascontiguousarray(rmc.reshape(128, 4, 64)), cvec=np.ascontiguousarray(np.stack([np.asarray(inp["c"], np.float32)[b], np.asarray(inp["c_ctx"], np.float32)])),
                 kmask=kmask, ctab=ctab, stab=stab)
        maps.append(d)
    return maps


def kernel(**inputs):
    if "nc" not in _NC_CACHE:
        _NC_CACHE["nc"] = build()
    nc = _NC_CACHE["nc"]
    maps = _host_inputs(inputs)
    res = run_bass_kernel_spmd(nc, maps, core_ids=list(range(8)))
    out = np.zeros((2, 8192, D), np.float32)
    for core in range(8):
        b, j = core // 4, core % 4
        out[b, j * 2048:(j + 1) * 2048] = res.results[core]["out"]
    return out
```

```python
import numpy as np
import concourse.bass as bass
import concourse.mybir as mybir
from concourse.bass_utils import run_bass_kernel_spmd

F32, BF16 = mybir.dt.float32, mybir.dt.bfloat16
AF = mybir.ActivationFunctionType
ALU = mybir.AluOpType

D = 1024
NT = 68
NT_MLA = 66
NS = 17
NSLOT = NT * 128
QC0, KV0 = 0, 896
C_KVLAT, C_KR, C_NK, C_NV = 896, 1152, 1184, 1696
EPS = 1e-6
import os
EVAC_MODE = int(os.environ.get('EVAC_MODE', '2'))
NEG = -1e30


class Trk:
    def __init__(self, nc):
        self.nc = nc
        self.eng = {}
        for name, h in (("pe", nc.tensor), ("act", nc.scalar), ("dve", nc.vector), ("pool", nc.gpsimd), ("sp", nc.sync)):
            self.eng[name] = dict(h=h, sem=nc.alloc_semaphore("s_" + name), cnt=0, seen={})
        self.res = {}
        self.dsem = {}

    def _r(self, k):
        if k not in self.res:
            self.res[k] = dict(w=None, r={})
        return self.res[k]

    def _waits(self, en, reads, writes):
        e = self.eng[en]
        need = {}
        def add(ev):
            if ev is None:
                return
            sem, val, src = ev
            if src == "pe" and en == "pe":
                return
            if need.get(sem.name, (None, 0))[1] < val:
                need[sem.name] = (sem, val)
        for k in reads:
            add(self._r(k)["w"])
        for k in writes:
            r = self._r(k)
            add(r["w"])
            for ev in r["r"].values():
                add(ev)
        for sn, (sem, val) in need.items():
            if e["seen"].get(sn, 0) < val:
                e["h"].wait_ge(sem, val)
                e["seen"][sn] = val

    def _mark(self, ev, reads, writes):
        for k in writes:
            r = self._r(k)
            r["w"] = ev
            r["r"] = {}
        for k in reads:
            r = self._r(k)
            r["r"][ev[0].name + ev[2]] = ev

    def op(self, en, fn, reads=(), writes=()):
        e = self.eng[en]
        self._waits(en, reads, writes)
        ins = fn(e["h"])
        e["cnt"] += 1
        ins.then_inc(e["sem"], 1)
        self._mark((e["sem"], e["cnt"], en), reads, writes)

    def group(self, en, fns, reads=(), writes=()):
        e = self.eng[en]
        self._waits(en, reads, writes)
        ins = None
        for fn in fns:
            ins = fn(e["h"])
        e["cnt"] += 1
        ins.then_inc(e["sem"], 1)
        self._mark((e["sem"], e["cnt"], en), reads, writes)

    def dma(self, qn, out, in_, reads=(), writes=(), key=None):
        e = self.eng[qn]
        self._waits(qn, reads, writes)
        key = key or (writes[0] if writes else reads[0])
        if key not in self.dsem:
            self.dsem[key] = [self.nc.alloc_semaphore("d%d" % len(self.dsem)), 0]
        ds = self.dsem[key]
        e["h"].dma_start(out=out, in_=in_).then_inc(ds[0], 16)
        ds[1] += 16
        self._mark((ds[0], ds[1], "dma"), reads, writes)

    def barrier(self):
        for en, e in self.eng.items():
            for fn, f in self.eng.items():
                if fn != en and f["cnt"] > e["seen"].get(f["sem"].name, 0):
                    e["h"].wait_ge(f["sem"], f["cnt"])
                    e["seen"][f["sem"].name] = f["cnt"]
            for k, (sem, tot) in self.dsem.items():
                if tot > e["seen"].get(sem.name, 0):
                    e["h"].wait_ge(sem, tot)
                    e["seen"][sem.name] = tot

    def finish(self):
        e = self.eng["sp"]
        for fn, f in self.eng.items():
            if fn != "sp" and f["cnt"] > e["seen"].get(f["sem"].name, 0):
                e["h"].wait_ge(f["sem"], f["cnt"])
        for k, (sem, tot) in self.dsem.items():
            if tot > e["seen"].get(sem.name, 0):
                e["h"].wait_ge(sem, tot)


def na_row_slot(l):
    if 4 <= l < 36:
        return (l - 4) * 64
    if l < 4:
        return 2048 + l * 64
    return 2304 + (l - 36) * 64


def build(stop_after=None, dbg=None):
    nc = bass.Bass("TRN2", target_bir_lowering=False)
    T = Trk(nc)

    def din(name, shape, dt=F32):
        return nc.dram_tensor(name, list(shape), dt, kind="ExternalInput").ap()

    xs = din("xs", [NSLOT, D])
    cvec = din("cvec", [2, D])
    w_mod = din("w_mod", [D, 6 * D]); b_mod = din("b_mod", [6 * D])
    g_attn = din("g_attn", [D]); g_ffn = din("g_ffn", [D]); g_fin = din("g_fin", [D])
    w_in = din("w_in", [D, 2208]); w_krs = din("w_krs", [D, 32])
    gq = din("gq", [384]); gkv = din("gkv", [256])
    w_uq = din("w_uq", [384, 768]); w_uqs = din("w_uqs", [384, 8, 32]); w_ukv = din("w_ukv", [256, 1024])
    w_out = din("w_out", [D, D])
    w_r = din("w_r", [D, 20]); b_r = din("b_r", [20])
    w_gate = din("w_gate", [16, D, 512]); w_up = din("w_up", [16, D, 512]); w_down = din("w_down", [16, 512, D])
    kmask_d = din("kmask", [128, NT])
    ctab = din("ctab", [32, NSLOT]); stab = din("stab", [32, NSLOT])
    nab = din("nab", [8, 8, 128, 512]); rmc_d = din("rmc", [128, 4, 64])
    out = nc.dram_tensor("out", [2048, D], F32, kind="ExternalOutput").ap()
    dbg_out = {}

    def sbat(name, shape, dt, at):
        nbytes = int(np.prod(shape[1:])) * (2 if dt == BF16 else 4)
        assert at % 32 == 0 and at + nbytes <= 229344, (name, at, nbytes)
        return nc.alloc_sbuf_tensor_at(name, list(shape), dt, offset=at)
    cur = [16512]
    def sb(name, shape, dt):
        nbytes = (int(np.prod(shape[1:])) * (2 if dt == BF16 else 4) + 31) // 32 * 32
        off = cur[0]; cur[0] += nbytes
        return sbat(name, shape, dt, off)
    ident = sb("ident", [128, 128], BF16)
    ones_bf = sb("ones_bf", [128, 128], BF16)
    onesf = sb("onesf", [128, 128], F32)
    epsc = sb("epsc", [128, 1], F32)
    modc = sb("modc", [128, 64], F32)
    gqc = sb("gqc", [128, 4], F32); gkvc = sb("gkvc", [128, 2], F32)
    kmask = sb("kmask_s", [128, NT], F32)
    ss = sb("ss", [128, NT], F32); sd = sb("sd", [128, NT], F32); rstd = sb("rstd", [128, NT], F32)
    rmc = sb("rmc_s", [128, 4, 64], BF16)
    shiftm = sb("shiftm", [64, 128], BF16)
    assert cur[0] <= 25728, cur[0]
    TMP = 25728
    cur[0] = TMP
    tmpf = [sb("tmpf%d" % i, [128, 512], F32) for i in range(3)]
    sq = [sb("sq%d" % i, [128, 512], BF16) for i in range(3)]
    sdb = sb("sdb", [128, 512], F32)
    rb = sb("rb", [128, 512], F32)
    ctt = sb("ctt", [128, 512], F32); stt = sb("stt", [128, 512], F32)
    r2 = sb("r2", [128, 512], F32)
    krot_tmp = sb("krot_tmp", [128, 3072], BF16)
    assert cur[0] <= 52352, cur[0]
    A0 = 52352
    kvn = sbat("kvn", [128, 2, NSLOT], BF16, A0)
    qlatn = sbat("qlatn", [128, 3, 2048], BF16, A0 + 34816)
    NR = A0 + 34816 + 12288
    KN = sbat("KN", [128, 4, 2816], BF16, NR)
    VN = sbat("VN", [128, 22, 8, 65], BF16, NR + 22528)
    QN = sbat("QN", [128, 4, 2048], BF16, NR + 22528 + 22880)
    NR_END = NR + 22528 + 22880 + 16384
    KH = [sbat("KH%d" % i, [128, NSLOT], BF16, NR + i * 17408) for i in range(2)]
    VH = [sbat("VH%d" % i, [128, NT, 65], BF16, NR + 34816 + i * 8864) for i in range(2)]
    QH = [sbat("QH%d" % i, [128, 2048], BF16, NR + 34816 + 17728 + i * 4096) for i in range(2)]
    assert NR + 34816 + 17728 + 8192 <= NR_END
    ATR = NR_END
    AT = sbat("AT", [128, 8, 2048], BF16, ATR)
    TAIL = ATR + 32768
    assert TAIL == 194016, TAIL
    cur[0] = TAIL
    PT2 = [sb("PT%d" % i, [128, 1024], BF16) for i in range(4)]
    osb = [sbat("osb%d" % i, [128, 512], F32, TMP + i * 2048) for i in range(2)]
    rc = sbat("rc", [128, 512], F32, TMP + 4096)
    onbs = [sbat("onb%d" % i, [64, 512], BF16, TMP + 6144 + i * 1024) for i in range(2)]
    ATT_W = cur[0]
    class Half:
        def __init__(self, t, off):
            self.t, self.off = t, off
        def __getitem__(self, key):
            r, c = key
            a = (c.start or 0) + self.off
            b_ = (c.stop if c.stop is not None else 512) + self.off
            return self.t[r, a:b_]
    PP = [nc.alloc_psum_tensor("pp%d" % i, [128, 1024], F32) for i in range(2)]
    PS = [nc.alloc_psum_tensor("ps%d" % i, [128, 512], F32) for i in range(4)]
    PS += [Half(PP[0], 0), Half(PP[0], 512), Half(PP[1], 0)]
    pTt = PP[1][:, 512:1024].bitcast(BF16)
    rr = [0]
    def psn(lo=0, hi=8):
        i = lo + rr[0] % (hi - lo); rr[0] += 1
        return i
    gscr = nc.dram_tensor("gscr", [2, D], F32).ap()

    T.op("pool", lambda e: e.memset(ident[:], 0.0), writes=["ident"])
    T.op("pool", lambda e: e.affine_select(out=ident[:], in_=ident[:], pattern=[[-1, 128]], compare_op=ALU.not_equal,
                                           fill=1.0, base=0, channel_multiplier=1), reads=["ident"], writes=["ident"])
    T.op("pool", lambda e: e.memset(shiftm[:], 0.0), writes=["shiftm"])
    T.op("pool", lambda e: e.affine_select(out=shiftm[:], in_=shiftm[:], pattern=[[-1, 128]], compare_op=ALU.not_equal,
                                           fill=1.0, base=64, channel_multiplier=1), reads=["shiftm"], writes=["shiftm"])
    T.op("pool", lambda e: e.memset(ones_bf[:], 1.0), writes=["ones_bf"])
    T.op("pool", lambda e: e.memset(onesf[:], 1.0), writes=["onesf"])
    T.op("pool", lambda e: e.memset(epsc[:], EPS), writes=["epsc"])
    T.op("pool", lambda e: e.memset(ss[:], 0.0), writes=["ss_all"])
    T.dma("sp", kmask[:], kmask_d[:, :], writes=["kmask"])
    T.dma("pool", rmc[:], rmc_d[:, :, :], writes=["rmc"])
    with nc.allow_non_contiguous_dma(reason="tiny column loads"):
        T.dma("sp", gqc[:, 0:3], gq.rearrange("(c p) -> p c", p=128), writes=["gqc"])
        T.dma("sp", gkvc[:, 0:2], gkv.rearrange("(c p) -> p c", p=128), writes=["gkvc"])

    PTR = 7
    pT = pTt

    def rms_feat(ps_list, nfeat, gcol, gkey, dst_fn, dkey):
        n = len(ps_list)
        for c, pi in enumerate(ps_list):
            T.op("act", lambda e, c=c, pi=pi: e.activation(out=tmpf[c][:], in_=PS[pi][:, :], func=AF.Copy), reads=["ps%d" % pi], writes=["tmpf%d" % c])
            T.op("act", lambda e, c=c: e.activation(out=sq[c][:], in_=tmpf[c][:], func=AF.Square), reads=["tmpf%d" % c], writes=["sq%d" % c])
        pq = psn(0, 7)
        T.group("pe", [lambda e, c=c, pq=pq: e.matmul(PS[pq][:, :], lhsT=ones_bf[:], rhs=sq[c][:], start=(c == 0), stop=(c == n - 1)) for c in range(n)],
                reads=["ones_bf"] + ["sq%d" % c for c in range(n)], writes=["ps%d" % pq])
        T.op("act", lambda e, pq=pq: e.activation(out=sdb[:], in_=PS[pq][:, :], func=AF.Sqrt, bias=epsc[:], scale=1.0 / nfeat),
             reads=["ps%d" % pq, "epsc"], writes=["sdb"])
        T.op("dve", lambda e: e.reciprocal(out=rb[:], in_=sdb[:]), reads=["sdb"], writes=["rb"])
        for c in range(n):
            T.op("dve", lambda e, c=c: e.scalar_tensor_tensor(out=dst_fn(c), in0=tmpf[c][:], scalar=gcol[:, c:c + 1], in1=rb[:], op0=ALU.mult, op1=ALU.mult),
                 reads=["tmpf%d" % c, "rb", gkey], writes=[dkey])

    def norm_a(t, xb, xk, nb, nk, rkey):
        T.op("act", lambda e: e.activation(out=nb[:], in_=xb, func=AF.Square, accum_out=ss[:, t:t + 1]), reads=[xk], writes=[nk, "ss%d" % t])
        T.op("act", lambda e: e.activation(out=sd[:, t:t + 1], in_=ss[:, t:t + 1], func=AF.Sqrt, bias=epsc[:], scale=1.0 / D),
             reads=["ss%d" % t, "epsc"], writes=["sd%d" % t])
        T.op("dve", lambda e: e.reciprocal(out=rstd[:, t:t + 1], in_=sd[:, t:t + 1]), reads=["sd%d" % t], writes=[rkey])
        T.op("act", lambda e: e.activation(out=nb[:], in_=xb, func=AF.Identity, scale=rstd[:, t:t + 1]), reads=[xk, rkey], writes=[nk])

    def norm_b(nb, nk, dst_fn, dkeys, cb):
        T.group("pe", [lambda e, kc=kc: e.transpose(out=pT[:, kc * 128:(kc + 1) * 128], in_=nb[:, kc * 128:(kc + 1) * 128], identity=ident[:])
                       for kc in range(8)], reads=[nk, "ident"], writes=["ps%d" % PTR])
        for kc in range(8):
            dst = dst_fn(kc); src = pT[:, kc * 128:(kc + 1) * 128]
            T.op("dve", lambda e, dst=dst, src=src, kc=kc: e.tensor_scalar(out=dst, in0=src, scalar1=modc[:, cb + kc:cb + kc + 1],
                                                                     scalar2=modc[:, cb + 8 + kc:cb + 9 + kc], op0=ALU.mult, op1=ALU.add),
                 reads=["ps%d" % PTR, "modc"], writes=[dkeys[kc]])

    def norm_transpose(src_ap, t, xb, xk, nb, nk, dst_fn, dkeys, cb, rkey):
        norm_a(t, xb, xk, nb, nk, rkey)
        norm_b(nb, nk, dst_fn, dkeys, cb)

    def phaseA(s_list, base, full, krot_dst, xn_base=None, state=None, load_only=False):
        if state is not None:
            return phaseA_run(s_list, full, krot_dst, state)
        o = [base]
        def wa(name, shape, dt):
            nbytes = (int(np.prod(shape[1:])) * (2 if dt == BF16 else 4) + 31) // 32 * 32
            t_ = sbat(name, shape, dt, o[0]); o[0] += nbytes
            return t_
        sfx = "f" if full else "p"
        ncols = 2208 if full else 256
        win_b = wa("win_b" + sfx, [128, 8, ncols], BF16)
        wkr_b = wa("wkr_b" + sfx, [128, 8, 96], BF16)
        wkrs_b = wa("wkrs_b" + sfx, [128, 8, 96], BF16)
        xt = [wa("xt%d%s" % (i, sfx), [128, D], F32) for i in range(2)]
        if xn_base is not None:
            xt += [sbat("xt%d%s" % (2 + i, sfx), [128, D], F32, xn_base + 4096 + i * 4096) for i in range(2)]
        if xn_base is None:
            xn = [wa("xn%d%s" % (i, sfx), [128, D], BF16) for i in range(2)]
        else:
            xn = [sbat("xn%d%s" % (i, sfx), [128, D], BF16, xn_base + i * 2048) for i in range(2)]
        hxT = [wa("hxT%d%s" % (i, sfx), [128, 8, 512], BF16) for i in range(2)]
        assert o[0] <= 229344, o[0]
        wkey = "win_b"
        if full:
            for kc in range(8):
                T.dma("pool", win_b[:, kc, :], w_in[kc * 128:(kc + 1) * 128, :], writes=[wkey])
            cko = C_KVLAT
        else:
            T.dma("pool", win_b[:], w_in[:, C_KVLAT:C_KVLAT + 256].rearrange("(kc p) c -> p kc c", p=128), writes=[wkey])
            cko = 0
        T.op("pool", lambda e: e.memset(wkr_b[:], 0.0), writes=["wkr_b"])
        T.op("pool", lambda e: e.memset(wkrs_b[:], 0.0), writes=["wkrs_b"])
        T.dma("pool", wkr_b[:, :, 64:96], w_in[:, C_KR:C_KR + 32].rearrange("(kc p) c -> p kc c", p=128), writes=["wkr_b"])
        T.dma("pool", wkrs_b[:, :, 64:96], w_krs.rearrange("(kc p) c -> p kc c", p=128), writes=["wkrs_b"])
        T.op("pool", lambda e: e.tensor_scalar(out=wkrs_b[:, :, 64:80], in0=wkrs_b[:, :, 64:80], scalar1=-1.0, scalar2=None, op0=ALU.mult),
             reads=["wkrs_b"], writes=["wkrs_b"])
        state_ = dict(win_b=win_b, wkr_b=wkr_b, wkrs_b=wkrs_b, xt=xt, xn=xn, hxT=hxT, wkey=wkey, cko=cko)
        if load_only:
            return state_
        return phaseA_run(s_list, full, krot_dst, state_)

    def phaseA_run(s_list, full, krot_dst, st_):
        win_b, wkr_b, wkrs_b, xt, xn, hxT, wkey, cko = (st_[k] for k in ("win_b", "wkr_b", "wkrs_b", "xt", "xn", "hxT", "wkey", "cko"))
        if stop_after == "paw":
            return "stop"
        def tinfo(s, tt):
            t = 4 * s + tt
            nx = len(xt)
            return t, xt[t % nx], "xt%d" % (t % nx), xn[t % 2], "xn%d" % (t % 2)
        def nt_dma(s, tt):
            t, xb, xk, nb, nk = tinfo(s, tt)
            T.dma("sp", xb[:], xs[t * 128:(t + 1) * 128, :], writes=[xk])
        def nt_a(s, tt):
            t, xb, xk, nb, nk = tinfo(s, tt)
            norm_a(t, xb[:], xk, nb, nk, "rstd%d" % t)
        def nt_b(s, tt):
            t, xb, xk, nb, nk = tinfo(s, tt)
            hb = hxT[s % 2]; hk = "hxT%d" % (s % 2)
            hks = [hk + "_%d" % kc for kc in range(8)]
            cb = 32 if t in (20, 21) else 0
            norm_b(nb, nk, lambda kc, tt=tt, hb=hb: hb[:, kc, tt * 128:(tt + 1) * 128], hks, cb)

        def mm_parts(s):
            hb = hxT[s % 2]; hk = "hxT%d" % (s % 2)
            hks = [hk + "_%d" % kc for kc in range(8)]
            sl = slice(s * 512, (s + 1) * 512)
            def p0():
                pl = []
                for c in range(2):
                    pi = psn(0, 7); pl.append(pi)
                    T.group("pe", [lambda e, kc=kc, pi=pi, c=c: e.matmul(PS[pi][:, :], lhsT=win_b[:, kc, cko + c * 128:cko + (c + 1) * 128], rhs=hb[:, kc, :],
                                                                     start=(kc == 0), stop=(kc == 7)) for kc in range(8)],
                            reads=hks + [wkey], writes=["ps%d" % pi])
                rms_feat(pl, 256, gkvc, "gkvc", lambda c: kvn[:, c, sl], "kvn")
                pk = psn(0, 7); pks = psn(0, 7)
                T.group("pe", [lambda e, kc=kc: e.matmul(PS[pk][0:96, :], lhsT=wkr_b[:, kc, :], rhs=hb[:, kc, :], start=(kc == 0), stop=(kc == 7)) for kc in range(8)],
                        reads=hks + ["wkr_b"], writes=["ps%d" % pk])
                T.group("pe", [lambda e, kc=kc: e.matmul(PS[pks][0:96, :], lhsT=wkrs_b[:, kc, :], rhs=hb[:, kc, :], start=(kc == 0), stop=(kc == 7)) for kc in range(8)],
                        reads=hks + ["wkrs_b"], writes=["ps%d" % pks])
                T.dma("sp", ctt[64:96, :], ctab[:, sl], writes=["ctt"])
                T.dma("sp", stt[64:96, :], stab[:, sl], writes=["stt"])
                T.op("dve", lambda e: e.tensor_tensor(out=tmpf[2][64:96, :], in0=PS[pk][64:96, :], in1=ctt[64:96, :], op=ALU.mult), reads=["ps%d" % pk, "ctt"], writes=["tmpf2"])
                T.op("dve", lambda e: e.tensor_tensor(out=r2[64:96, :], in0=PS[pks][64:96, :], in1=stt[64:96, :], op=ALU.mult), reads=["ps%d" % pks, "stt"], writes=["r2"])
                for dst, dk in krot_dst(s):
                    T.op("pool", lambda e, dst=dst: e.tensor_tensor(out=dst, in0=tmpf[2][64:96, :], in1=r2[64:96, :], op=ALU.add), reads=["tmpf2", "r2"], writes=[dk])
            def p1():
                if not (full and s < 6):
                    return
                nsl = slice(s * 512, (s + 1) * 512) if s < 5 else slice(2560, 2816)
                ncol = slice(0, 512) if s < 5 else slice(0, 256)
                for j in range(4):
                    pi = psn(0, 7)
                    T.group("pe", [lambda e, kc=kc, pi=pi, j=j: e.matmul(PS[pi][:, :], lhsT=win_b[:, kc, C_NK + j * 128:C_NK + (j + 1) * 128], rhs=hb[:, kc, :],
                                                                     start=(kc == 0), stop=(kc == 7)) for kc in range(8)],
                            reads=hks + [wkey], writes=["ps%d" % pi])
                    if j % 2 == 0:
                        T.op("act", lambda e, pi=pi, j=j: e.activation(out=KN[:, j, nsl], in_=PS[pi][:, ncol], func=AF.Copy), reads=["ps%d" % pi], writes=["KN"])
                    else:
                        T.op("dve", lambda e, pi=pi, j=j: e.tensor_copy(out=KN[:, j, nsl], in_=PS[pi][:, ncol]), reads=["ps%d" % pi], writes=["KN"])
            def p2():
                if not (full and s < 6):
                    return
                for tt in range(4 if s < 5 else 2):
                    pi = psn(0, 7); t = 4 * s + tt
                    T.group("pe", [lambda e, kc=kc, pi=pi, tt=tt: e.matmul(PS[pi][:, :], lhsT=hb[:, kc, tt * 128:(tt + 1) * 128], rhs=win_b[:, kc, C_NV:C_NV + 512],
                                                                       start=(kc == 0), stop=(kc == 7)) for kc in range(8)],
                            reads=hks + [wkey], writes=["ps%d" % pi])
                    src = PS[pi][:, :].rearrange("p (h d) -> p h d", h=8)
                    if tt % 2 == 0:
                        T.op("dve", lambda e, src=src, t=t: e.tensor_copy(out=VN[:, t, :, 0:64], in_=src), reads=["ps%d" % pi], writes=["VN"])
                    else:
                        T.op("act", lambda e, src=src, t=t: e.activation(out=VN[:, t, :, 0:64], in_=src, func=AF.Copy), reads=["ps%d" % pi], writes=["VN"])
            def p3():
                if not (full and s < 4):
                    return
                pl = []
                for c in range(3):
                    pi = psn(0, 7); pl.append(pi)
                    T.group("pe", [lambda e, kc=kc, pi=pi, c=c: e.matmul(PS[pi][:, :], lhsT=win_b[:, kc, c * 128:(c + 1) * 128], rhs=hb[:, kc, :],
                                                                     start=(kc == 0), stop=(kc == 7)) for kc in range(8)],
                            reads=hks + [wkey], writes=["ps%d" % pi])
                rms_feat(pl, 384, gqc, "gqc", lambda c: qlatn[:, c, sl], "qlatn")
                for j in range(4):
                    pi = psn(0, 7)
                    T.group("pe", [lambda e, kc=kc, pi=pi, j=j: e.matmul(PS[pi][:, :], lhsT=win_b[:, kc, 384 + j * 128:384 + (j + 1) * 128], rhs=hb[:, kc, :],
                                                                     start=(kc == 0), stop=(kc == 7)) for kc in range(8)],
                            reads=hks + [wkey], writes=["ps%d" % pi])
                    T.op("act", lambda e, pi=pi, j=j: e.activation(out=QN[:, j, sl], in_=PS[pi][:, :], func=AF.Copy, scale=0.125), reads=["ps%d" % pi], writes=["QN"])
            return [p0, p1, p2, p3]

        s_list = list(s_list)
        seq = [(s, tt) for s in s_list for tt in range(4)]
        depth = len(xt) - 1
        for k0 in range(min(depth, len(seq))):
            nt_dma(*seq[k0])
        nt_a(*seq[0])
        prev_parts = None
        for k, (s, tt) in enumerate(seq):
            if k + depth < len(seq):
                nt_dma(*seq[k + depth])
            if k + 1 < len(seq):
                nt_a(*seq[k + 1])
            nt_b(s, tt)
            if prev_parts is not None:
                prev_parts[tt]()
            if tt == 3:
                prev_parts = mm_parts(s)
        for p_ in prev_parts:
            p_()

    PA1 = phaseA(None, ATR, True, None, load_only=True)
    ccol = sbat("ccol", [128, 2, 8], F32, A0)
    scol = sbat("scol", [128, 2, 8], BF16, A0 + 64)
    wmb = [sbat("wmb%d" % i, [128, 8, 512], BF16, A0 + 128 + i * 8192) for i in range(2)]
    brow = sbat("brow", [1, 512], F32, A0 + 128 + 16384)
    grow = sbat("grow", [1, 512], F32, A0 + 128 + 16384 + 2048)
    mrow = [sbat("mrow%d" % i, [1, 512], F32, A0 + 128 + 16384 + 4096 + i * 2048) for i in range(2)]
    with nc.allow_non_contiguous_dma(reason="tiny column loads"):
        T.dma("sp", ccol[:], cvec.rearrange("r (kc p) -> p r kc", p=128), writes=["ccol"])
    T.op("act", lambda e: e.activation(out=scol[:], in_=ccol[:], func=AF.Silu), reads=["ccol"], writes=["scol"])
    PCOL = 6
    for j in range(12):
        v, hf = j // 2, j % 2
        wb = wmb[j % 2]; wk = "wmb%d" % (j % 2)
        T.dma("pool", wb[:], w_mod[:, j * 512:(j + 1) * 512].rearrange("(kc p) c -> p kc c", p=128), writes=[wk])
        T.dma("sp", brow[:], b_mod[None, j * 512:(j + 1) * 512], writes=["brow"])
        if v in (1, 4):
            gsrc = g_attn if v == 1 else g_ffn
            T.dma("sp", grow[:], gsrc[None, hf * 512:(hf + 1) * 512], writes=["grow"])
        for r in range(2):
            if r == 1 and v > 1:
                continue
            pi = psn(0, 6)
            T.group("pe", [lambda e, kc=kc, pi=pi, r=r, wb=wb: e.matmul(PS[pi][0:1, :], lhsT=scol[:, r, kc:kc + 1], rhs=wb[:, kc, :],
                                                                    start=(kc == 0), stop=(kc == 7)) for kc in range(8)],
                    reads=["scol", wk], writes=["ps%d" % pi])
            mr = mrow[r]; mk = "mrow%d" % r
            T.op("dve", lambda e, pi=pi, mr=mr: e.tensor_tensor(out=mr[:], in0=PS[pi][0:1, :], in1=brow[:], op=ALU.add),
                 reads=["ps%d" % pi, "brow"], writes=[mk])
            if v in (1, 4):
                T.op("dve", lambda e, mr=mr: e.scalar_tensor_tensor(out=mr[:], in0=mr[:], scalar=1.0, in1=grow[:], op0=ALU.add, op1=ALU.mult),
                     reads=[mk, "grow"], writes=[mk])
            if v in (2, 5):
                gi = 0 if v == 2 else 1
                T.dma("sp", gscr[gi:gi + 1, hf * 512:(hf + 1) * 512], mr[:], reads=[mk], writes=["gscr_r"], key="gscr")
            else:
                if r == 0:
                    base = {1: 0, 0: 8, 4: 16, 3: 24}[v]
                else:
                    base = {1: 32, 0: 40}[v]
                T.group("pe", [lambda e, i=i, mr=mr, base=base, hf=hf: e.matmul(PS[PCOL][:, base + hf * 4 + i: base + hf * 4 + i + 1],
                                                                             lhsT=mr[0:1, i * 128:(i + 1) * 128], rhs=onesf[0:1, 0:1],
                                                                             start=True, stop=True) for i in range(4)],
                        reads=[mk, "onesf"], writes=["pscol"])
    T.op("dve", lambda e: e.tensor_copy(out=modc[:, 0:48], in_=PS[PCOL][:, 0:48]), reads=["pscol"], writes=["modc"])
    if stop_after == "adaln":
        dbg_out["modc"] = (modc, [128, 64], F32)
        return finish(nc, T, dbg_out, out)
    T.barrier()

    T.op("pool", lambda e: e.memset(VN[:, :, :, 64:65], 1.0), writes=["VN"])
    rv = phaseA(range(6), ATR, True, lambda s: [(krot_tmp[64:96, s * 512:(s + 1) * 512], "krot_tmp")], state=PA1)
    if rv == "stop":
        return finish(nc, T, dbg_out, out)
    if stop_after == "phaseA":
        dbg_out["kvn"] = (kvn, [128, 2, NSLOT], BF16); dbg_out["qlatn"] = (qlatn, [128, 3, 2048], BF16)
        dbg_out["KN"] = (KN, [128, 4, 2816], BF16); dbg_out["QN"] = (QN, [128, 4, 2048], BF16)
        dbg_out["VN"] = (VN, [128, 22, 8, 65], BF16); dbg_out["krot_tmp"] = (krot_tmp, [128, 3072], BF16)
        return finish(nc, T, dbg_out, out)
    T.barrier()

    nrm = [0]
    pending = []
    def run_pending(i, flush=False):
        for item in [x for x in pending if flush or x[0] <= i]:
            pending.remove(item)
            item[1]()
    def normalize_out(po, h_at, qsl, i, scr=3, off=0, act_recip=False):
        ob = osb[nrm[0] % 2]; ok = "osb%d" % (nrm[0] % 2); onb = onbs[nrm[0] % 2]; onk = "onb%d" % (nrm[0] % 2); nrm[0] += 1
        if act_recip:
            T.op("act", lambda e: e.activation(out=ob[0:65, :], in_=PS[po][0:65, :], func=AF.Copy), reads=["ps%d" % po], writes=[ok])
            def rcp():
                T.op("act", lambda e: e.activation(out=rc[64:65, :], in_=ob[64:65, :], func=AF.Ln), reads=[ok], writes=["rc"])
                T.op("act", lambda e: e.activation(out=ob[64:65, :], in_=rc[64:65, :], func=AF.Exp, scale=-1.0), reads=["rc"], writes=[ok])
            pending.append([i + 1 + off, rcp])
        else:
            T.op("dve", lambda e: e.tensor_copy(out=ob[0:65, :], in_=PS[po][0:65, :]), reads=["ps%d" % po], writes=[ok])
            for q4 in range(4):
                pending.append([i + 1 + off + q4, lambda q4=q4: T.op("dve", lambda e: e.reciprocal(out=ob[64:65, q4 * 128:(q4 + 1) * 128], in_=ob[64:65, q4 * 128:(q4 + 1) * 128]),
                                                                 reads=[ok], writes=[ok])])
        def st3():
            T.group("pe", [lambda e: e.matmul(PS[scr][:, :], lhsT=shiftm[:], rhs=onb[:], start=True, stop=True)], reads=[onk, "shiftm"], writes=["ps%d" % scr])
            T.op("dve", lambda e: e.tensor_copy(out=AT[64:128, h_at // 2, qsl], in_=PS[scr][64:128, :]), reads=["ps%d" % scr], writes=["AT"])
        def st2():
            T.group("pe", [lambda e: e.matmul(PS[scr][0:64, :], lhsT=onesf[64:65, 0:64], rhs=ob[64:65, :], start=True, stop=True)],
                    reads=[ok, "onesf"], writes=["ps%d" % scr])
            if h_at % 2 == 0:
                T.op("dve", lambda e: e.tensor_tensor(out=AT[0:64, h_at // 2, qsl], in0=ob[0:64, :], in1=PS[scr][0:64, :], op=ALU.mult),
                     reads=[ok, "ps%d" % scr], writes=["AT"])
            else:
                T.op("dve", lambda e: e.tensor_tensor(out=onb[:], in0=ob[0:64, :], in1=PS[scr][0:64, :], op=ALU.mult),
                     reads=[ok, "ps%d" % scr], writes=[onk])
                pending.append([i + 9 + off, st3])
        pending.append([i + 6 + off, st2])

    cur[0] = ATT_W
    nabt = [sb("nabt%d" % i, [128, 8, 512], BF16) for i in range(2)]
    assert cur[0] <= 229344
    stg = [sbat("stg%d" % i, [128, 1024], F32, TMP + 8192 + i * 4096) for i in range(2)]
    combt = [sbat("combt%d" % i, [128, 8, 512], BF16, ATR + i * 8192) for i in range(2)]
    na_steps = [(h, g, p) for h in range(8) for g in range(4) for p in range(5)]
    pti = [0]
    na_pt = {}
    def na_load(h):
        T.dma("pool", nabt[h % 2][:], nab[h].rearrange("c p f -> p c f"), writes=["nabt%d" % (h % 2)])
    def na_comb(h, g):
        k = (h * 4 + g) % 2
        T.op("pool", lambda e: e.tensor_tensor(out=combt[k][:].rearrange("p c (i q) -> p c i q", q=64),
                                               in0=nabt[h % 2][:].rearrange("p c (i q) -> p c i q", q=64),
                                               in1=rmc[:, g, :].rearrange("p (c i) -> p c i", i=8).unsqueeze(3).to_broadcast([128, 8, 8, 64]), op=ALU.add),
             reads=["nabt%d" % (h % 2), "rmc"], writes=["combt%d" % k])
    qz = [[sb("qz%d_%d" % (par, k_), [128, 512], BF16) for k_ in range(2)] for par in range(2)]
    assert cur[0] <= 229344
    for par in range(2):
        for k_ in range(2):
            T.op("pool", lambda e, par=par, k_=k_: e.memset(qz[par][k_][:], 0.0), writes=["qz%d_%d" % (par, k_)])
    def na_qz(h, g):
        par = h % 2; k_ = (h * 4 + g) // 1 % 2
        pb_ = par * 64
        T.op("pool", lambda e: e.tensor_copy(out=qz[par][k_][pb_:pb_ + 64, :], in_=QN[pb_:pb_ + 64, h // 2, g * 512:(g + 1) * 512]),
             reads=["QN"], writes=["qz%d_%d" % (par, k_)])
    na_load(0); na_load(1); na_comb(0, 0); na_qz(0, 0)
    def na_qk(i):
        h, g, p = na_steps[i]
        j = h // 2; pb = (h % 2) * 64
        if p == 0:
            nh, ng = (h, g + 1) if g < 3 else (h + 1, 0)
            if nh < 8:
                if ng == 0 and nh + 1 < 8 and False:
                    pass
                na_comb(nh, ng)
                na_qz(nh, ng)
        if p == 4 and g == 3 and h + 2 < 8:
            na_load(h + 2)
        qsl = slice(g * 512, (g + 1) * 512)
        pp = i % 2
        for half in range(2):
            c = 2 * p + half
            ks = na_row_slot(8 * g + 2 * c) if c < 8 else 2560 + (c - 8) * 128
            dst = PP[pp][:, half * 512:(half + 1) * 512]
            qzb = qz[h % 2][(h * 4 + g) % 2]
            T.group("pe", [lambda e, ks=ks, dst=dst: e.matmul(dst, lhsT=KN[:, j, ks:ks + 128], rhs=qzb[:, :], start=True, stop=True)],
                    reads=["KN", "qz%d_%d" % (h % 2, (h * 4 + g) % 2)], writes=["ps%d" % (4 + 2 * pp + half)])
        k = pti[0] % 4; pti[0] += 1
        na_pt[i] = k
        pk_ = ["ps%d" % (4 + 2 * pp), "ps%d" % (5 + 2 * pp)]
        if p < 4:
            ck = (h * 4 + g) % 2
            sg_ = stg[i % 2]; sgk = "stg%d" % (i % 2)
            T.op("dve", lambda e: e.tensor_tensor(out=sg_[:, :], in0=PP[pp][:, :], in1=combt[ck][:, 2 * p:2 * p + 2, :].rearrange("p c f -> p (c f)"), op=ALU.add),
                 reads=pk_ + ["combt%d" % ck], writes=[sgk])
            T.op("act", lambda e: e.activation(out=PT2[k][:, :], in_=sg_[:, :], func=AF.Exp), reads=[sgk], writes=["PT%d" % k])
        else:
            T.op("act", lambda e: e.activation(out=PT2[k][:, :], in_=PP[pp][:, :], func=AF.Exp), reads=pk_, writes=["PT%d" % k])
    def na_pv(i):
        h, g, p = na_steps[i]
        k = na_pt.pop(i)
        po = (h * 4 + g) % 2
        fns = []
        for half in range(2):
            c = 2 * p + half
            ks = na_row_slot(8 * g + 2 * c) if c < 8 else 2560 + (c - 8) * 128
            fns.append(lambda e, ks=ks, half=half, c=c: e.matmul(PS[po][0:65, :], lhsT=VN[:, ks // 128, h, 0:65], rhs=PT2[k][:, half * 512:(half + 1) * 512],
                                                               start=(c == 0), stop=(c == 9)))
        T.group("pe", fns, reads=["VN", "PT%d" % k], writes=["ps%d" % po])
        if p == 4:
            normalize_out(po, 8 + h, slice(g * 512, (g + 1) * 512), i, act_recip=True)
    LA = 3
    for i in range(len(na_steps) + LA):
        if i < len(na_steps):
            na_qk(i)
        if i >= LA:
            na_pv(i - LA)
        run_pending(i - LA)
    for _ in range(8):
        run_pending(0, flush=True)
    if stop_after == "na":
        dbg_out["AT"] = (AT, [128, 8, 2048], BF16)
        return finish(nc, T, dbg_out, out)
    T.barrier()

    for i in range(2):
        T.op("pool", lambda e, i=i: e.tensor_copy(out=KH[i][64:96, 0:3072], in_=krot_tmp[64:96, :]), reads=["krot_tmp"], writes=["KHr%d" % i])
    phaseA(range(6, NS), TAIL, False, lambda s: [(KH[i][64:96, s * 512:(s + 1) * 512], "KHr%d" % i) for i in range(2)], xn_base=ATR)
    T.barrier()

    cur[0] = ATT_W
    wuq_b = sb("wuq_b", [128, 3, 768], BF16)
    wuqs_b = sb("wuqs_b", [128, 3, 8, 96], BF16)
    wukv_b = sb("wukv_b", [128, 2, 1024], BF16)
    ctq = sbat("ctq", [128, 512], F32, TMP + 8192); stq = sbat("stq", [128, 512], F32, TMP + 10240)
    r1q = sbat("r1q", [128, 512], F32, TMP + 12288); r2q = sbat("r2q", [128, 512], F32, TMP + 14336)
    assert cur[0] <= 229344, cur[0]
    T.dma("pool", wuq_b[:], w_uq.rearrange("(kc p) c -> p kc c", p=128), writes=["wuq_b"])
    T.op("pool", lambda e: e.memset(wuqs_b[:], 0.0), writes=["wuqs_b"])
    for kc in range(3):
        T.dma("pool", wuqs_b[:, kc, :, 64:96], w_uqs[kc * 128:(kc + 1) * 128, :, :], writes=["wuqs_b"])
    T.op("pool", lambda e: e.tensor_scalar(out=wuqs_b[:, :, :, 64:80], in0=wuqs_b[:, :, :, 64:80], scalar1=-1.0, scalar2=None, op0=ALU.mult),
         reads=["wuqs_b"], writes=["wuqs_b"])
    T.dma("pool", wukv_b[:], w_ukv.rearrange("(kc p) c -> p kc c", p=128), writes=["wukv_b"])
    for i in range(2):
        T.op("pool", lambda e, i=i: e.memset(VH[i][:, :, 64:65], 1.0), writes=["VH%d" % i])
    SCALE = 96 ** -0.5

    def prep_units(h):
        b = h % 2
        units = []
        pbk = [0]
        def nb_():
            pbk[0] += 1
            return idle_set[0][pbk[0] % 2]
        for s in range(NS):
            def ku(s=s):
                PREP = nb_()
                T.group("pe", [lambda e, kc=kc: e.matmul(PS[PREP][0:64, :], lhsT=wukv_b[:, kc, h * 128:h * 128 + 64], rhs=kvn[:, kc, s * 512:(s + 1) * 512],
                                                         start=(kc == 0), stop=(kc == 1)) for kc in range(2)], reads=["kvn", "wukv_b"], writes=["ps%d" % PREP])
                T.op("dve", lambda e: e.tensor_copy(out=KH[b][0:64, s * 512:(s + 1) * 512], in_=PS[PREP][0:64, :]), reads=["ps%d" % PREP], writes=["KH%d" % b])
            units.append(ku)
            def vu(s=s):
                PREP = nb_()
                fns = []
                for tt in range(4):
                    for kc in range(2):
                        fns.append(lambda e, tt=tt, kc=kc: e.matmul(PS[PREP][:, tt * 64:(tt + 1) * 64], lhsT=kvn[:, kc, (4 * s + tt) * 128:(4 * s + tt + 1) * 128],
                                                                    rhs=wukv_b[:, kc, h * 128 + 64:h * 128 + 128], start=(kc == 0), stop=(kc == 1)))
                T.group("pe", fns, reads=["kvn", "wukv_b"], writes=["ps%d" % PREP])
                T.op("dve", lambda e: e.tensor_copy(out=VH[b][:, 4 * s:4 * s + 4, 0:64], in_=PS[PREP][:, 0:256].rearrange("p (t d) -> p t d", t=4)),
                     reads=["ps%d" % PREP], writes=["VH%d" % b])
            units.append(vu)
        for qc in range(4):
            def qu(qc=qc):
                PA_, PB_ = idle_set[0]
                qsl = slice(qc * 512, (qc + 1) * 512)
                T.dma("sp", ctq[64:96, :], ctab[:, qsl], writes=["ctq"])
                T.dma("sp", stq[64:96, :], stab[:, qsl], writes=["stq"])
                T.group("pe", [lambda e, kc=kc: e.matmul(PS[PA_][0:96, :], lhsT=wuq_b[:, kc, h * 96:(h + 1) * 96], rhs=qlatn[:, kc, qsl],
                                                         start=(kc == 0), stop=(kc == 2)) for kc in range(3)], reads=["qlatn", "wuq_b"], writes=["ps%d" % PA_])
                T.group("pe", [lambda e, kc=kc: e.matmul(PS[PB_][0:96, :], lhsT=wuqs_b[:, kc, h, :], rhs=qlatn[:, kc, qsl],
                                                         start=(kc == 0), stop=(kc == 2)) for kc in range(3)], reads=["qlatn", "wuqs_b"], writes=["ps%d" % PB_])
                T.op("dve", lambda e: e.tensor_copy(out=QH[b][0:64, qsl], in_=PS[PA_][0:64, :]), reads=["ps%d" % PA_], writes=["QH%d" % b])
                T.op("dve", lambda e: e.tensor_tensor(out=r1q[64:96, :], in0=PS[PA_][64:96, :], in1=ctq[64:96, :], op=ALU.mult), reads=["ps%d" % PA_, "ctq"], writes=["r1q"])
                T.op("dve", lambda e: e.tensor_tensor(out=r2q[64:96, :], in0=PS[PB_][64:96, :], in1=stq[64:96, :], op=ALU.mult), reads=["ps%d" % PB_, "stq"], writes=["r2q"])
                T.op("pool", lambda e: e.tensor_tensor(out=QH[b][64:96, qsl], in0=r1q[64:96, :], in1=r2q[64:96, :], op=ALU.add), reads=["r1q", "r2q"], writes=["QH%d" % b])
            units.append(qu)
        return units

    idle_set = [(2, 3)]
    for u in prep_units(0):
        u()
    gstep = [0]
    for h in range(8):
        b = h % 2
        nxt = prep_units(h + 1) if h < 7 else []
        ui = [0]
        steps = [(qp, kc) for qp in range(2) for kc in range(NT_MLA)]
        n = len(steps)
        ptm = {}
        def m_qk(i):
            qp, kc = steps[i]; pp = i % 2
            for half in range(2):
                qc = 2 * qp + half
                dst = PP[pp][:, half * 512:(half + 1) * 512]
                T.group("pe", [lambda e, dst=dst, kc=kc, qc=qc: e.matmul(dst, lhsT=KH[b][0:96, kc * 128:(kc + 1) * 128], rhs=QH[b][0:96, qc * 512:(qc + 1) * 512],
                                                                     start=True, stop=True)], reads=["KH%d" % b, "KHr%d" % b, "QH%d" % b], writes=["ps%d" % (4 + 2 * pp + half)])
            k = pti[0] % 3; pti[0] += 1
            ptm[i] = k
            T.op("act", lambda e: e.activation(out=PT2[k][:, :], in_=PP[pp][:, :], func=AF.Exp, bias=kmask[:, kc:kc + 1], scale=SCALE),
                 reads=["ps%d" % (4 + 2 * pp), "ps%d" % (5 + 2 * pp), "kmask"], writes=["PT%d" % k])
        def m_pv(i):
            qp, kc = steps[i]
            k = ptm.pop(i)
            ob_ = (0, 1) if qp == 0 else (2, 3)
            if kc == 0:
                idle_set[0] = (2, 3) if qp == 0 else (0, 1)
            for half in range(2):
                T.group("pe", [lambda e, half=half: e.matmul(PS[ob_[half]][0:65, :], lhsT=VH[b][:, kc, 0:65], rhs=PT2[k][:, half * 512:(half + 1) * 512],
                                                           start=(kc == 0), stop=(kc == NT_MLA - 1))], reads=["VH%d" % b, "PT%d" % k], writes=["ps%d" % ob_[half]])
            if kc == NT_MLA - 1:
                for half in range(2):
                    qc = 2 * qp + half
                    normalize_out(ob_[half], h, slice(qc * 512, (qc + 1) * 512), gstep[0], scr=ob_[half], off=4 * half)
            if nxt:
                want = min(len(nxt), (len(nxt) * (i + 1)) // (n - 16))
                while ui[0] < want:
                    nxt[ui[0]](); ui[0] += 1
        for i in range(n + 2):
            if i < n:
                m_qk(i)
            if i >= 2:
                gstep[0] += 1
                m_pv(i - 2)
                run_pending(gstep[0])
    for _ in range(8):
        run_pending(0, flush=True)
    if stop_after == "mla":
        dbg_out["AT"] = (AT, [128, 8, 2048], BF16)
        return finish(nc, T, dbg_out, out)
    T.barrier()

    T.op("pool", lambda e: e.memset(ss[:], 0.0), writes=["ss_all"])
    T.barrier()
    xnew = sbat("xnew", [128, 16, D], F32, A0)
    wout_b = sbat("wout_b", [128, 8, D], BF16, 117888)
    g1b = sbat("g1b", [128, D], F32, 134272)
    hx2T = sbat("hx2T", [128, 8, 2048], BF16, TAIL)
    xr = [sbat("xr%d" % i, [128, D], F32, TMP + i * 4096) for i in range(4)]
    xn2 = [sbat("xn2_%d" % i, [128, D], BF16, TMP + 16384 + i * 2048) for i in range(2)]
    T.dma("pool", wout_b[:], w_out.rearrange("(kc p) c -> p kc c", p=128), writes=["wout_b"])
    T.dma("sp", g1b[:], gscr[0:1, :].to_broadcast([128, D]), reads=["gscr_r"], writes=["g1b"], key="g1b")
    T.op("pool", lambda e: e.tensor_tensor(out=wout_b[:], in0=wout_b[:], in1=g1b[:].unsqueeze(1).to_broadcast([128, 8, D]), op=ALU.mult),
         reads=["wout_b", "g1b"], writes=["wout_b"])
    def wo_dma(t):
        T.dma("sp", xr[t % 4][:], xs[t * 128:(t + 1) * 128, :], writes=["xr%d" % (t % 4)])
    def wo_a(t):
        xb = xr[t % 4]; xk = "xr%d" % (t % 4)
        for hf in range(2):
            pi = psn(0, 7)
            T.group("pe", [lambda e, kc=kc, pi=pi, hf=hf: e.matmul(PS[pi][:, :], lhsT=AT[:, kc, t * 128:(t + 1) * 128], rhs=wout_b[:, kc, hf * 512:(hf + 1) * 512],
                                                               start=(kc == 0), stop=(kc == 7)) for kc in range(8)], reads=["AT", "wout_b"], writes=["ps%d" % pi])
            T.op("dve", lambda e, pi=pi, hf=hf: e.tensor_tensor(out=xnew[:, t, hf * 512:(hf + 1) * 512], in0=PS[pi][:, :], in1=xb[:, hf * 512:(hf + 1) * 512], op=ALU.add),
                 reads=["ps%d" % pi, xk], writes=["xnew%d" % t])
    def wo_b(t):
        norm_a(t, xnew[:, t, :], "xnew%d" % t, xn2[t % 2], "xn2_%d" % (t % 2), "rstd2_%d" % t)
    def wo_c(t):
        norm_b(xn2[t % 2], "xn2_%d" % (t % 2), lambda kc, t=t: hx2T[:, kc, t * 128:(t + 1) * 128], ["hx2T"] * 8, 16)
    for t in range(3):
        wo_dma(t)
    for k in range(16 + 2):
        if k + 3 < 16:
            wo_dma(k + 3)
        if k < 16:
            wo_a(k)
        if 1 <= k < 17:
            wo_b(k - 1)
        if k >= 2:
            wo_c(k - 2)
    if stop_after == "wout":
        dbg_out["xnew"] = (xnew, [128, 16, D], F32)
        return finish(nc, T, dbg_out, out)
    T.barrier()

    WB = 117888
    wexp = [[sbat("wg%d" % i, [128, 8, 512], BF16, WB + i * 24576),
             sbat("wu%d" % i, [128, 8, 512], BF16, WB + i * 24576 + 8192),
             sbat("wd%d" % i, [128, 4, D], BF16, WB + i * 24576 + 16384)] for i in range(2)]
    hidT = [sbat("hidT%d" % i, [128, 4, 512], BF16, WB + 49152 + i * 4096) for i in range(2)]
    g2b = sbat("g2b", [128, D], F32, WB + 57344)
    gfb = sbat("gfb", [128, D], F32, WB + 61440)
    assert WB + 65536 <= TAIL
    cur[0] = TMP
    wr_b = sb("wr_b", [128, 8, 20], BF16)
    brb = sb("brb", [128, 20], F32)
    lg = sb("lg", [128, 16, 20], F32)
    comb = sb("comb", [128, 16, 16], F32)
    rt = {n_: sb("rt_" + n_, [128, 16, 4], F32) for n_ in ("GE", "OH", "EL", "MK", "SEL", "EX", "TM")}
    rv = {n_: sb("rv_" + n_, [128, 16], F32) for n_ in ("GM", "SG", "GW", "M1", "M2", "SE", "RS", "WS")}
    sg = sb("sg", [128, 4, 512], BF16)
    ofin = [sb("ofin%d" % i, [128, D], F32) for i in range(2)]
    assert cur[0] <= A0, cur[0]
    T.dma("pool", wr_b[:], w_r.rearrange("(kc p) c -> p kc c", p=128), writes=["wr_b"])
    T.dma("sp", brb[:], b_r[None, :].to_broadcast([128, 20]), writes=["brb"])
    T.dma("sp", g2b[:], gscr[1:2, :].to_broadcast([128, D]), reads=["gscr_r"], writes=["g2b"], key="g2b")
    T.dma("sp", gfb[:], g_fin[None, :].to_broadcast([128, D]), writes=["gfb"])
    pr = psn(0, 4)
    for t in range(16):
        T.group("pe", [lambda e, kc=kc, t=t: e.matmul(PS[pr][:, t * 20:(t + 1) * 20], lhsT=hx2T[:, kc, t * 128:(t + 1) * 128], rhs=wr_b[:, kc, :], start=(kc == 0), stop=(kc == 7))
                       for kc in range(8)], reads=["hx2T", "wr_b"], writes=["ps%d" % pr])
    AXX = mybir.AxisListType.X
    def V(fn, rd, wr):
        T.op("dve", fn, reads=rd, writes=wr)
    def B4(ap):
        return ap.unsqueeze(2).to_broadcast([128, 16, 4])
    GL = lg[:, :, 0:4]
    V(lambda e: e.tensor_tensor(out=lg[:], in0=PS[pr][:, 0:320].rearrange("p (t c) -> p t c", c=20), in1=brb[:].unsqueeze(1).to_broadcast([128, 16, 20]), op=ALU.add),
      ["ps%d" % pr, "brb"], ["lg"])
    V(lambda e: e.reduce_max(out=rv["GM"][:], in_=GL, axis=AXX), ["lg"], ["GM"])
    V(lambda e: e.tensor_tensor(out=rt["GE"][:], in0=GL, in1=B4(rv["GM"][:]), op=ALU.subtract), ["lg", "GM"], ["GE"])
    T.op("act", lambda e: e.activation(out=rt["GE"][:], in_=rt["GE"][:], func=AF.Exp), reads=["GE"], writes=["GE"])
    V(lambda e: e.reduce_sum(out=rv["SG"][:], in_=rt["GE"][:], axis=AXX), ["GE"], ["SG"])
    V(lambda e: e.reciprocal(out=rv["GW"][:], in_=rv["SG"][:]), ["SG"], ["GW"])
    V(lambda e: e.tensor_tensor(out=rt["OH"][:], in0=GL, in1=B4(rv["GM"][:]), op=ALU.is_equal), ["lg", "GM"], ["OH"])
    for g in range(4):
        dst = rt["EL"] if g == 0 else rt["TM"]
        V(lambda e, g=g, dst=dst: e.tensor_tensor(out=dst[:], in0=lg[:, :, 4 + 4 * g:8 + 4 * g], in1=rt["OH"][:, :, g:g + 1].to_broadcast([128, 16, 4]), op=ALU.mult),
          ["lg", "OH"], ["EL" if g == 0 else "TM"])
        if g > 0:
            V(lambda e: e.tensor_tensor(out=rt["EL"][:], in0=rt["EL"][:], in1=rt["TM"][:], op=ALU.add), ["EL", "TM"], ["EL"])
    V(lambda e: e.reduce_max(out=rv["M1"][:], in_=rt["EL"][:], axis=AXX), ["EL"], ["M1"])
    V(lambda e: e.tensor_tensor(out=rt["MK"][:], in0=rt["EL"][:], in1=B4(rv["M1"][:]), op=ALU.is_equal), ["EL", "M1"], ["MK"])
    V(lambda e: e.scalar_tensor_tensor(out=rt["MK"][:], in0=rt["MK"][:], scalar=NEG, in1=rt["EL"][:], op0=ALU.mult, op1=ALU.add), ["MK", "EL"], ["MK"])
    V(lambda e: e.reduce_max(out=rv["M2"][:], in_=rt["MK"][:], axis=AXX), ["MK"], ["M2"])
    V(lambda e: e.tensor_tensor(out=rt["SEL"][:], in0=rt["EL"][:], in1=B4(rv["M2"][:]), op=ALU.is_ge), ["EL", "M2"], ["SEL"])
    V(lambda e: e.tensor_tensor(out=rt["EX"][:], in0=rt["EL"][:], in1=B4(rv["M1"][:]), op=ALU.subtract), ["EL", "M1"], ["EX"])
    T.op("act", lambda e: e.activation(out=rt["EX"][:], in_=rt["EX"][:], func=AF.Exp), reads=["EX"], writes=["EX"])
    V(lambda e: e.tensor_tensor(out=rt["EX"][:], in0=rt["EX"][:], in1=rt["SEL"][:], op=ALU.mult), ["EX", "SEL"], ["EX"])
    V(lambda e: e.reduce_sum(out=rv["SE"][:], in_=rt["EX"][:], axis=AXX), ["EX"], ["SE"])
    V(lambda e: e.reciprocal(out=rv["RS"][:], in_=rv["SE"][:]), ["SE"], ["RS"])
    V(lambda e: e.tensor_tensor(out=rv["WS"][:], in0=rv["RS"][:], in1=rv["GW"][:], op=ALU.mult), ["RS", "GW"], ["WS"])
    V(lambda e: e.tensor_tensor(out=rt["EX"][:], in0=rt["EX"][:], in1=B4(rv["WS"][:]), op=ALU.mult), ["EX", "WS"], ["EX"])
    for g in range(4):
        V(lambda e, g=g: e.tensor_tensor(out=comb[:, :, 4 * g:4 * g + 4], in0=rt["EX"][:], in1=rt["OH"][:, :, g:g + 1].to_broadcast([128, 16, 4]), op=ALU.mult),
          ["EX", "OH"], ["comb"])

    msteps = [(ex, s_) for ex in range(16) for s_ in range(4)]
    def moe_gu(i):
        ex, s_ = msteps[i]
        wg, wu, wd = wexp[ex % 2]; wk = "wexp%d" % (ex % 2)
        if s_ == 0:
            T.dma("pool", wg[:], w_gate[ex].rearrange("(kc p) f -> p kc f", p=128), writes=[wk + "g"])
            T.dma("pool", wu[:], w_up[ex].rearrange("(kc p) f -> p kc f", p=128), writes=[wk + "u"])
            T.dma("pool", wd[:], w_down[ex].rearrange("(fc p) d -> p fc d", p=128), writes=[wk + "d"])
            T.op("pool", lambda e: e.tensor_tensor(out=wd[:], in0=wd[:], in1=g2b[:].unsqueeze(1).to_broadcast([128, 4, D]), op=ALU.mult),
                 reads=[wk + "d", "g2b"], writes=[wk + "d"])
        hT = hidT[i % 2]; hk2 = "hidT%d" % (i % 2)
        tsl = slice(s_ * 512, (s_ + 1) * 512)
        for fc in range(4):
            pg = psn(0, 4); pu = psn(0, 4)
            T.group("pe", [lambda e, kc=kc, pg=pg, fc=fc: e.matmul(PS[pg][:, :], lhsT=wg[:, kc, fc * 128:(fc + 1) * 128], rhs=hx2T[:, kc, tsl], start=(kc == 0), stop=(kc == 7))
                           for kc in range(8)], reads=["hx2T", wk + "g"], writes=["ps%d" % pg])
            T.group("pe", [lambda e, kc=kc, pu=pu, fc=fc: e.matmul(PS[pu][:, :], lhsT=wu[:, kc, fc * 128:(fc + 1) * 128], rhs=hx2T[:, kc, tsl], start=(kc == 0), stop=(kc == 7))
                           for kc in range(8)], reads=["hx2T", wk + "u"], writes=["ps%d" % pu])
            T.op("act", lambda e, pg=pg, fc=fc: e.activation(out=sg[:, fc, :], in_=PS[pg][:, :], func=AF.Silu), reads=["ps%d" % pg], writes=["sg%d" % fc])
            T.op("dve", lambda e, pu=pu, fc=fc, hT=hT: e.tensor_tensor(out=hT[:, fc, :], in0=PS[pu][:, :], in1=sg[:, fc, :], op=ALU.mult),
                 reads=["ps%d" % pu, "sg%d" % fc], writes=[hk2 + "_%d" % fc])
    def moe_down(i):
        ex, s_ = msteps[i]
        wg, wu, wd = wexp[ex % 2]; wk = "wexp%d" % (ex % 2)
        hT = hidT[i % 2]; hk2 = "hidT%d" % (i % 2)
        for tt in range(4):
            t = 4 * s_ + tt
            for hf in range(2):
                py = psn(4, 7)
                T.group("pe", [lambda e, fc=fc, py=py, tt=tt, hf=hf: e.matmul(PS[py][:, :], lhsT=hT[:, fc, tt * 128:(tt + 1) * 128], rhs=wd[:, fc, hf * 512:(hf + 1) * 512],
                                                                         start=(fc == 0), stop=(fc == 3)) for fc in range(4)],
                        reads=[hk2 + "_%d" % fc for fc in range(4)] + [wk + "d"], writes=["ps%d" % py])
                T.op("dve", lambda e, py=py, hf=hf, t=t: e.scalar_tensor_tensor(out=xnew[:, t, hf * 512:(hf + 1) * 512], in0=PS[py][:, :], scalar=comb[:, t, ex:ex + 1],
                                                                         in1=xnew[:, t, hf * 512:(hf + 1) * 512], op0=ALU.mult, op1=ALU.add),
                     reads=["ps%d" % py, "comb", "xnew%d" % t], writes=["xnew%d" % t])
    for i in range(len(msteps) + 1):
        if i < len(msteps):
            moe_gu(i)
        if i >= 1:
            moe_down(i - 1)

    for t in range(16):
        ob = ofin[t % 2]; ok = "ofin%d" % (t % 2)
        T.op("act", lambda e, ob=ob, t=t: e.activation(out=ob[:], in_=xnew[:, t, :], func=AF.Square, accum_out=ss[:, 32 + t:33 + t]), reads=["xnew%d" % t], writes=[ok, "ss3_%d" % t])
        T.op("act", lambda e, t=t: e.activation(out=sd[:, 32 + t:33 + t], in_=ss[:, 32 + t:33 + t], func=AF.Sqrt, bias=epsc[:], scale=1.0 / D), reads=["ss3_%d" % t, "epsc"], writes=["sd3_%d" % t])
        T.op("dve", lambda e, t=t: e.reciprocal(out=rstd[:, 32 + t:33 + t], in_=sd[:, 32 + t:33 + t]), reads=["sd3_%d" % t], writes=["rstd3_%d" % t])
        T.op("dve", lambda e, ob=ob, t=t: e.scalar_tensor_tensor(out=ob[:], in0=xnew[:, t, :], scalar=rstd[:, 32 + t:33 + t], in1=gfb[:], op0=ALU.mult, op1=ALU.mult),
             reads=["xnew%d" % t, "rstd3_%d" % t, "gfb"], writes=[ok])
        T.dma("sp", out[t * 128:(t + 1) * 128, :], ob[:], reads=[ok], key="outst%d" % (t % 2))
    return finish(nc, T, dbg_out, out)


def finish(nc, T, dbg_out, out):
    for name, (tens, shape, dt) in dbg_out.items():
        d = nc.dram_tensor("dbg_" + name, list(shape), dt, kind="ExternalOutput").ap()
        idx = tuple(slice(None) for _ in shape)
        T.dma("sp", d[idx], tens[idx], reads=[name], key="dbg_" + name)
    T.finish()
    return nc


GRID_W = 64
_NC_CACHE = {}


def _rope_tables(rows, cols):
    half = 16
    inv_freq = (10000.0 ** (-np.arange(0, half, 2, dtype=np.float32) / half)).astype(np.float32)
    ang = np.concatenate([rows.astype(np.float32)[:, None] * inv_freq, cols.astype(np.float32)[:, None] * inv_freq], axis=-1)
    return np.cos(ang).astype(np.float32), np.sin(ang).astype(np.float32)


def _core_layout(j):
    R0 = 32 * j
    tok = np.full(NSLOT, -1, np.int64)
    tok[0:2048] = np.arange(R0 * 64, (R0 + 32) * 64)
    used = np.zeros(8192, bool); used[R0 * 64:(R0 + 32) * 64] = True
    for i, r in enumerate(list(range(R0 - 4, R0)) + list(range(R0 + 32, R0 + 36))):
        if 0 <= r < 128:
            tok[2048 + i * 64:2048 + (i + 1) * 64] = np.arange(r * 64, (r + 1) * 64)
            used[r * 64:(r + 1) * 64] = True
    tok[2560:2816] = -2 - np.arange(256)
    others = np.nonzero(~used)[0]
    free_halo = np.nonzero(tok[2048:2560] == -1)[0] + 2048
    nfill = len(free_halo)
    if nfill:
        tok[free_halo] = others[len(others) - nfill:]
        others = others[:len(others) - nfill]
    assert len(others) == 5632, len(others)
    tok[2816:2816 + len(others)] = others
    assert (tok[:8448] != -1).all() and (tok[8448:] == -1).all()
    return tok


def _host_inputs(inp):
    x = np.asarray(inp["x"], np.float32); ctx = np.asarray(inp["ctx"], np.float32)
    w_in = np.ascontiguousarray(np.asarray(inp["w_in"], np.float32)[0])
    w_uq = np.ascontiguousarray(np.asarray(inp["w_uq"], np.float32)[0])
    perm = np.concatenate([np.arange(16, 32), np.arange(0, 16)])
    w_krs = np.ascontiguousarray(w_in[:, C_KR:C_KR + 32][:, perm])
    w_uqs = np.ascontiguousarray(w_uq.reshape(384, 8, 96)[:, :, 64:96][:, :, perm])
    rel = np.asarray(inp["na_rel_bias"], np.float32)[0]
    a = np.arange(2)[:, None, None, None]; ck = np.arange(64)[None, :, None, None]
    i = np.arange(8)[None, None, :, None]; cq = np.arange(64)[None, None, None, :]
    cstart = np.clip(cq - 8, 0, 48)
    col_in = (ck >= cstart) & (ck < cstart + 16)
    dc = ck - cq + 15
    nab = np.full((8, 8, 2, 64, 8, 64), NEG, np.float32)
    for c in range(8):
        dr = 2 * c + a - i + 3
        ok = (dr >= 0) & (dr <= 14) & col_in
        okb = np.broadcast_to(ok, (2, 64, 8, 64))
        drb = np.broadcast_to(np.clip(dr, 0, 14), (2, 64, 8, 64)); dcb = np.broadcast_to(np.clip(dc, 0, 30), (2, 64, 8, 64))
        for h in range(8):
            g = rel[h][drb, dcb]
            nab[h, c] = np.where(okb, g, np.float32(NEG))
    nab = np.ascontiguousarray(nab.reshape(8, 8, 128, 512))
    rowsel = np.zeros((16, 8, 2, 64), np.float32)
    for c in range(8):
        for aa in range(2):
            rowsel[2 * c + aa, c, aa, :] = 1.0
    rowsel = rowsel.reshape(16, 8, 128)
    common = dict(
        w_mod=np.ascontiguousarray(np.asarray(inp["w_mod"], np.float32)[0]), b_mod=np.ascontiguousarray(np.asarray(inp["b_mod"], np.float32)[0]),
        g_attn=np.ascontiguousarray(np.asarray(inp["norm_attn_g"], np.float32)[0]), g_ffn=np.ascontiguousarray(np.asarray(inp["norm_ffn_g"], np.float32)[0]),
        g_fin=np.asarray(inp["final_norm_g"], np.float32), w_in=w_in, w_krs=w_krs,
        gq=np.ascontiguousarray(np.asarray(inp["q_a_norm_g"], np.float32)[0]), gkv=np.ascontiguousarray(np.asarray(inp["kv_a_norm_g"], np.float32)[0]),
        w_uq=w_uq, w_uqs=w_uqs, w_ukv=np.ascontiguousarray(np.asarray(inp["w_ukv"], np.float32)[0]),
        w_out=np.ascontiguousarray(np.asarray(inp["w_out"], np.float32)[0]),
        w_r=np.ascontiguousarray(np.concatenate([np.asarray(inp["w_router_group"], np.float32)[0], np.asarray(inp["w_router_expert"], np.float32)[0]], axis=1)),
        b_r=np.ascontiguousarray(np.concatenate([np.asarray(inp["b_router_group"], np.float32)[0], np.asarray(inp["b_router_expert"], np.float32)[0]])),
        w_gate=np.ascontiguousarray(np.asarray(inp["w_gate"], np.float32)[0]), w_up=np.ascontiguousarray(np.asarray(inp["w_up"], np.float32)[0]),
        w_down=np.ascontiguousarray(np.asarray(inp["w_down"], np.float32)[0]),
        nab=nab,
    )
    maps = []
    for core in range(8):
        b, j = core // 4, core % 4
        R0 = 32 * j
        tok = _core_layout(j)
        xs = np.zeros((NSLOT, D), np.float32)
        m = tok >= 0
        xs[m] = x[b][tok[m]]
        xs[2560:2816] = ctx[b]
        kmask = np.where(tok == -1, np.float32(NEG), np.float32(0)).astype(np.float32).reshape(NT, 128).T.copy()
        rows = np.where(m, tok // GRID_W, 0); cols = np.where(m, tok % GRID_W, 0)
        cos, sin = _rope_tables(rows, cols)
        cos[~m] = 1.0; sin[~m] = 0.0
        ctab = np.ascontiguousarray(np.concatenate([cos, cos], axis=1).T)
        stab = np.ascontiguousarray(np.concatenate([sin, sin], axis=1).T)
        narm = np.full((16, 4, 8, 64), NEG, np.float32)
        for g in range(4):
            for i_ in range(8):
                r = R0 + 8 * g + i_
                start = min(max(r - 4, 0), 120)
                for lp in range(16):
                    kr = R0 - 4 + 8 * g + lp
                    if start <= kr < start + 8 and 0 <= kr < 128:
                        narm[lp, g, i_, :] = 0.0
        d = dict(common)
        RM = narm[:, :, :, 0]
        rmc = np.zeros((128, 4, 8, 8), np.float32)
        for c_ in range(8):
            for a_ in range(2):
                rmc[a_ * 64:(a_ + 1) * 64, :, c_, :] = RM[2 * c_ + a_][None, :, :]
        d.update(xs=xs, rmc=np.ascontiguousarray(rmc.reshape(128, 4, 64)), cvec=np.ascontiguousarray(np.stack([np.asarray(inp["c"], np.float32)[b], np.asarray(inp["c_ctx"], np.float32)])),
                 kmask=kmask, ctab=ctab, stab=stab)
        maps.append(d)
    return maps


def kernel(**inputs):
    if "nc" not in _NC_CACHE:
        _NC_CACHE["nc"] = build()
    nc = _NC_CACHE["nc"]
    maps = _host_inputs(inputs)
    res = run_bass_kernel_spmd(nc, maps, core_ids=list(range(8)))
    out = np.zeros((2, 8192, D), np.float32)
    for core in range(8):
        b, j = core // 4, core % 4
        out[b, j * 2048:(j + 1) * 2048] = res.results[core]["out"]
    return out
```

```python
import numpy as np
import concourse.bass as bass
import concourse.mybir as mybir
from concourse.bass_utils import run_bass_kernel_spmd

F32, BF16 = mybir.dt.float32, mybir.dt.bfloat16
AF = mybir.ActivationFunctionType
ALU = mybir.AluOpType

D = 1024
NT = 68
NT_MLA = 66
NS = 17
NSLOT = NT * 128
QC0, KV0 = 0, 896
C_KVLAT, C_KR, C_NK, C_NV = 896, 1152, 1184, 1696
EPS = 1e-6
import os
EVAC_MODE = int(os.environ.get('EVAC_MODE', '2'))
NEG = -1e30


class Trk:
    def __init__(self, nc):
        self.nc = nc
        self.eng = {}
        for name, h in (("pe", nc.tensor), ("act", nc.scalar), ("dve", nc.vector), ("pool", nc.gpsimd), ("sp", nc.sync)):
            self.eng[name] = dict(h=h, sem=nc.alloc_semaphore("s_" + name), cnt=0, seen={})
        self.res = {}
        self.dsem = {}

    def _r(self, k):
        if k not in self.res:
            self.res[k] = dict(w=None, r={})
        return self.res[k]

    def _waits(self, en, reads, writes):
        e = self.eng[en]
        need = {}
        def add(ev):
            if ev is None:
                return
            sem, val, src = ev
            if src == "pe" and en == "pe":
                return
            if need.get(sem.name, (None, 0))[1] < val:
                need[sem.name] = (sem, val)
        for k in reads:
            add(self._r(k)["w"])
        for k in writes:
            r = self._r(k)
            add(r["w"])
            for ev in r["r"].values():
                add(ev)
        for sn, (sem, val) in need.items():
            if e["seen"].get(sn, 0) < val:
                e["h"].wait_ge(sem, val)
                e["seen"][sn] = val

    def _mark(self, ev, reads, writes):
        for k in writes:
            r = self._r(k)
            r["w"] = ev
            r["r"] = {}
        for k in reads:
            r = self._r(k)
            r["r"][ev[0].name + ev[2]] = ev

    def op(self, en, fn, reads=(), writes=()):
        e = self.eng[en]
        self._waits(en, reads, writes)
        ins = fn(e["h"])
        e["cnt"] += 1
        ins.then_inc(e["sem"], 1)
        self._mark((e["sem"], e["cnt"], en), reads, writes)

    def group(self, en, fns, reads=(), writes=()):
        e = self.eng[en]
        self._waits(en, reads, writes)
        ins = None
        for fn in fns:
            ins = fn(e["h"])
        e["cnt"] += 1
        ins.then_inc(e["sem"], 1)
        self._mark((e["sem"], e["cnt"], en), reads, writes)

    def dma(self, qn, out, in_, reads=(), writes=(), key=None):
        e = self.eng[qn]
        self._waits(qn, reads, writes)
        key = key or (writes[0] if writes else reads[0])
        if key not in self.dsem:
            self.dsem[key] = [self.nc.alloc_semaphore("d%d" % len(self.dsem)), 0]
        ds = self.dsem[key]
        e["h"].dma_start(out=out, in_=in_).then_inc(ds[0], 16)
        ds[1] += 16
        self._mark((ds[0], ds[1], "dma"), reads, writes)

    def barrier(self):
        for en, e in self.eng.items():
            for fn, f in self.eng.items():
                if fn != en and f["cnt"] > e["seen"].get(f["sem"].name, 0):
                    e["h"].wait_ge(f["sem"], f["cnt"])
                    e["seen"][f["sem"].name] = f["cnt"]
            for k, (sem, tot) in self.dsem.items():
                if tot > e["seen"].get(sem.name, 0):
                    e["h"].wait_ge(sem, tot)
                    e["seen"][sem.name] = tot

    def finish(self):
        e = self.eng["sp"]
        for fn, f in self.eng.items():
            if fn != "sp" and f["cnt"] > e["seen"].get(f["sem"].name, 0):
                e["h"].wait_ge(f["sem"], f["cnt"])
        for k, (sem, tot) in self.dsem.items():
            if tot > e["seen"].get(sem.name, 0):
                e["h"].wait_ge(sem, tot)


def na_row_slot(l):
    if 4 <= l < 36:
        return (l - 4) * 64
    if l < 4:
        return 2048 + l * 64
    return 2304 + (l - 36) * 64


def build(stop_after=None, dbg=None):
    nc = bass.Bass("TRN2", target_bir_lowering=False)
    T = Trk(nc)

    def din(name, shape, dt=F32):
        return nc.dram_tensor(name, list(shape), dt, kind="ExternalInput").ap()

    xs = din("xs", [NSLOT, D])
    cvec = din("cvec", [2, D])
    w_mod = din("w_mod", [D, 6 * D]); b_mod = din("b_mod", [6 * D])
    g_attn = din("g_attn", [D]); g_ffn = din("g_ffn", [D]); g_fin = din("g_fin", [D])
    w_in = din("w_in", [D, 2208]); w_krs = din("w_krs", [D, 32])
    gq = din("gq", [384]); gkv = din("gkv", [256])
    w_uq = din("w_uq", [384, 768]); w_uqs = din("w_uqs", [384, 8, 32]); w_ukv = din("w_ukv", [256, 1024])
    w_out = din("w_out", [D, D])
    w_r = din("w_r", [D, 20]); b_r = din("b_r", [20])
    w_gate = din("w_gate", [16, D, 512]); w_up = din("w_up", [16, D, 512]); w_down = din("w_down", [16, 512, D])
    kmask_d = din("kmask", [128, NT])
    ctab = din("ctab", [32, NSLOT]); stab = din("stab", [32, NSLOT])
    nab = din("nab", [8, 8, 128, 512]); rmc_d = din("rmc", [128, 4, 64])
    out = nc.dram_tensor("out", [2048, D], F32, kind="ExternalOutput").ap()
    dbg_out = {}

    def sbat(name, shape, dt, at):
        nbytes = int(np.prod(shape[1:])) * (2 if dt == BF16 else 4)
        assert at % 32 == 0 and at + nbytes <= 229344, (name, at, nbytes)
        return nc.alloc_sbuf_tensor_at(name, list(shape), dt, offset=at)
    cur = [16512]
    def sb(name, shape, dt):
        nbytes = (int(np.prod(shape[1:])) * (2 if dt == BF16 else 4) + 31) // 32 * 32
        off = cur[0]; cur[0] += nbytes
        return sbat(name, shape, dt, off)
    ident = sb("ident", [128, 128], BF16)
    ones_bf = sb("ones_bf", [128, 128], BF16)
    onesf = sb("onesf", [128, 128], F32)
    epsc = sb("epsc", [128, 1], F32)
    modc = sb("modc", [128, 64], F32)
    gqc = sb("gqc", [128, 4], F32); gkvc = sb("gkvc", [128, 2], F32)
    kmask = sb("kmask_s", [128, NT], F32)
    ss = sb("ss", [128, NT], F32); sd = sb("sd", [128, NT], F32); rstd = sb("rstd", [128, NT], F32)
    rmc = sb("rmc_s", [128, 4, 64], BF16)
    shiftm = sb("shiftm", [64, 128], BF16)
    assert cur[0] <= 25728, cur[0]
    TMP = 25728
    cur[0] = TMP
    tmpf = [sb("tmpf%d" % i, [128, 512], F32) for i in range(3)]
    sq = [sb("sq%d" % i, [128, 512], BF16) for i in range(3)]
    sdb = sb("sdb", [128, 512], F32)
    rb = sb("rb", [128, 512], F32)
    ctt = sb("ctt", [128, 512], F32); stt = sb("stt", [128, 512], F32)
    r2 = sb("r2", [128, 512], F32)
    krot_tmp = sb("krot_tmp", [128, 3072], BF16)
    assert cur[0] <= 52352, cur[0]
    A0 = 52352
    kvn = sbat("kvn", [128, 2, NSLOT], BF16, A0)
    qlatn = sbat("qlatn", [128, 3, 2048], BF16, A0 + 34816)
    NR = A0 + 34816 + 12288
    KN = sbat("KN", [128, 4, 2816], BF16, NR)
    VN = sbat("VN", [128, 22, 8, 65], BF16, NR + 22528)
    QN = sbat("QN", [128, 4, 2048], BF16, NR + 22528 + 22880)
    NR_END = NR + 22528 + 22880 + 16384
    KH = [sbat("KH%d" % i, [128, NSLOT], BF16, NR + i * 17408) for i in range(2)]
    VH = [sbat("VH%d" % i, [128, NT, 65], BF16, NR + 34816 + i * 8864) for i in range(2)]
    QH = [sbat("QH%d" % i, [128, 2048], BF16, NR + 34816 + 17728 + i * 4096) for i in range(2)]
    assert NR + 34816 + 17728 + 8192 <= NR_END
    ATR = NR_END
    AT = sbat("AT", [128, 8, 2048], BF16, ATR)
    TAIL = ATR + 32768
    assert TAIL == 194016, TAIL
    cur[0] = TAIL
    PT2 = [sb("PT%d" % i, [128, 1024], BF16) for i in range(4)]
    osb = [sbat("osb%d" % i, [128, 512], F32, TMP + i * 2048) for i in range(2)]
    rc = sbat("rc", [128, 512], F32, TMP + 4096)
    onbs = [sbat("onb%d" % i, [64, 512], BF16, TMP + 6144 + i * 1024) for i in range(2)]
    ATT_W = cur[0]
    class Half:
        def __init__(self, t, off):
            self.t, self.off = t, off
        def __getitem__(self, key):
            r, c = key
            a = (c.start or 0) + self.off
            b_ = (c.stop if c.stop is not None else 512) + self.off
            return self.t[r, a:b_]
    PP = [nc.alloc_psum_tensor("pp%d" % i, [128, 1024], F32) for i in range(2)]
    PS = [nc.alloc_psum_tensor("ps%d" % i, [128, 512], F32) for i in range(4)]
    PS += [Half(PP[0], 0), Half(PP[0], 512), Half(PP[1], 0)]
    pTt = PP[1][:, 512:1024].bitcast(BF16)
    rr = [0]
    def psn(lo=0, hi=8):
        i = lo + rr[0] % (hi - lo); rr[0] += 1
        return i
    gscr = nc.dram_tensor("gscr", [2, D], F32).ap()

    T.op("pool", lambda e: e.memset(ident[:], 0.0), writes=["ident"])
    T.op("pool", lambda e: e.affine_select(out=ident[:], in_=ident[:], pattern=[[-1, 128]], compare_op=ALU.not_equal,
                                           fill=1.0, base=0, channel_multiplier=1), reads=["ident"], writes=["ident"])
    T.op("pool", lambda e: e.memset(shiftm[:], 0.0), writes=["shiftm"])
    T.op("pool", lambda e: e.affine_select(out=shiftm[:], in_=shiftm[:], pattern=[[-1, 128]], compare_op=ALU.not_equal,
                                           fill=1.0, base=64, channel_multiplier=1), reads=["shiftm"], writes=["shiftm"])
    T.op("pool", lambda e: e.memset(ones_bf[:], 1.0), writes=["ones_bf"])
    T.op("pool", lambda e: e.memset(onesf[:], 1.0), writes=["onesf"])
    T.op("pool", lambda e: e.memset(epsc[:], EPS), writes=["epsc"])
    T.op("pool", lambda e: e.memset(ss[:], 0.0), writes=["ss_all"])
    T.dma("sp", kmask[:], kmask_d[:, :], writes=["kmask"])
    T.dma("pool", rmc[:], rmc_d[:, :, :], writes=["rmc"])
    with nc.allow_non_contiguous_dma(reason="tiny column loads"):
        T.dma("sp", gqc[:, 0:3], gq.rearrange("(c p) -> p c", p=128), writes=["gqc"])
        T.dma("sp", gkvc[:, 0:2], gkv.rearrange("(c p) -> p c", p=128), writes=["gkvc"])

    PTR = 7
    pT = pTt

    def rms_feat(ps_list, nfeat, gcol, gkey, dst_fn, dkey):
        n = len(ps_list)
        for c, pi in enumerate(ps_list):
            T.op("act", lambda e, c=c, pi=pi: e.activation(out=tmpf[c][:], in_=PS[pi][:, :], func=AF.Copy), reads=["ps%d" % pi], writes=["tmpf%d" % c])
            T.op("act", lambda e, c=c: e.activation(out=sq[c][:], in_=tmpf[c][:], func=AF.Square), reads=["tmpf%d" % c], writes=["sq%d" % c])
        pq = psn(0, 7)
        T.group("pe", [lambda e, c=c, pq=pq: e.matmul(PS[pq][:, :], lhsT=ones_bf[:], rhs=sq[c][:], start=(c == 0), stop=(c == n - 1)) for c in range(n)],
                reads=["ones_bf"] + ["sq%d" % c for c in range(n)], writes=["ps%d" % pq])
        T.op("act", lambda e, pq=pq: e.activation(out=sdb[:], in_=PS[pq][:, :], func=AF.Sqrt, bias=epsc[:], scale=1.0 / nfeat),
             reads=["ps%d" % pq, "epsc"], writes=["sdb"])
        T.op("dve", lambda e: e.reciprocal(out=rb[:], in_=sdb[:]), reads=["sdb"], writes=["rb"])
        for c in range(n):
            T.op("dve", lambda e, c=c: e.scalar_tensor_tensor(out=dst_fn(c), in0=tmpf[c][:], scalar=gcol[:, c:c + 1], in1=rb[:], op0=ALU.mult, op1=ALU.mult),
                 reads=["tmpf%d" % c, "rb", gkey], writes=[dkey])

    def norm_a(t, xb, xk, nb, nk, rkey):
        T.op("act", lambda e: e.activation(out=nb[:], in_=xb, func=AF.Square, accum_out=ss[:, t:t + 1]), reads=[xk], writes=[nk, "ss%d" % t])
        T.op("act", lambda e: e.activation(out=sd[:, t:t + 1], in_=ss[:, t:t + 1], func=AF.Sqrt, bias=epsc[:], scale=1.0 / D),
             reads=["ss%d" % t, "epsc"], writes=["sd%d" % t])
        T.op("dve", lambda e: e.reciprocal(out=rstd[:, t:t + 1], in_=sd[:, t:t + 1]), reads=["sd%d" % t], writes=[rkey])
        T.op("act", lambda e: e.activation(out=nb[:], in_=xb, func=AF.Identity, scale=rstd[:, t:t + 1]), reads=[xk, rkey], writes=[nk])

    def norm_b(nb, nk, dst_fn, dkeys, cb):
        T.group("pe", [lambda e, kc=kc: e.transpose(out=pT[:, kc * 128:(kc + 1) * 128], in_=nb[:, kc * 128:(kc + 1) * 128], identity=ident[:])
                       for kc in range(8)], reads=[nk, "ident"], writes=["ps%d" % PTR])
        for kc in range(8):
            dst = dst_fn(kc); src = pT[:, kc * 128:(kc + 1) * 128]
            T.op("dve", lambda e, dst=dst, src=src, kc=kc: e.tensor_scalar(out=dst, in0=src, scalar1=modc[:, cb + kc:cb + kc + 1],
                                                                     scalar2=modc[:, cb + 8 + kc:cb + 9 + kc], op0=ALU.mult, op1=ALU.add),
                 reads=["ps%d" % PTR, "modc"], writes=[dkeys[kc]])

    def norm_transpose(src_ap, t, xb, xk, nb, nk, dst_fn, dkeys, cb, rkey):
        norm_a(t, xb, xk, nb, nk, rkey)
        norm_b(nb, nk, dst_fn, dkeys, cb)

    def phaseA(s_list, base, full, krot_dst, xn_base=None, state=None, load_only=False):
        if state is not None:
            return phaseA_run(s_list, full, krot_dst, state)
        o = [base]
        def wa(name, shape, dt):
            nbytes = (int(np.prod(shape[1:])) * (2 if dt == BF16 else 4) + 31) // 32 * 32
            t_ = sbat(name, shape, dt, o[0]); o[0] += nbytes
            return t_
        sfx = "f" if full else "p"
        ncols = 2208 if full else 256
        win_b = wa("win_b" + sfx, [128, 8, ncols], BF16)
        wkr_b = wa("wkr_b" + sfx, [128, 8, 96], BF16)
        wkrs_b = wa("wkrs_b" + sfx, [128, 8, 96], BF16)
        xt = [wa("xt%d%s" % (i, sfx), [128, D], F32) for i in range(2)]
        if xn_base is not None:
            xt += [sbat("xt%d%s" % (2 + i, sfx), [128, D], F32, xn_base + 4096 + i * 4096) for i in range(2)]
        if xn_base is None:
            xn = [wa("xn%d%s" % (i, sfx), [128, D], BF16) for i in range(2)]
        else:
            xn = [sbat("xn%d%s" % (i, sfx), [128, D], BF16, xn_base + i * 2048) for i in range(2)]
        hxT = [wa("hxT%d%s" % (i, sfx), [128, 8, 512], BF16) for i in range(2)]
        assert o[0] <= 229344, o[0]
        wkey = "win_b"
        if full:
            for kc in range(8):
                T.dma("pool", win_b[:, kc, :], w_in[kc * 128:(kc + 1) * 128, :], writes=[wkey])
            cko = C_KVLAT
        else:
            T.dma("pool", win_b[:], w_in[:, C_KVLAT:C_KVLAT + 256].rearrange("(kc p) c -> p kc c", p=128), writes=[wkey])
            cko = 0
        T.op("pool", lambda e: e.memset(wkr_b[:], 0.0), writes=["wkr_b"])
        T.op("pool", lambda e: e.memset(wkrs_b[:], 0.0), writes=["wkrs_b"])
        T.dma("pool", wkr_b[:, :, 64:96], w_in[:, C_KR:C_KR + 32].rearrange("(kc p) c -> p kc c", p=128), writes=["wkr_b"])
        T.dma("pool", wkrs_b[:, :, 64:96], w_krs.rearrange("(kc p) c -> p kc c", p=128), writes=["wkrs_b"])
        T.op("pool", lambda e: e.tensor_scalar(out=wkrs_b[:, :, 64:80], in0=wkrs_b[:, :, 64:80], scalar1=-1.0, scalar2=None, op0=ALU.mult),
             reads=["wkrs_b"], writes=["wkrs_b"])
        state_ = dict(win_b=win_b, wkr_b=wkr_b, wkrs_b=wkrs_b, xt=xt, xn=xn, hxT=hxT, wkey=wkey, cko=cko)
        if load_only:
            return state_
        return phaseA_run(s_list, full, krot_dst, state_)

    def phaseA_run(s_list, full, krot_dst, st_):
        win_b, wkr_b, wkrs_b, xt, xn, hxT, wkey, cko = (st_[k] for k in ("win_b", "wkr_b", "wkrs_b", "xt", "xn", "hxT", "wkey", "cko"))
        if stop_after == "paw":
            return "stop"
        def tinfo(s, tt):
            t = 4 * s + tt
            nx = len(xt)
            return t, xt[t % nx], "xt%d" % (t % nx), xn[t % 2], "xn%d" % (t % 2)
        def nt_dma(s, tt):
            t, xb, xk, nb, nk = tinfo(s, tt)
            T.dma("sp", xb[:], xs[t * 128:(t + 1) * 128, :], writes=[xk])
        def nt_a(s, tt):
            t, xb, xk, nb, nk = tinfo(s, tt)
            norm_a(t, xb[:], xk, nb, nk, "rstd%d" % t)
        def nt_b(s, tt):
            t, xb, xk, nb, nk = tinfo(s, tt)
            hb = hxT[s % 2]; hk = "hxT%d" % (s % 2)
            hks = [hk + "_%d" % kc for kc in range(8)]
            cb = 32 if t in (20, 21) else 0
            norm_b(nb, nk, lambda kc, tt=tt, hb=hb: hb[:, kc, tt * 128:(tt + 1) * 128], hks, cb)

        def mm_parts(s):
            hb = hxT[s % 2]; hk = "hxT%d" % (s % 2)
            hks = [hk + "_%d" % kc for kc in range(8)]
            sl = slice(s * 512, (s + 1) * 512)
            def p0():
                pl = []
                for c in range(2):
                    pi = psn(0, 7); pl.append(pi)
                    T.group("pe", [lambda e, kc=kc, pi=pi, c=c: e.matmul(PS[pi][:, :], lhsT=win_b[:, kc, cko + c * 128:cko + (c + 1) * 128], rhs=hb[:, kc, :],
                                                                     start=(kc == 0), stop=(kc == 7)) for kc in range(8)],
                            reads=hks + [wkey], writes=["ps%d" % pi])
                rms_feat(pl, 256, gkvc, "gkvc", lambda c: kvn[:, c, sl], "kvn")
                pk = psn(0, 7); pks = psn(0, 7)
                T.group("pe", [lambda e, kc=kc: e.matmul(PS[pk][0:96, :], lhsT=wkr_b[:, kc, :], rhs=hb[:, kc, :], start=(kc == 0), stop=(kc == 7)) for kc in range(8)],
                        reads=hks + ["wkr_b"], writes=["ps%d" % pk])
                T.group("pe", [lambda e, kc=kc: e.matmul(PS[pks][0:96, :], lhsT=wkrs_b[:, kc, :], rhs=hb[:, kc, :], start=(kc == 0), stop=(kc == 7)) for kc in range(8)],
                        reads=hks + ["wkrs_b"], writes=["ps%d" % pks])
                T.dma("sp", ctt[64:96, :], ctab[:, sl], writes=["ctt"])
                T.dma("sp", stt[64:96, :], stab[:, sl], writes=["stt"])
                T.op("dve", lambda e: e.tensor_tensor(out=tmpf[2][64:96, :], in0=PS[pk][64:96, :], in1=ctt[64:96, :], op=ALU.mult), reads=["ps%d" % pk, "ctt"], writes=["tmpf2"])
                T.op("dve", lambda e: e.tensor_tensor(out=r2[64:96, :], in0=PS[pks][64:96, :], in1=stt[64:96, :], op=ALU.mult), reads=["ps%d" % pks, "stt"], writes=["r2"])
                for dst, dk in krot_dst(s):
                    T.op("pool", lambda e, dst=dst: e.tensor_tensor(out=dst, in0=tmpf[2][64:96, :], in1=r2[64:96, :], op=ALU.add), reads=["tmpf2", "r2"], writes=[dk])
            def p1():
                if not (full and s < 6):
                    return
                nsl = slice(s * 512, (s + 1) * 512) if s < 5 else slice(2560, 2816)
                ncol = slice(0, 512) if s < 5 else slice(0, 256)
                for j in range(4):
                    pi = psn(0, 7)
                    T.group("pe", [lambda e, kc=kc, pi=pi, j=j: e.matmul(PS[pi][:, :], lhsT=win_b[:, kc, C_NK + j * 128:C_NK + (j + 1) * 128], rhs=hb[:, kc, :],
                                                                     start=(kc == 0), stop=(kc == 7)) for kc in range(8)],
                            reads=hks + [wkey], writes=["ps%d" % pi])
                    if j % 2 == 0:
                        T.op("act", lambda e, pi=pi, j=j: e.activation(out=KN[:, j, nsl], in_=PS[pi][:, ncol], func=AF.Copy), reads=["ps%d" % pi], writes=["KN"])
                    else:
                        T.op("dve", lambda e, pi=pi, j=j: e.tensor_copy(out=KN[:, j, nsl], in_=PS[pi][:, ncol]), reads=["ps%d" % pi], writes=["KN"])
            def p2():
                if not (full and s < 6):
                    return
                for tt in range(4 if s < 5 else 2):
                    pi = psn(0, 7); t = 4 * s + tt
                    T.group("pe", [lambda e, kc=kc, pi=pi, tt=tt: e.matmul(PS[pi][:, :], lhsT=hb[:, kc, tt * 128:(tt + 1) * 128], rhs=win_b[:, kc, C_NV:C_NV + 512],
                                                                       start=(kc == 0), stop=(kc == 7)) for kc in range(8)],
                            reads=hks + [wkey], writes=["ps%d" % pi])
                    src = PS[pi][:, :].rearrange("p (h d) -> p h d", h=8)
                    if tt % 2 == 0:
                        T.op("dve", lambda e, src=src, t=t: e.tensor_copy(out=VN[:, t, :, 0:64], in_=src), reads=["ps%d" % pi], writes=["VN"])
                    else:
                        T.op("act", lambda e, src=src, t=t: e.activation(out=VN[:, t, :, 0:64], in_=src, func=AF.Copy), reads=["ps%d" % pi], writes=["VN"])
            def p3():
                if not (full and s < 4):
                    return
                pl = []
                for c in range(3):
                    pi = psn(0, 7); pl.append(pi)
                    T.group("pe", [lambda e, kc=kc, pi=pi, c=c: e.matmul(PS[pi][:, :], lhsT=win_b[:, kc, c * 128:(c + 1) * 128], rhs=hb[:, kc, :],
                                                                     start=(kc == 0), stop=(kc == 7)) for kc in range(8)],
                            reads=hks + [wkey], writes=["ps%d" % pi])
                rms_feat(pl, 384, gqc, "gqc", lambda c: qlatn[:, c, sl], "qlatn")
                for j in range(4):
                    pi = psn(0, 7)
                    T.group("pe", [lambda e, kc=kc, pi=pi, j=j: e.matmul(PS[pi][:, :], lhsT=win_b[:, kc, 384 + j * 128:384 + (j + 1) * 128], rhs=hb[:, kc, :],
                                                                     start=(kc == 0), stop=(kc == 7)) for kc in range(8)],
                            reads=hks + [wkey], writes=["ps%d" % pi])
                    T.op("act", lambda e, pi=pi, j=j: e.activation(out=QN[:, j, sl], in_=PS[pi][:, :], func=AF.Copy, scale=0.125), reads=["ps%d" % pi], writes=["QN"])
            return [p0, p1, p2, p3]

        s_list = list(s_list)
        seq = [(s, tt) for s in s_list for tt in range(4)]
        depth = len(xt) - 1
        for k0 in range(min(depth, len(seq))):
            nt_dma(*seq[k0])
        nt_a(*seq[0])
        prev_parts = None
        for k, (s, tt) in enumerate(seq):
            if k + depth < len(seq):
                nt_dma(*seq[k + depth])
            if k + 1 < len(seq):
                nt_a(*seq[k + 1])
            nt_b(s, tt)
            if prev_parts is not None:
                prev_parts[tt]()
            if tt == 3:
                prev_parts = mm_parts(s)
        for p_ in prev_parts:
            p_()

    PA1 = phaseA(None, ATR, True, None, load_only=True)
    ccol = sbat("ccol", [128, 2, 8], F32, A0)
    scol = sbat("scol", [128, 2, 8], BF16, A0 + 64)
    wmb = [sbat("wmb%d" % i, [128, 8, 512], BF16, A0 + 128 + i * 8192) for i in range(2)]
    brow = sbat("brow", [1, 512], F32, A0 + 128 + 16384)
    grow = sbat("grow", [1, 512], F32, A0 + 128 + 16384 + 2048)
    mrow = [sbat("mrow%d" % i, [1, 512], F32, A0 + 128 + 16384 + 4096 + i * 2048) for i in range(2)]
    with nc.allow_non_contiguous_dma(reason="tiny column loads"):
        T.dma("sp", ccol[:], cvec.rearrange("r (kc p) -> p r kc", p=128), writes=["ccol"])
    T.op("act", lambda e: e.activation(out=scol[:], in_=ccol[:], func=AF.Silu), reads=["ccol"], writes=["scol"])
    PCOL = 6
    for j in range(12):
        v, hf = j // 2, j % 2
        wb = wmb[j % 2]; wk = "wmb%d" % (j % 2)
        T.dma("pool", wb[:], w_mod[:, j * 512:(j + 1) * 512].rearrange("(kc p) c -> p kc c", p=128), writes=[wk])
        T.dma("sp", brow[:], b_mod[None, j * 512:(j + 1) * 512], writes=["brow"])
        if v in (1, 4):
            gsrc = g_attn if v == 1 else g_ffn
            T.dma("sp", grow[:], gsrc[None, hf * 512:(hf + 1) * 512], writes=["grow"])
        for r in range(2):
            if r == 1 and v > 1:
                continue
            pi = psn(0, 6)
            T.group("pe", [lambda e, kc=kc, pi=pi, r=r, wb=wb: e.matmul(PS[pi][0:1, :], lhsT=scol[:, r, kc:kc + 1], rhs=wb[:, kc, :],
                                                                    start=(kc == 0), stop=(kc == 7)) for kc in range(8)],
                    reads=["scol", wk], writes=["ps%d" % pi])
            mr = mrow[r]; mk = "mrow%d" % r
            T.op("dve", lambda e, pi=pi, mr=mr: e.tensor_tensor(out=mr[:], in0=PS[pi][0:1, :], in1=brow[:], op=ALU.add),
                 reads=["ps%d" % pi, "brow"], writes=[mk])
            if v in (1, 4):
                T.op("dve", lambda e, mr=mr: e.scalar_tensor_tensor(out=mr[:], in0=mr[:], scalar=1.0, in1=grow[:], op0=ALU.add, op1=ALU.mult),
                     reads=[mk, "grow"], writes=[mk])
            if v in (2, 5):
                gi = 0 if v == 2 else 1
                T.dma("sp", gscr[gi:gi + 1, hf * 512:(hf + 1) * 512], mr[:], reads=[mk], writes=["gscr_r"], key="gscr")
            else:
                if r == 0:
                    base = {1: 0, 0: 8, 4: 16, 3: 24}[v]
                else:
                    base = {1: 32, 0: 40}[v]
                T.group("pe", [lambda e, i=i, mr=mr, base=base, hf=hf: e.matmul(PS[PCOL][:, base + hf * 4 + i: base + hf * 4 + i + 1],
                                                                             lhsT=mr[0:1, i * 128:(i + 1) * 128], rhs=onesf[0:1, 0:1],
                                                                             start=True, stop=True) for i in range(4)],
                        reads=[mk, "onesf"], writes=["pscol"])
    T.op("dve", lambda e: e.tensor_copy(out=modc[:, 0:48], in_=PS[PCOL][:, 0:48]), reads=["pscol"], writes=["modc"])
    if stop_after == "adaln":
        dbg_out["modc"] = (modc, [128, 64], F32)
        return finish(nc, T, dbg_out, out)
    T.barrier()

    T.op("pool", lambda e: e.memset(VN[:, :, :, 64:65], 1.0), writes=["VN"])
    rv = phaseA(range(6), ATR, True, lambda s: [(krot_tmp[64:96, s * 512:(s + 1) * 512], "krot_tmp")], state=PA1)
    if rv == "stop":
        return finish(nc, T, dbg_out, out)
    if stop_after == "phaseA":
        dbg_out["kvn"] = (kvn, [128, 2, NSLOT], BF16); dbg_out["qlatn"] = (qlatn, [128, 3, 2048], BF16)
        dbg_out["KN"] = (KN, [128, 4, 2816], BF16); dbg_out["QN"] = (QN, [128, 4, 2048], BF16)
        dbg_out["VN"] = (VN, [128, 22, 8, 65], BF16); dbg_out["krot_tmp"] = (krot_tmp, [128, 3072], BF16)
        return finish(nc, T, dbg_out, out)
    T.barrier()

    nrm = [0]
    pending = []
    def run_pending(i, flush=False):
        for item in [x for x in pending if flush or x[0] <= i]:
            pending.remove(item)
            item[1]()
    def normalize_out(po, h_at, qsl, i, scr=3, off=0, act_evac=False):
        ob = osb[nrm[0] % 2]; ok = "osb%d" % (nrm[0] % 2); onb = onbs[nrm[0] % 2]; onk = "onb%d" % (nrm[0] % 2); nrm[0] += 1
        if act_evac:
            T.op("act", lambda e: e.activation(out=ob[0:65, :], in_=PS[po][0:65, :], func=AF.Copy), reads=["ps%d" % po], writes=[ok])
        else:
            T.op("dve", lambda e: e.tensor_copy(out=ob[0:65, :], in_=PS[po][0:65, :]), reads=["ps%d" % po], writes=[ok])
        for q4 in range(4):
            pending.append([i + 1 + off + q4, lambda q4=q4: T.op("dve", lambda e: e.reciprocal(out=ob[64:65, q4 * 128:(q4 + 1) * 128], in_=ob[64:65, q4 * 128:(q4 + 1) * 128]),
                                                             reads=[ok], writes=[ok])])
        def st3():
            T.group("pe", [lambda e: e.matmul(PS[scr][:, :], lhsT=shiftm[:], rhs=onb[:], start=True, stop=True)], reads=[onk, "shiftm"], writes=["ps%d" % scr])
            T.op("dve", lambda e: e.tensor_copy(out=AT[64:128, h_at // 2, qsl], in_=PS[scr][64:128, :]), reads=["ps%d" % scr], writes=["AT"])
        def st2():
            T.group("pe", [lambda e: e.matmul(PS[scr][0:64, :], lhsT=onesf[64:65, 0:64], rhs=ob[64:65, :], start=True, stop=True)],
                    reads=[ok, "onesf"], writes=["ps%d" % scr])
            if h_at % 2 == 0:
                T.op("dve", lambda e: e.tensor_tensor(out=AT[0:64, h_at // 2, qsl], in0=ob[0:64, :], in1=PS[scr][0:64, :], op=ALU.mult),
                     reads=[ok, "ps%d" % scr], writes=["AT"])
            else:
                T.op("dve", lambda e: e.tensor_tensor(out=onb[:], in0=ob[0:64, :], in1=PS[scr][0:64, :], op=ALU.mult),
                     reads=[ok, "ps%d" % scr], writes=[onk])
                pending.append([i + 9 + off, st3])
        pending.append([i + 6 + off, st2])

    cur[0] = ATT_W
    nabt = [sb("nabt%d" % i, [128, 8, 512], BF16) for i in range(2)]
    assert cur[0] <= 229344
    stg = [sbat("stg%d" % i, [128, 1024], F32, TMP + 8192 + i * 4096) for i in range(2)]
    combt = [sbat("combt%d" % i, [128, 8, 512], BF16, ATR + i * 8192) for i in range(2)]
    na_steps = [(h, g, p) for h in range(8) for g in range(4) for p in range(5)]
    pti = [0]
    na_pt = {}
    def na_load(h):
        T.dma("pool", nabt[h % 2][:], nab[h].rearrange("c p f -> p c f"), writes=["nabt%d" % (h % 2)])
    def na_comb(h, g):
        k = (h * 4 + g) % 2
        T.op("pool", lambda e: e.tensor_tensor(out=combt[k][:].rearrange("p c (i q) -> p c i q", q=64),
                                               in0=nabt[h % 2][:].rearrange("p c (i q) -> p c i q", q=64),
                                               in1=rmc[:, g, :].rearrange("p (c i) -> p c i", i=8).unsqueeze(3).to_broadcast([128, 8, 8, 64]), op=ALU.add),
             reads=["nabt%d" % (h % 2), "rmc"], writes=["combt%d" % k])
    qz = [[sb("qz%d_%d" % (par, k_), [128, 512], BF16) for k_ in range(2)] for par in range(2)]
    assert cur[0] <= 229344
    for par in range(2):
        for k_ in range(2):
            T.op("pool", lambda e, par=par, k_=k_: e.memset(qz[par][k_][:], 0.0), writes=["qz%d_%d" % (par, k_)])
    def na_qz(h, g):
        par = h % 2; k_ = (h * 4 + g) // 1 % 2
        pb_ = par * 64
        T.op("pool", lambda e: e.tensor_copy(out=qz[par][k_][pb_:pb_ + 64, :], in_=QN[pb_:pb_ + 64, h // 2, g * 512:(g + 1) * 512]),
             reads=["QN"], writes=["qz%d_%d" % (par, k_)])
    na_load(0); na_load(1); na_comb(0, 0); na_qz(0, 0)
    def na_qk(i):
        h, g, p = na_steps[i]
        j = h // 2; pb = (h % 2) * 64
        if p == 0:
            nh, ng = (h, g + 1) if g < 3 else (h + 1, 0)
            if nh < 8:
                if ng == 0 and nh + 1 < 8 and False:
                    pass
                na_comb(nh, ng)
                na_qz(nh, ng)
        if p == 4 and g == 3 and h + 2 < 8:
            na_load(h + 2)
        qsl = slice(g * 512, (g + 1) * 512)
        pp = i % 2
        for half in range(2):
            c = 2 * p + half
            ks = na_row_slot(8 * g + 2 * c) if c < 8 else 2560 + (c - 8) * 128
            dst = PP[pp][:, half * 512:(half + 1) * 512]
            qzb = qz[h % 2][(h * 4 + g) % 2]
            T.group("pe", [lambda e, ks=ks, dst=dst: e.matmul(dst, lhsT=KN[:, j, ks:ks + 128], rhs=qzb[:, :], start=True, stop=True)],
                    reads=["KN", "qz%d_%d" % (h % 2, (h * 4 + g) % 2)], writes=["ps%d" % (4 + 2 * pp + half)])
        k = pti[0] % 4; pti[0] += 1
        na_pt[i] = k
        pk_ = ["ps%d" % (4 + 2 * pp), "ps%d" % (5 + 2 * pp)]
        if p < 4:
            ck = (h * 4 + g) % 2
            sg_ = stg[i % 2]; sgk = "stg%d" % (i % 2)
            T.op("dve", lambda e: e.tensor_tensor(out=sg_[:, :], in0=PP[pp][:, :], in1=combt[ck][:, 2 * p:2 * p + 2, :].rearrange("p c f -> p (c f)"), op=ALU.add),
                 reads=pk_ + ["combt%d" % ck], writes=[sgk])
            T.op("act", lambda e: e.activation(out=PT2[k][:, :], in_=sg_[:, :], func=AF.Exp), reads=[sgk], writes=["PT%d" % k])
        else:
            T.op("act", lambda e: e.activation(out=PT2[k][:, :], in_=PP[pp][:, :], func=AF.Exp), reads=pk_, writes=["PT%d" % k])
    def na_pv(i):
        h, g, p = na_steps[i]
        k = na_pt.pop(i)
        po = (h * 4 + g) % 2
        fns = []
        for half in range(2):
            c = 2 * p + half
            ks = na_row_slot(8 * g + 2 * c) if c < 8 else 2560 + (c - 8) * 128
            fns.append(lambda e, ks=ks, half=half, c=c: e.matmul(PS[po][0:65, :], lhsT=VN[:, ks // 128, h, 0:65], rhs=PT2[k][:, half * 512:(half + 1) * 512],
                                                               start=(c == 0), stop=(c == 9)))
        T.group("pe", fns, reads=["VN", "PT%d" % k], writes=["ps%d" % po])
        if p == 4:
            normalize_out(po, 8 + h, slice(g * 512, (g + 1) * 512), i, act_evac=True)
    LA = 3
    for i in range(len(na_steps) + LA):
        if i < len(na_steps):
            na_qk(i)
        if i >= LA:
            na_pv(i - LA)
        run_pending(i - LA)
    for _ in range(8):
        run_pending(0, flush=True)
    if stop_after == "na":
        dbg_out["AT"] = (AT, [128, 8, 2048], BF16)
        return finish(nc, T, dbg_out, out)
    T.barrier()

    for i in range(2):
        T.op("pool", lambda e, i=i: e.tensor_copy(out=KH[i][64:96, 0:3072], in_=krot_tmp[64:96, :]), reads=["krot_tmp"], writes=["KHr%d" % i])
    phaseA(range(6, NS), TAIL, False, lambda s: [(KH[i][64:96, s * 512:(s + 1) * 512], "KHr%d" % i) for i in range(2)], xn_base=ATR)
    T.barrier()

    cur[0] = ATT_W
    wuq_b = sb("wuq_b", [128, 3, 768], BF16)
    wuqs_b = sb("wuqs_b", [128, 3, 8, 96], BF16)
    wukv_b = sb("wukv_b", [128, 2, 1024], BF16)
    ctq = sbat("ctq", [128, 512], F32, TMP + 8192); stq = sbat("stq", [128, 512], F32, TMP + 10240)
    r1q = sbat("r1q", [128, 512], F32, TMP + 12288); r2q = sbat("r2q", [128, 512], F32, TMP + 14336)
    assert cur[0] <= 229344, cur[0]
    T.dma("pool", wuq_b[:], w_uq.rearrange("(kc p) c -> p kc c", p=128), writes=["wuq_b"])
    T.op("pool", lambda e: e.memset(wuqs_b[:], 0.0), writes=["wuqs_b"])
    for kc in range(3):
        T.dma("pool", wuqs_b[:, kc, :, 64:96], w_uqs[kc * 128:(kc + 1) * 128, :, :], writes=["wuqs_b"])
    T.op("pool", lambda e: e.tensor_scalar(out=wuqs_b[:, :, :, 64:80], in0=wuqs_b[:, :, :, 64:80], scalar1=-1.0, scalar2=None, op0=ALU.mult),
         reads=["wuqs_b"], writes=["wuqs_b"])
    T.dma("pool", wukv_b[:], w_ukv.rearrange("(kc p) c -> p kc c", p=128), writes=["wukv_b"])
    for i in range(2):
        T.op("pool", lambda e, i=i: e.memset(VH[i][:, :, 64:65], 1.0), writes=["VH%d" % i])
    SCALE = 96 ** -0.5

    def prep_units(h):
        b = h % 2
        units = []
        pbk = [0]
        def nb_():
            pbk[0] += 1
            return idle_set[0][pbk[0] % 2]
        for s in range(NS):
            def ku(s=s):
                PREP = nb_()
                T.group("pe", [lambda e, kc=kc: e.matmul(PS[PREP][0:64, :], lhsT=wukv_b[:, kc, h * 128:h * 128 + 64], rhs=kvn[:, kc, s * 512:(s + 1) * 512],
                                                         start=(kc == 0), stop=(kc == 1)) for kc in range(2)], reads=["kvn", "wukv_b"], writes=["ps%d" % PREP])
                T.op("dve", lambda e: e.tensor_copy(out=KH[b][0:64, s * 512:(s + 1) * 512], in_=PS[PREP][0:64, :]), reads=["ps%d" % PREP], writes=["KH%d" % b])
            units.append(ku)
            def vu(s=s):
                PREP = nb_()
                fns = []
                for tt in range(4):
                    for kc in range(2):
                        fns.append(lambda e, tt=tt, kc=kc: e.matmul(PS[PREP][:, tt * 64:(tt + 1) * 64], lhsT=kvn[:, kc, (4 * s + tt) * 128:(4 * s + tt + 1) * 128],
                                                                    rhs=wukv_b[:, kc, h * 128 + 64:h * 128 + 128], start=(kc == 0), stop=(kc == 1)))
                T.group("pe", fns, reads=["kvn", "wukv_b"], writes=["ps%d" % PREP])
                T.op("dve", lambda e: e.tensor_copy(out=VH[b][:, 4 * s:4 * s + 4, 0:64], in_=PS[PREP][:, 0:256].rearrange("p (t d) -> p t d", t=4)),
                     reads=["ps%d" % PREP], writes=["VH%d" % b])
            units.append(vu)
        for qc in range(4):
            def qu(qc=qc):
                PA_, PB_ = idle_set[0]
                qsl = slice(qc * 512, (qc + 1) * 512)
                T.dma("sp", ctq[64:96, :], ctab[:, qsl], writes=["ctq"])
                T.dma("sp", stq[64:96, :], stab[:, qsl], writes=["stq"])
                T.group("pe", [lambda e, kc=kc: e.matmul(PS[PA_][0:96, :], lhsT=wuq_b[:, kc, h * 96:(h + 1) * 96], rhs=qlatn[:, kc, qsl],
                                                         start=(kc == 0), stop=(kc == 2)) for kc in range(3)], reads=["qlatn", "wuq_b"], writes=["ps%d" % PA_])
                T.group("pe", [lambda e, kc=kc: e.matmul(PS[PB_][0:96, :], lhsT=wuqs_b[:, kc, h, :], rhs=qlatn[:, kc, qsl],
                                                         start=(kc == 0), stop=(kc == 2)) for kc in range(3)], reads=["qlatn", "wuqs_b"], writes=["ps%d" % PB_])
                T.op("dve", lambda e: e.tensor_copy(out=QH[b][0:64, qsl], in_=PS[PA_][0:64, :]), reads=["ps%d" % PA_], writes=["QH%d" % b])
                T.op("dve", lambda e: e.tensor_tensor(out=r1q[64:96, :], in0=PS[PA_][64:96, :], in1=ctq[64:96, :], op=ALU.mult), reads=["ps%d" % PA_, "ctq"], writes=["r1q"])
                T.op("dve", lambda e: e.tensor_tensor(out=r2q[64:96, :], in0=PS[PB_][64:96, :], in1=stq[64:96, :], op=ALU.mult), reads=["ps%d" % PB_, "stq"], writes=["r2q"])
                T.op("pool", lambda e: e.tensor_tensor(out=QH[b][64:96, qsl], in0=r1q[64:96, :], in1=r2q[64:96, :], op=ALU.add), reads=["r1q", "r2q"], writes=["QH%d" % b])
            units.append(qu)
        return units

    idle_set = [(2, 3)]
    for u in prep_units(0):
        u()
    gstep = [0]
    for h in range(8):
        b = h % 2
        nxt = prep_units(h + 1) if h < 7 else []
        ui = [0]
        steps = [(qp, kc) for qp in range(2) for kc in range(NT_MLA)]
        n = len(steps)
        ptm = {}
        def m_qk(i):
            qp, kc = steps[i]; pp = i % 2
            for half in range(2):
                qc = 2 * qp + half
                dst = PP[pp][:, half * 512:(half + 1) * 512]
                T.group("pe", [lambda e, dst=dst, kc=kc, qc=qc: e.matmul(dst, lhsT=KH[b][0:96, kc * 128:(kc + 1) * 128], rhs=QH[b][0:96, qc * 512:(qc + 1) * 512],
                                                                     start=True, stop=True)], reads=["KH%d" % b, "KHr%d" % b, "QH%d" % b], writes=["ps%d" % (4 + 2 * pp + half)])
            k = pti[0] % 3; pti[0] += 1
            ptm[i] = k
            T.op("act", lambda e: e.activation(out=PT2[k][:, :], in_=PP[pp][:, :], func=AF.Exp, bias=kmask[:, kc:kc + 1], scale=SCALE),
                 reads=["ps%d" % (4 + 2 * pp), "ps%d" % (5 + 2 * pp), "kmask"], writes=["PT%d" % k])
        def m_pv(i):
            qp, kc = steps[i]
            k = ptm.pop(i)
            ob_ = (0, 1) if qp == 0 else (2, 3)
            if kc == 0:
                idle_set[0] = (2, 3) if qp == 0 else (0, 1)
            for half in range(2):
                T.group("pe", [lambda e, half=half: e.matmul(PS[ob_[half]][0:65, :], lhsT=VH[b][:, kc, 0:65], rhs=PT2[k][:, half * 512:(half + 1) * 512],
                                                           start=(kc == 0), stop=(kc == NT_MLA - 1))], reads=["VH%d" % b, "PT%d" % k], writes=["ps%d" % ob_[half]])
            if kc == NT_MLA - 1:
                for half in range(2):
                    qc = 2 * qp + half
                    normalize_out(ob_[half], h, slice(qc * 512, (qc + 1) * 512), gstep[0], scr=ob_[half], off=4 * half)
            if nxt:
                want = min(len(nxt), (len(nxt) * (i + 1)) // (n - 16))
                while ui[0] < want:
                    nxt[ui[0]](); ui[0] += 1
        for i in range(n + 2):
            if i < n:
                m_qk(i)
            if i >= 2:
                gstep[0] += 1
                m_pv(i - 2)
                run_pending(gstep[0])
    for _ in range(8):
        run_pending(0, flush=True)
    if stop_after == "mla":
        dbg_out["AT"] = (AT, [128, 8, 2048], BF16)
        return finish(nc, T, dbg_out, out)
    T.barrier()

    T.op("pool", lambda e: e.memset(ss[:], 0.0), writes=["ss_all"])
    T.barrier()
    xnew = sbat("xnew", [128, 16, D], F32, A0)
    wout_b = sbat("wout_b", [128, 8, D], BF16, 117888)
    g1b = sbat("g1b", [128, D], F32, 134272)
    hx2T = sbat("hx2T", [128, 8, 2048], BF16, TAIL)
    xr = [sbat("xr%d" % i, [128, D], F32, TMP + i * 4096) for i in range(4)]
    xn2 = [sbat("xn2_%d" % i, [128, D], BF16, TMP + 16384 + i * 2048) for i in range(2)]
    T.dma("pool", wout_b[:], w_out.rearrange("(kc p) c -> p kc c", p=128), writes=["wout_b"])
    T.dma("sp", g1b[:], gscr[0:1, :].to_broadcast([128, D]), reads=["gscr_r"], writes=["g1b"], key="g1b")
    T.op("pool", lambda e: e.tensor_tensor(out=wout_b[:], in0=wout_b[:], in1=g1b[:].unsqueeze(1).to_broadcast([128, 8, D]), op=ALU.mult),
         reads=["wout_b", "g1b"], writes=["wout_b"])
    def wo_dma(t):
        T.dma("sp", xr[t % 4][:], xs[t * 128:(t + 1) * 128, :], writes=["xr%d" % (t % 4)])
    def wo_a(t):
        xb = xr[t % 4]; xk = "xr%d" % (t % 4)
        for hf in range(2):
            pi = psn(0, 7)
            T.group("pe", [lambda e, kc=kc, pi=pi, hf=hf: e.matmul(PS[pi][:, :], lhsT=AT[:, kc, t * 128:(t + 1) * 128], rhs=wout_b[:, kc, hf * 512:(hf + 1) * 512],
                                                               start=(kc == 0), stop=(kc == 7)) for kc in range(8)], reads=["AT", "wout_b"], writes=["ps%d" % pi])
            T.op("dve", lambda e, pi=pi, hf=hf: e.tensor_tensor(out=xnew[:, t, hf * 512:(hf + 1) * 512], in0=PS[pi][:, :], in1=xb[:, hf * 512:(hf + 1) * 512], op=ALU.add),
                 reads=["ps%d" % pi, xk], writes=["xnew%d" % t])
    def wo_b(t):
        norm_a(t, xnew[:, t, :], "xnew%d" % t, xn2[t % 2], "xn2_%d" % (t % 2), "rstd2_%d" % t)
    def wo_c(t):
        norm_b(xn2[t % 2], "xn2_%d" % (t % 2), lambda kc, t=t: hx2T[:, kc, t * 128:(t + 1) * 128], ["hx2T"] * 8, 16)
    for t in range(3):
        wo_dma(t)
    for k in range(16 + 2):
        if k + 3 < 16:
            wo_dma(k + 3)
        if k < 16:
            wo_a(k)
        if 1 <= k < 17:
            wo_b(k - 1)
        if k >= 2:
            wo_c(k - 2)
    if stop_after == "wout":
        dbg_out["xnew"] = (xnew, [128, 16, D], F32)
        return finish(nc, T, dbg_out, out)
    T.barrier()

    WB = 117888
    wexp = [[sbat("wg%d" % i, [128, 8, 512], BF16, WB + i * 24576),
             sbat("wu%d" % i, [128, 8, 512], BF16, WB + i * 24576 + 8192),
             sbat("wd%d" % i, [128, 4, D], BF16, WB + i * 24576 + 16384)] for i in range(2)]
    hidT = [sbat("hidT%d" % i, [128, 4, 512], BF16, WB + 49152 + i * 4096) for i in range(2)]
    g2b = sbat("g2b", [128, D], F32, WB + 57344)
    gfb = sbat("gfb", [128, D], F32, WB + 61440)
    assert WB + 65536 <= TAIL
    cur[0] = TMP
    wr_b = sb("wr_b", [128, 8, 20], BF16)
    brb = sb("brb", [128, 20], F32)
    lg = sb("lg", [128, 16, 20], F32)
    comb = sb("comb", [128, 16, 16], F32)
    rt = {n_: sb("rt_" + n_, [128, 16, 4], F32) for n_ in ("GE", "OH", "EL", "MK", "SEL", "EX", "TM")}
    rv = {n_: sb("rv_" + n_, [128, 16], F32) for n_ in ("GM", "SG", "GW", "M1", "M2", "SE", "RS", "WS")}
    sg = sb("sg", [128, 4, 512], BF16)
    ofin = [sb("ofin%d" % i, [128, D], F32) for i in range(2)]
    assert cur[0] <= A0, cur[0]
    T.dma("pool", wr_b[:], w_r.rearrange("(kc p) c -> p kc c", p=128), writes=["wr_b"])
    T.dma("sp", brb[:], b_r[None, :].to_broadcast([128, 20]), writes=["brb"])
    T.dma("sp", g2b[:], gscr[1:2, :].to_broadcast([128, D]), reads=["gscr_r"], writes=["g2b"], key="g2b")
    T.dma("sp", gfb[:], g_fin[None, :].to_broadcast([128, D]), writes=["gfb"])
    pr = psn(0, 4)
    for t in range(16):
        T.group("pe", [lambda e, kc=kc, t=t: e.matmul(PS[pr][:, t * 20:(t + 1) * 20], lhsT=hx2T[:, kc, t * 128:(t + 1) * 128], rhs=wr_b[:, kc, :], start=(kc == 0), stop=(kc == 7))
                       for kc in range(8)], reads=["hx2T", "wr_b"], writes=["ps%d" % pr])
    AXX = mybir.AxisListType.X
    def V(fn, rd, wr):
        T.op("dve", fn, reads=rd, writes=wr)
    def B4(ap):
        return ap.unsqueeze(2).to_broadcast([128, 16, 4])
    GL = lg[:, :, 0:4]
    V(lambda e: e.tensor_tensor(out=lg[:], in0=PS[pr][:, 0:320].rearrange("p (t c) -> p t c", c=20), in1=brb[:].unsqueeze(1).to_broadcast([128, 16, 20]), op=ALU.add),
      ["ps%d" % pr, "brb"], ["lg"])
    V(lambda e: e.reduce_max(out=rv["GM"][:], in_=GL, axis=AXX), ["lg"], ["GM"])
    V(lambda e: e.tensor_tensor(out=rt["GE"][:], in0=GL, in1=B4(rv["GM"][:]), op=ALU.subtract), ["lg", "GM"], ["GE"])
    T.op("act", lambda e: e.activation(out=rt["GE"][:], in_=rt["GE"][:], func=AF.Exp), reads=["GE"], writes=["GE"])
    V(lambda e: e.reduce_sum(out=rv["SG"][:], in_=rt["GE"][:], axis=AXX), ["GE"], ["SG"])
    V(lambda e: e.reciprocal(out=rv["GW"][:], in_=rv["SG"][:]), ["SG"], ["GW"])
    V(lambda e: e.tensor_tensor(out=rt["OH"][:], in0=GL, in1=B4(rv["GM"][:]), op=ALU.is_equal), ["lg", "GM"], ["OH"])
    for g in range(4):
        dst = rt["EL"] if g == 0 else rt["TM"]
        V(lambda e, g=g, dst=dst: e.tensor_tensor(out=dst[:], in0=lg[:, :, 4 + 4 * g:8 + 4 * g], in1=rt["OH"][:, :, g:g + 1].to_broadcast([128, 16, 4]), op=ALU.mult),
          ["lg", "OH"], ["EL" if g == 0 else "TM"])
        if g > 0:
            V(lambda e: e.tensor_tensor(out=rt["EL"][:], in0=rt["EL"][:], in1=rt["TM"][:], op=ALU.add), ["EL", "TM"], ["EL"])
    V(lambda e: e.reduce_max(out=rv["M1"][:], in_=rt["EL"][:], axis=AXX), ["EL"], ["M1"])
    V(lambda e: e.tensor_tensor(out=rt["MK"][:], in0=rt["EL"][:], in1=B4(rv["M1"][:]), op=ALU.is_equal), ["EL", "M1"], ["MK"])
    V(lambda e: e.scalar_tensor_tensor(out=rt["MK"][:], in0=rt["MK"][:], scalar=NEG, in1=rt["EL"][:], op0=ALU.mult, op1=ALU.add), ["MK", "EL"], ["MK"])
    V(lambda e: e.reduce_max(out=rv["M2"][:], in_=rt["MK"][:], axis=AXX), ["MK"], ["M2"])
    V(lambda e: e.tensor_tensor(out=rt["SEL"][:], in0=rt["EL"][:], in1=B4(rv["M2"][:]), op=ALU.is_ge), ["EL", "M2"], ["SEL"])
    V(lambda e: e.tensor_tensor(out=rt["EX"][:], in0=rt["EL"][:], in1=B4(rv["M1"][:]), op=ALU.subtract), ["EL", "M1"], ["EX"])
    T.op("act", lambda e: e.activation(out=rt["EX"][:], in_=rt["EX"][:], func=AF.Exp), reads=["EX"], writes=["EX"])
    V(lambda e: e.tensor_tensor(out=rt["EX"][:], in0=rt["EX"][:], in1=rt["SEL"][:], op=ALU.mult), ["EX", "SEL"], ["EX"])
    V(lambda e: e.reduce_sum(out=rv["SE"][:], in_=rt["EX"][:], axis=AXX), ["EX"], ["SE"])
    V(lambda e: e.reciprocal(out=rv["RS"][:], in_=rv["SE"][:]), ["SE"], ["RS"])
    V(lambda e: e.tensor_tensor(out=rv["WS"][:], in0=rv["RS"][:], in1=rv["GW"][:], op=ALU.mult), ["RS", "GW"], ["WS"])
    V(lambda e: e.tensor_tensor(out=rt["EX"][:], in0=rt["EX"][:], in1=B4(rv["WS"][:]), op=ALU.mult), ["EX", "WS"], ["EX"])
    for g in range(4):
        V(lambda e, g=g: e.tensor_tensor(out=comb[:, :, 4 * g:4 * g + 4], in0=rt["EX"][:], in1=rt["OH"][:, :, g:g + 1].to_broadcast([128, 16, 4]), op=ALU.mult),
          ["EX", "OH"], ["comb"])

    msteps = [(ex, s_) for ex in range(16) for s_ in range(4)]
    def moe_gu(i):
        ex, s_ = msteps[i]
        wg, wu, wd = wexp[ex % 2]; wk = "wexp%d" % (ex % 2)
        if s_ == 0:
            T.dma("pool", wg[:], w_gate[ex].rearrange("(kc p) f -> p kc f", p=128), writes=[wk + "g"])
            T.dma("pool", wu[:], w_up[ex].rearrange("(kc p) f -> p kc f", p=128), writes=[wk + "u"])
            T.dma("pool", wd[:], w_down[ex].rearrange("(fc p) d -> p fc d", p=128), writes=[wk + "d"])
            T.op("pool", lambda e: e.tensor_tensor(out=wd[:], in0=wd[:], in1=g2b[:].unsqueeze(1).to_broadcast([128, 4, D]), op=ALU.mult),
                 reads=[wk + "d", "g2b"], writes=[wk + "d"])
        hT = hidT[i % 2]; hk2 = "hidT%d" % (i % 2)
        tsl = slice(s_ * 512, (s_ + 1) * 512)
        for fc in range(4):
            pg = psn(0, 4); pu = psn(0, 4)
            T.group("pe", [lambda e, kc=kc, pg=pg, fc=fc: e.matmul(PS[pg][:, :], lhsT=wg[:, kc, fc * 128:(fc + 1) * 128], rhs=hx2T[:, kc, tsl], start=(kc == 0), stop=(kc == 7))
                           for kc in range(8)], reads=["hx2T", wk + "g"], writes=["ps%d" % pg])
            T.group("pe", [lambda e, kc=kc, pu=pu, fc=fc: e.matmul(PS[pu][:, :], lhsT=wu[:, kc, fc * 128:(fc + 1) * 128], rhs=hx2T[:, kc, tsl], start=(kc == 0), stop=(kc == 7))
                           for kc in range(8)], reads=["hx2T", wk + "u"], writes=["ps%d" % pu])
            T.op("act", lambda e, pg=pg, fc=fc: e.activation(out=sg[:, fc, :], in_=PS[pg][:, :], func=AF.Silu), reads=["ps%d" % pg], writes=["sg%d" % fc])
            T.op("dve", lambda e, pu=pu, fc=fc, hT=hT: e.tensor_tensor(out=hT[:, fc, :], in0=PS[pu][:, :], in1=sg[:, fc, :], op=ALU.mult),
                 reads=["ps%d" % pu, "sg%d" % fc], writes=[hk2 + "_%d" % fc])
    def moe_down(i):
        ex, s_ = msteps[i]
        wg, wu, wd = wexp[ex % 2]; wk = "wexp%d" % (ex % 2)
        hT = hidT[i % 2]; hk2 = "hidT%d" % (i % 2)
        for tt in range(4):
            t = 4 * s_ + tt
            for hf in range(2):
                py = psn(4, 7)
                T.group("pe", [lambda e, fc=fc, py=py, tt=tt, hf=hf: e.matmul(PS[py][:, :], lhsT=hT[:, fc, tt * 128:(tt + 1) * 128], rhs=wd[:, fc, hf * 512:(hf + 1) * 512],
                                                                         start=(fc == 0), stop=(fc == 3)) for fc in range(4)],
                        reads=[hk2 + "_%d" % fc for fc in range(4)] + [wk + "d"], writes=["ps%d" % py])
                T.op("dve", lambda e, py=py, hf=hf, t=t: e.scalar_tensor_tensor(out=xnew[:, t, hf * 512:(hf + 1) * 512], in0=PS[py][:, :], scalar=comb[:, t, ex:ex + 1],
                                                                         in1=xnew[:, t, hf * 512:(hf + 1) * 512], op0=ALU.mult, op1=ALU.add),
                     reads=["ps%d" % py, "comb", "xnew%d" % t], writes=["xnew%d" % t])
    for i in range(len(msteps) + 1):
        if i < len(msteps):
            moe_gu(i)
        if i >= 1:
            moe_down(i - 1)

    for t in range(16):
        ob = ofin[t % 2]; ok = "ofin%d" % (t % 2)
        T.op("act", lambda e, ob=ob, t=t: e.activation(out=ob[:], in_=xnew[:, t, :], func=AF.Square, accum_out=ss[:, 32 + t:33 + t]), reads=["xnew%d" % t], writes=[ok, "ss3_%d" % t])
        T.op("act", lambda e, t=t: e.activation(out=sd[:, 32 + t:33 + t], in_=ss[:, 32 + t:33 + t], func=AF.Sqrt, bias=epsc[:], scale=1.0 / D), reads=["ss3_%d" % t, "epsc"], writes=["sd3_%d" % t])
        T.op("dve", lambda e, t=t: e.reciprocal(out=rstd[:, 32 + t:33 + t], in_=sd[:, 32 + t:33 + t]), reads=["sd3_%d" % t], writes=["rstd3_%d" % t])
        T.op("dve", lambda e, ob=ob, t=t: e.scalar_tensor_tensor(out=ob[:], in0=xnew[:, t, :], scalar=rstd[:, 32 + t:33 + t], in1=gfb[:], op0=ALU.mult, op1=ALU.mult),
             reads=["xnew%d" % t, "rstd3_%d" % t, "gfb"], writes=[ok])
        T.dma("sp", out[t * 128:(t + 1) * 128, :], ob[:], reads=[ok], key="outst%d" % (t % 2))
    return finish(nc, T, dbg_out, out)


def finish(nc, T, dbg_out, out):
    for name, (tens, shape, dt) in dbg_out.items():
        d = nc.dram_tensor("dbg_" + name, list(shape), dt, kind="ExternalOutput").ap()
        idx = tuple(slice(None) for _ in shape)
        T.dma("sp", d[idx], tens[idx], reads=[name], key="dbg_" + name)
    T.finish()
    return nc


GRID_W = 64
_NC_CACHE = {}


def _rope_tables(rows, cols):
    half = 16
    inv_freq = (10000.0 ** (-np.arange(0, half, 2, dtype=np.float32) / half)).astype(np.float32)
    ang = np.concatenate([rows.astype(np.float32)[:, None] * inv_freq, cols.astype(np.float32)[:, None] * inv_freq], axis=-1)
    return np.cos(ang).astype(np.float32), np.sin(ang).astype(np.float32)


def _core_layout(j):
    R0 = 32 * j
    tok = np.full(NSLOT, -1, np.int64)
    tok[0:2048] = np.arange(R0 * 64, (R0 + 32) * 64)
    used = np.zeros(8192, bool); used[R0 * 64:(R0 + 32) * 64] = True
    for i, r in enumerate(list(range(R0 - 4, R0)) + list(range(R0 + 32, R0 + 36))):
        if 0 <= r < 128:
            tok[2048 + i * 64:2048 + (i + 1) * 64] = np.arange(r * 64, (r + 1) * 64)
            used[r * 64:(r + 1) * 64] = True
    tok[2560:2816] = -2 - np.arange(256)
    others = np.nonzero(~used)[0]
    free_halo = np.nonzero(tok[2048:2560] == -1)[0] + 2048
    nfill = len(free_halo)
    if nfill:
        tok[free_halo] = others[len(others) - nfill:]
        others = others[:len(others) - nfill]
    assert len(others) == 5632, len(others)
    tok[2816:2816 + len(others)] = others
    assert (tok[:8448] != -1).all() and (tok[8448:] == -1).all()
    return tok


def _host_inputs(inp):
    x = np.asarray(inp["x"], np.float32); ctx = np.asarray(inp["ctx"], np.float32)
    w_in = np.ascontiguousarray(np.asarray(inp["w_in"], np.float32)[0])
    w_uq = np.ascontiguousarray(np.asarray(inp["w_uq"], np.float32)[0])
    perm = np.concatenate([np.arange(16, 32), np.arange(0, 16)])
    w_krs = np.ascontiguousarray(w_in[:, C_KR:C_KR + 32][:, perm])
    w_uqs = np.ascontiguousarray(w_uq.reshape(384, 8, 96)[:, :, 64:96][:, :, perm])
    rel = np.asarray(inp["na_rel_bias"], np.float32)[0]
    a = np.arange(2)[:, None, None, None]; ck = np.arange(64)[None, :, None, None]
    i = np.arange(8)[None, None, :, None]; cq = np.arange(64)[None, None, None, :]
    cstart = np.clip(cq - 8, 0, 48)
    col_in = (ck >= cstart) & (ck < cstart + 16)
    dc = ck - cq + 15
    nab = np.full((8, 8, 2, 64, 8, 64), NEG, np.float32)
    for c in range(8):
        dr = 2 * c + a - i + 3
        ok = (dr >= 0) & (dr <= 14) & col_in
        okb = np.broadcast_to(ok, (2, 64, 8, 64))
        drb = np.broadcast_to(np.clip(dr, 0, 14), (2, 64, 8, 64)); dcb = np.broadcast_to(np.clip(dc, 0, 30), (2, 64, 8, 64))
        for h in range(8):
            g = rel[h][drb, dcb]
            nab[h, c] = np.where(okb, g, np.float32(NEG))
    nab = np.ascontiguousarray(nab.reshape(8, 8, 128, 512))
    rowsel = np.zeros((16, 8, 2, 64), np.float32)
    for c in range(8):
        for aa in range(2):
            rowsel[2 * c + aa, c, aa, :] = 1.0
    rowsel = rowsel.reshape(16, 8, 128)
    common = dict(
        w_mod=np.ascontiguousarray(np.asarray(inp["w_mod"], np.float32)[0]), b_mod=np.ascontiguousarray(np.asarray(inp["b_mod"], np.float32)[0]),
        g_attn=np.ascontiguousarray(np.asarray(inp["norm_attn_g"], np.float32)[0]), g_ffn=np.ascontiguousarray(np.asarray(inp["norm_ffn_g"], np.float32)[0]),
        g_fin=np.asarray(inp["final_norm_g"], np.float32), w_in=w_in, w_krs=w_krs,
        gq=np.ascontiguousarray(np.asarray(inp["q_a_norm_g"], np.float32)[0]), gkv=np.ascontiguousarray(np.asarray(inp["kv_a_norm_g"], np.float32)[0]),
        w_uq=w_uq, w_uqs=w_uqs, w_ukv=np.ascontiguousarray(np.asarray(inp["w_ukv"], np.float32)[0]),
        w_out=np.ascontiguousarray(np.asarray(inp["w_out"], np.float32)[0]),
        w_r=np.ascontiguousarray(np.concatenate([np.asarray(inp["w_router_group"], np.float32)[0], np.asarray(inp["w_router_expert"], np.float32)[0]], axis=1)),
        b_r=np.ascontiguousarray(np.concatenate([np.asarray(inp["b_router_group"], np.float32)[0], np.asarray(inp["b_router_expert"], np.float32)[0]])),
        w_gate=np.ascontiguousarray(np.asarray(inp["w_gate"], np.float32)[0]), w_up=np.ascontiguousarray(np.asarray(inp["w_up"], np.float32)[0]),
        w_down=np.ascontiguousarray(np.asarray(inp["w_down"], np.float32)[0]),
        nab=nab,
    )
    maps = []
    for core in range(8):
        b, j = core // 4, core % 4
        R0 = 32 * j
        tok = _core_layout(j)
        xs = np.zeros((NSLOT, D), np.float32)
        m = tok >= 0
        xs[m] = x[b][tok[m]]
        xs[2560:2816] = ctx[b]
        kmask = np.where(tok == -1, np.float32(NEG), np.float32(0)).astype(np.float32).reshape(NT, 128).T.copy()
        rows = np.where(m, tok // GRID_W, 0); cols = np.where(m, tok % GRID_W, 0)
        cos, sin = _rope_tables(rows, cols)
        cos[~m] = 1.0; sin[~m] = 0.0
        ctab = np.ascontiguousarray(np.concatenate([cos, cos], axis=1).T)
        stab = np.ascontiguousarray(np.concatenate([sin, sin], axis=1).T)
        narm = np.full((16, 4, 8, 64), NEG, np.float32)
        for g in range(4):
            for i_ in range(8):
                r = R0 + 8 * g + i_
                start = min(max(r - 4, 0), 120)
                for lp in range(16):
                    kr = R0 - 4 + 8 * g + lp
                    if start <= kr < start + 8 and 0 <= kr < 128:
                        narm[lp, g, i_, :] = 0.0
        d = dict(common)
        RM = narm[:, :, :, 0]
        rmc = np.zeros((128, 4, 8, 8), np.float32)
        for c_ in range(8):
            for a_ in range(2):
                rmc[a_ * 64:(a_ + 1) * 64, :, c_, :] = RM[2 * c_ + a_][None, :, :]
        d.update(xs=xs, rmc=np.ascontiguousarray(rmc.reshape(128, 4, 64)), cvec=np.ascontiguousarray(np.stack([np.asarray(inp["c"], np.float32)[b], np.asarray(inp["c_ctx"], np.float32)])),
                 kmask=kmask, ctab=ctab, stab=stab)
        maps.append(d)
    return maps


def kernel(**inputs):
    if "nc" not in _NC_CACHE:
        _NC_CACHE["nc"] = build()
    nc = _NC_CACHE["nc"]
    maps = _host_inputs(inputs)
    res = run_bass_kernel_spmd(nc, maps, core_ids=list(range(8)))
    out = np.zeros((2, 8192, D), np.float32)
    for core in range(8):
        b, j = core // 4, core % 4
        out[b, j * 2048:(j + 1) * 2048] = res.results[core]["out"]
    return out
```

```python
import numpy as np
import concourse.bass as bass
import concourse.mybir as mybir
from concourse.bass_utils import run_bass_kernel_spmd

F32, BF16 = mybir.dt.float32, mybir.dt.bfloat16
AF = mybir.ActivationFunctionType
ALU = mybir.AluOpType

D = 1024
NT = 68
NT_MLA = 66
NS = 17
NSLOT = NT * 128
QC0, KV0 = 0, 896
C_KVLAT, C_KR, C_NK, C_NV = 896, 1152, 1184, 1696
EPS = 1e-6
import os
EVAC_MODE = int(os.environ.get('EVAC_MODE', '2'))
NEG = -1e30


class Trk:
    def __init__(self, nc):
        self.nc = nc
        self.eng = {}
        for name, h in (("pe", nc.tensor), ("act", nc.scalar), ("dve", nc.vector), ("pool", nc.gpsimd), ("sp", nc.sync)):
            self.eng[name] = dict(h=h, sem=nc.alloc_semaphore("s_" + name), cnt=0, seen={})
        self.res = {}
        self.dsem = {}

    def _r(self, k):
        if k not in self.res:
            self.res[k] = dict(w=None, r={})
        return self.res[k]

    def _waits(self, en, reads, writes):
        e = self.eng[en]
        need = {}
        def add(ev):
            if ev is None:
                return
            sem, val, src = ev
            if src == "pe" and en == "pe":
                return
            if need.get(sem.name, (None, 0))[1] < val:
                need[sem.name] = (sem, val)
        for k in reads:
            add(self._r(k)["w"])
        for k in writes:
            r = self._r(k)
            add(r["w"])
            for ev in r["r"].values():
                add(ev)
        for sn, (sem, val) in need.items():
            if e["seen"].get(sn, 0) < val:
                e["h"].wait_ge(sem, val)
                e["seen"][sn] = val

    def _mark(self, ev, reads, writes):
        for k in writes:
            r = self._r(k)
            r["w"] = ev
            r["r"] = {}
        for k in reads:
            r = self._r(k)
            r["r"][ev[0].name + ev[2]] = ev

    def op(self, en, fn, reads=(), writes=()):
        e = self.eng[en]
        self._waits(en, reads, writes)
        ins = fn(e["h"])
        e["cnt"] += 1
        ins.then_inc(e["sem"], 1)
        self._mark((e["sem"], e["cnt"], en), reads, writes)

    def group(self, en, fns, reads=(), writes=()):
        e = self.eng[en]
        self._waits(en, reads, writes)
        ins = None
        for fn in fns:
            ins = fn(e["h"])
        e["cnt"] += 1
        ins.then_inc(e["sem"], 1)
        self._mark((e["sem"], e["cnt"], en), reads, writes)

    def dma(self, qn, out, in_, reads=(), writes=(), key=None):
        e = self.eng[qn]
        self._waits(qn, reads, writes)
        key = key or (writes[0] if writes else reads[0])
        if key not in self.dsem:
            self.dsem[key] = [self.nc.alloc_semaphore("d%d" % len(self.dsem)), 0]
        ds = self.dsem[key]
        e["h"].dma_start(out=out, in_=in_).then_inc(ds[0], 16)
        ds[1] += 16
        self._mark((ds[0], ds[1], "dma"), reads, writes)

    def barrier(self):
        for en, e in self.eng.items():
            for fn, f in self.eng.items():
                if fn != en and f["cnt"] > e["seen"].get(f["sem"].name, 0):
                    e["h"].wait_ge(f["sem"], f["cnt"])
                    e["seen"][f["sem"].name] = f["cnt"]
            for k, (sem, tot) in self.dsem.items():
                if tot > e["seen"].get(sem.name, 0):
                    e["h"].wait_ge(sem, tot)
                    e["seen"][sem.name] = tot

    def finish(self):
        e = self.eng["sp"]
        for fn, f in self.eng.items():
            if fn != "sp" and f["cnt"] > e["seen"].get(f["sem"].name, 0):
                e["h"].wait_ge(f["sem"], f["cnt"])
        for k, (sem, tot) in self.dsem.items():
            if tot > e["seen"].get(sem.name, 0):
                e["h"].wait_ge(sem, tot)


def na_row_slot(l):
    if 4 <= l < 36:
        return (l - 4) * 64
    if l < 4:
        return 2048 + l * 64
    return 2304 + (l - 36) * 64


def build(stop_after=None, dbg=None):
    nc = bass.Bass("TRN2", target_bir_lowering=False)
    T = Trk(nc)

    def din(name, shape, dt=F32):
        return nc.dram_tensor(name, list(shape), dt, kind="ExternalInput").ap()

    xs = din("xs", [NSLOT, D])
    cvec = din("cvec", [2, D])
    w_mod = din("w_mod", [D, 6 * D]); b_mod = din("b_mod", [6 * D])
    g_attn = din("g_attn", [D]); g_ffn = din("g_ffn", [D]); g_fin = din("g_fin", [D])
    w_in = din("w_in", [D, 2208]); w_krs = din("w_krs", [D, 32])
    gq = din("gq", [384]); gkv = din("gkv", [256])
    w_uq = din("w_uq", [384, 768]); w_uqs = din("w_uqs", [384, 8, 32]); w_ukv = din("w_ukv", [256, 1024])
    w_out = din("w_out", [D, D])
    w_r = din("w_r", [D, 20]); b_r = din("b_r", [20])
    w_gate = din("w_gate", [16, D, 512]); w_up = din("w_up", [16, D, 512]); w_down = din("w_down", [16, 512, D])
    kmask_d = din("kmask", [128, NT])
    ctab = din("ctab", [32, NSLOT]); stab = din("stab", [32, NSLOT])
    nab = din("nab", [8, 8, 128, 512]); rmc_d = din("rmc", [128, 4, 64])
    out = nc.dram_tensor("out", [2048, D], F32, kind="ExternalOutput").ap()
    dbg_out = {}

    def sbat(name, shape, dt, at):
        nbytes = int(np.prod(shape[1:])) * (2 if dt == BF16 else 4)
        assert at % 32 == 0 and at + nbytes <= 229344, (name, at, nbytes)
        return nc.alloc_sbuf_tensor_at(name, list(shape), dt, offset=at)
    cur = [16512]
    def sb(name, shape, dt):
        nbytes = (int(np.prod(shape[1:])) * (2 if dt == BF16 else 4) + 31) // 32 * 32
        off = cur[0]; cur[0] += nbytes
        return sbat(name, shape, dt, off)
    ident = sb("ident", [128, 128], BF16)
    ones_bf = sb("ones_bf", [128, 128], BF16)
    onesf = sb("onesf", [128, 128], F32)
    epsc = sb("epsc", [128, 1], F32)
    modc = sb("modc", [128, 64], F32)
    gqc = sb("gqc", [128, 4], F32); gkvc = sb("gkvc", [128, 2], F32)
    kmask = sb("kmask_s", [128, NT], F32)
    ss = sb("ss", [128, NT], F32); sd = sb("sd", [128, NT], F32); rstd = sb("rstd", [128, NT], F32)
    rmc = sb("rmc_s", [128, 4, 64], BF16)
    shiftm = sb("shiftm", [64, 128], BF16)
    assert cur[0] <= 25728, cur[0]
    TMP = 25728
    cur[0] = TMP
    tmpf = [sb("tmpf%d" % i, [128, 512], F32) for i in range(3)]
    sq = [sb("sq%d" % i, [128, 512], BF16) for i in range(3)]
    sdb = sb("sdb", [128, 512], F32)
    rb = sb("rb", [128, 512], F32)
    ctt = sb("ctt", [128, 512], F32); stt = sb("stt", [128, 512], F32)
    r2 = sb("r2", [128, 512], F32)
    krot_tmp = sb("krot_tmp", [128, 3072], BF16)
    assert cur[0] <= 52352, cur[0]
    A0 = 52352
    kvn = sbat("kvn", [128, 2, NSLOT], BF16, A0)
    qlatn = sbat("qlatn", [128, 3, 2048], BF16, A0 + 34816)
    NR = A0 + 34816 + 12288
    KN = sbat("KN", [128, 4, 2816], BF16, NR)
    VN = sbat("VN", [128, 22, 8, 65], BF16, NR + 22528)
    QN = sbat("QN", [128, 4, 2048], BF16, NR + 22528 + 22880)
    NR_END = NR + 22528 + 22880 + 16384
    KH = [sbat("KH%d" % i, [128, NSLOT], BF16, NR + i * 17408) for i in range(2)]
    VH = [sbat("VH%d" % i, [128, NT, 65], BF16, NR + 34816 + i * 8864) for i in range(2)]
    QH = [sbat("QH%d" % i, [128, 2048], BF16, NR + 34816 + 17728 + i * 4096) for i in range(2)]
    assert NR + 34816 + 17728 + 8192 <= NR_END
    ATR = NR_END
    AT = sbat("AT", [128, 8, 2048], BF16, ATR)
    TAIL = ATR + 32768
    assert TAIL == 194016, TAIL
    cur[0] = TAIL
    PT2 = [sb("PT%d" % i, [128, 1024], BF16) for i in range(4)]
    osb = [sbat("osb%d" % i, [128, 512], F32, TMP + i * 2048) for i in range(2)]
    rc = sbat("rc", [128, 512], F32, TMP + 4096)
    onbs = [sbat("onb%d" % i, [64, 512], BF16, TMP + 6144 + i * 1024) for i in range(2)]
    ATT_W = cur[0]
    class Half:
        def __init__(self, t, off):
            self.t, self.off = t, off
        def __getitem__(self, key):
            r, c = key
            a = (c.start or 0) + self.off
            b_ = (c.stop if c.stop is not None else 512) + self.off
            return self.t[r, a:b_]
    PP = [nc.alloc_psum_tensor("pp%d" % i, [128, 1024], F32) for i in range(2)]
    PS = [nc.alloc_psum_tensor("ps%d" % i, [128, 512], F32) for i in range(4)]
    PS += [Half(PP[0], 0), Half(PP[0], 512), Half(PP[1], 0)]
    pTt = PP[1][:, 512:1024].bitcast(BF16)
    rr = [0]
    def psn(lo=0, hi=8):
        i = lo + rr[0] % (hi - lo); rr[0] += 1
        return i
    gscr = nc.dram_tensor("gscr", [2, D], F32).ap()

    T.op("pool", lambda e: e.memset(ident[:], 0.0), writes=["ident"])
    T.op("pool", lambda e: e.affine_select(out=ident[:], in_=ident[:], pattern=[[-1, 128]], compare_op=ALU.not_equal,
                                           fill=1.0, base=0, channel_multiplier=1), reads=["ident"], writes=["ident"])
    T.op("pool", lambda e: e.memset(shiftm[:], 0.0), writes=["shiftm"])
    T.op("pool", lambda e: e.affine_select(out=shiftm[:], in_=shiftm[:], pattern=[[-1, 128]], compare_op=ALU.not_equal,
                                           fill=1.0, base=64, channel_multiplier=1), reads=["shiftm"], writes=["shiftm"])
    T.op("pool", lambda e: e.memset(ones_bf[:], 1.0), writes=["ones_bf"])
    T.op("pool", lambda e: e.memset(onesf[:], 1.0), writes=["onesf"])
    T.op("pool", lambda e: e.memset(epsc[:], EPS), writes=["epsc"])
    T.op("pool", lambda e: e.memset(ss[:], 0.0), writes=["ss_all"])
    T.dma("sp", kmask[:], kmask_d[:, :], writes=["kmask"])
    T.dma("pool", rmc[:], rmc_d[:, :, :], writes=["rmc"])
    with nc.allow_non_contiguous_dma(reason="tiny column loads"):
        T.dma("sp", gqc[:, 0:3], gq.rearrange("(c p) -> p c", p=128), writes=["gqc"])
        T.dma("sp", gkvc[:, 0:2], gkv.rearrange("(c p) -> p c", p=128), writes=["gkvc"])

    PTR = 7
    pT = pTt

    def rms_feat(ps_list, nfeat, gcol, gkey, dst_fn, dkey):
        n = len(ps_list)
        for c, pi in enumerate(ps_list):
            T.op("act", lambda e, c=c, pi=pi: e.activation(out=tmpf[c][:], in_=PS[pi][:, :], func=AF.Copy), reads=["ps%d" % pi], writes=["tmpf%d" % c])
            T.op("act", lambda e, c=c: e.activation(out=sq[c][:], in_=tmpf[c][:], func=AF.Square), reads=["tmpf%d" % c], writes=["sq%d" % c])
        pq = psn(0, 7)
        T.group("pe", [lambda e, c=c, pq=pq: e.matmul(PS[pq][:, :], lhsT=ones_bf[:], rhs=sq[c][:], start=(c == 0), stop=(c == n - 1)) for c in range(n)],
                reads=["ones_bf"] + ["sq%d" % c for c in range(n)], writes=["ps%d" % pq])
        T.op("act", lambda e, pq=pq: e.activation(out=sdb[:], in_=PS[pq][:, :], func=AF.Sqrt, bias=epsc[:], scale=1.0 / nfeat),
             reads=["ps%d" % pq, "epsc"], writes=["sdb"])
        T.op("dve", lambda e: e.reciprocal(out=rb[:], in_=sdb[:]), reads=["sdb"], writes=["rb"])
        for c in range(n):
            T.op("dve", lambda e, c=c: e.scalar_tensor_tensor(out=dst_fn(c), in0=tmpf[c][:], scalar=gcol[:, c:c + 1], in1=rb[:], op0=ALU.mult, op1=ALU.mult),
                 reads=["tmpf%d" % c, "rb", gkey], writes=[dkey])

    def norm_a(t, xb, xk, nb, nk, rkey):
        T.op("act", lambda e: e.activation(out=nb[:], in_=xb, func=AF.Square, accum_out=ss[:, t:t + 1]), reads=[xk], writes=[nk, "ss%d" % t])
        T.op("act", lambda e: e.activation(out=sd[:, t:t + 1], in_=ss[:, t:t + 1], func=AF.Sqrt, bias=epsc[:], scale=1.0 / D),
             reads=["ss%d" % t, "epsc"], writes=["sd%d" % t])
        T.op("dve", lambda e: e.reciprocal(out=rstd[:, t:t + 1], in_=sd[:, t:t + 1]), reads=["sd%d" % t], writes=[rkey])
        T.op("act", lambda e: e.activation(out=nb[:], in_=xb, func=AF.Identity, scale=rstd[:, t:t + 1]), reads=[xk, rkey], writes=[nk])

    def norm_b(nb, nk, dst_fn, dkeys, cb):
        T.group("pe", [lambda e, kc=kc: e.transpose(out=pT[:, kc * 128:(kc + 1) * 128], in_=nb[:, kc * 128:(kc + 1) * 128], identity=ident[:])
                       for kc in range(8)], reads=[nk, "ident"], writes=["ps%d" % PTR])
        for kc in range(8):
            dst = dst_fn(kc); src = pT[:, kc * 128:(kc + 1) * 128]
            T.op("dve", lambda e, dst=dst, src=src, kc=kc: e.tensor_scalar(out=dst, in0=src, scalar1=modc[:, cb + kc:cb + kc + 1],
                                                                     scalar2=modc[:, cb + 8 + kc:cb + 9 + kc], op0=ALU.mult, op1=ALU.add),
                 reads=["ps%d" % PTR, "modc"], writes=[dkeys[kc]])

    def norm_transpose(src_ap, t, xb, xk, nb, nk, dst_fn, dkeys, cb, rkey):
        norm_a(t, xb, xk, nb, nk, rkey)
        norm_b(nb, nk, dst_fn, dkeys, cb)

    def phaseA(s_list, base, full, krot_dst, xn_base=None, state=None, load_only=False):
        if state is not None:
            return phaseA_run(s_list, full, krot_dst, state)
        o = [base]
        def wa(name, shape, dt):
            nbytes = (int(np.prod(shape[1:])) * (2 if dt == BF16 else 4) + 31) // 32 * 32
            t_ = sbat(name, shape, dt, o[0]); o[0] += nbytes
            return t_
        sfx = "f" if full else "p"
        ncols = 2208 if full else 256
        win_b = wa("win_b" + sfx, [128, 8, ncols], BF16)
        wkr_b = wa("wkr_b" + sfx, [128, 8, 96], BF16)
        wkrs_b = wa("wkrs_b" + sfx, [128, 8, 96], BF16)
        xt = [wa("xt%d%s" % (i, sfx), [128, D], F32) for i in range(2)]
        if xn_base is not None:
            xt += [sbat("xt%d%s" % (2 + i, sfx), [128, D], F32, xn_base + 4096 + i * 4096) for i in range(2)]
        if xn_base is None:
            xn = [wa("xn%d%s" % (i, sfx), [128, D], BF16) for i in range(2)]
        else:
            xn = [sbat("xn%d%s" % (i, sfx), [128, D], BF16, xn_base + i * 2048) for i in range(2)]
        hxT = [wa("hxT%d%s" % (i, sfx), [128, 8, 512], BF16) for i in range(2)]
        assert o[0] <= 229344, o[0]
        wkey = "win_b"
        if full:
            for kc in range(8):
                T.dma("pool", win_b[:, kc, :], w_in[kc * 128:(kc + 1) * 128, :], writes=[wkey])
            cko = C_KVLAT
        else:
            T.dma("pool", win_b[:], w_in[:, C_KVLAT:C_KVLAT + 256].rearrange("(kc p) c -> p kc c", p=128), writes=[wkey])
            cko = 0
        T.op("pool", lambda e: e.memset(wkr_b[:], 0.0), writes=["wkr_b"])
        T.op("pool", lambda e: e.memset(wkrs_b[:], 0.0), writes=["wkrs_b"])
        T.dma("pool", wkr_b[:, :, 64:96], w_in[:, C_KR:C_KR + 32].rearrange("(kc p) c -> p kc c", p=128), writes=["wkr_b"])
        T.dma("pool", wkrs_b[:, :, 64:96], w_krs.rearrange("(kc p) c -> p kc c", p=128), writes=["wkrs_b"])
        T.op("pool", lambda e: e.tensor_scalar(out=wkrs_b[:, :, 64:80], in0=wkrs_b[:, :, 64:80], scalar1=-1.0, scalar2=None, op0=ALU.mult),
             reads=["wkrs_b"], writes=["wkrs_b"])
        state_ = dict(win_b=win_b, wkr_b=wkr_b, wkrs_b=wkrs_b, xt=xt, xn=xn, hxT=hxT, wkey=wkey, cko=cko)
        if load_only:
            return state_
        return phaseA_run(s_list, full, krot_dst, state_)

    def phaseA_run(s_list, full, krot_dst, st_):
        win_b, wkr_b, wkrs_b, xt, xn, hxT, wkey, cko = (st_[k] for k in ("win_b", "wkr_b", "wkrs_b", "xt", "xn", "hxT", "wkey", "cko"))
        if stop_after == "paw":
            return "stop"
        def tinfo(s, tt):
            t = 4 * s + tt
            nx = len(xt)
            return t, xt[t % nx], "xt%d" % (t % nx), xn[t % 2], "xn%d" % (t % 2)
        def nt_dma(s, tt):
            t, xb, xk, nb, nk = tinfo(s, tt)
            T.dma("sp", xb[:], xs[t * 128:(t + 1) * 128, :], writes=[xk])
        def nt_a(s, tt):
            t, xb, xk, nb, nk = tinfo(s, tt)
            norm_a(t, xb[:], xk, nb, nk, "rstd%d" % t)
        def nt_b(s, tt):
            t, xb, xk, nb, nk = tinfo(s, tt)
            hb = hxT[s % 2]; hk = "hxT%d" % (s % 2)
            hks = [hk + "_%d" % kc for kc in range(8)]
            cb = 32 if t in (20, 21) else 0
            norm_b(nb, nk, lambda kc, tt=tt, hb=hb: hb[:, kc, tt * 128:(tt + 1) * 128], hks, cb)

        def mm_parts(s):
            hb = hxT[s % 2]; hk = "hxT%d" % (s % 2)
            hks = [hk + "_%d" % kc for kc in range(8)]
            sl = slice(s * 512, (s + 1) * 512)
            def p0():
                pl = []
                for c in range(2):
                    pi = psn(0, 7); pl.append(pi)
                    T.group("pe", [lambda e, kc=kc, pi=pi, c=c: e.matmul(PS[pi][:, :], lhsT=win_b[:, kc, cko + c * 128:cko + (c + 1) * 128], rhs=hb[:, kc, :],
                                                                     start=(kc == 0), stop=(kc == 7)) for kc in range(8)],
                            reads=hks + [wkey], writes=["ps%d" % pi])
                rms_feat(pl, 256, gkvc, "gkvc", lambda c: kvn[:, c, sl], "kvn")
                pk = psn(0, 7); pks = psn(0, 7)
                T.group("pe", [lambda e, kc=kc: e.matmul(PS[pk][0:96, :], lhsT=wkr_b[:, kc, :], rhs=hb[:, kc, :], start=(kc == 0), stop=(kc == 7)) for kc in range(8)],
                        reads=hks + ["wkr_b"], writes=["ps%d" % pk])
                T.group("pe", [lambda e, kc=kc: e.matmul(PS[pks][0:96, :], lhsT=wkrs_b[:, kc, :], rhs=hb[:, kc, :], start=(kc == 0), stop=(kc == 7)) for kc in range(8)],
                        reads=hks + ["wkrs_b"], writes=["ps%d" % pks])
                T.dma("sp", ctt[64:96, :], ctab[:, sl], writes=["ctt"])
                T.dma("sp", stt[64:96, :], stab[:, sl], writes=["stt"])
                T.op("dve", lambda e: e.tensor_tensor(out=tmpf[2][64:96, :], in0=PS[pk][64:96, :], in1=ctt[64:96, :], op=ALU.mult), reads=["ps%d" % pk, "ctt"], writes=["tmpf2"])
                T.op("dve", lambda e: e.tensor_tensor(out=r2[64:96, :], in0=PS[pks][64:96, :], in1=stt[64:96, :], op=ALU.mult), reads=["ps%d" % pks, "stt"], writes=["r2"])
                for dst, dk in krot_dst(s):
                    T.op("pool", lambda e, dst=dst: e.tensor_tensor(out=dst, in0=tmpf[2][64:96, :], in1=r2[64:96, :], op=ALU.add), reads=["tmpf2", "r2"], writes=[dk])
            def p1():
                if not (full and s < 6):
                    return
                nsl = slice(s * 512, (s + 1) * 512) if s < 5 else slice(2560, 2816)
                ncol = slice(0, 512) if s < 5 else slice(0, 256)
                for j in range(4):
                    pi = psn(0, 7)
                    T.group("pe", [lambda e, kc=kc, pi=pi, j=j: e.matmul(PS[pi][:, :], lhsT=win_b[:, kc, C_NK + j * 128:C_NK + (j + 1) * 128], rhs=hb[:, kc, :],
                                                                     start=(kc == 0), stop=(kc == 7)) for kc in range(8)],
                            reads=hks + [wkey], writes=["ps%d" % pi])
                    if j % 2 == 0:
                        T.op("act", lambda e, pi=pi, j=j: e.activation(out=KN[:, j, nsl], in_=PS[pi][:, ncol], func=AF.Copy), reads=["ps%d" % pi], writes=["KN"])
                    else:
                        T.op("dve", lambda e, pi=pi, j=j: e.tensor_copy(out=KN[:, j, nsl], in_=PS[pi][:, ncol]), reads=["ps%d" % pi], writes=["KN"])
            def p2():
                if not (full and s < 6):
                    return
                for tt in range(4 if s < 5 else 2):
                    pi = psn(0, 7); t = 4 * s + tt
                    T.group("pe", [lambda e, kc=kc, pi=pi, tt=tt: e.matmul(PS[pi][:, :], lhsT=hb[:, kc, tt * 128:(tt + 1) * 128], rhs=win_b[:, kc, C_NV:C_NV + 512],
                                                                       start=(kc == 0), stop=(kc == 7)) for kc in range(8)],
                            reads=hks + [wkey], writes=["ps%d" % pi])
                    src = PS[pi][:, :].rearrange("p (h d) -> p h d", h=8)
                    if tt % 2 == 0:
                        T.op("dve", lambda e, src=src, t=t: e.tensor_copy(out=VN[:, t, :, 0:64], in_=src), reads=["ps%d" % pi], writes=["VN"])
                    else:
                        T.op("act", lambda e, src=src, t=t: e.activation(out=VN[:, t, :, 0:64], in_=src, func=AF.Copy), reads=["ps%d" % pi], writes=["VN"])
            def p3():
                if not (full and s < 4):
                    return
                pl = []
                for c in range(3):
                    pi = psn(0, 7); pl.append(pi)
                    T.group("pe", [lambda e, kc=kc, pi=pi, c=c: e.matmul(PS[pi][:, :], lhsT=win_b[:, kc, c * 128:(c + 1) * 128], rhs=hb[:, kc, :],
                                                                     start=(kc == 0), stop=(kc == 7)) for kc in range(8)],
                            reads=hks + [wkey], writes=["ps%d" % pi])
                rms_feat(pl, 384, gqc, "gqc", lambda c: qlatn[:, c, sl], "qlatn")
                for j in range(4):
                    pi = psn(0, 7)
                    T.group("pe", [lambda e, kc=kc, pi=pi, j=j: e.matmul(PS[pi][:, :], lhsT=win_b[:, kc, 384 + j * 128:384 + (j + 1) * 128], rhs=hb[:, kc, :],
                                                                     start=(kc == 0), stop=(kc == 7)) for kc in range(8)],
                            reads=hks + [wkey], writes=["ps%d" % pi])
                    T.op("act", lambda e, pi=pi, j=j: e.activation(out=QN[:, j, sl], in_=PS[pi][:, :], func=AF.Copy, scale=0.125), reads=["ps%d" % pi], writes=["QN"])
            return [p0, p1, p2, p3]

        s_list = list(s_list)
        seq = [(s, tt) for s in s_list for tt in range(4)]
        depth = len(xt) - 1
        for k0 in range(min(depth, len(seq))):
            nt_dma(*seq[k0])
        nt_a(*seq[0])
        prev_parts = None
        for k, (s, tt) in enumerate(seq):
            if k + depth < len(seq):
                nt_dma(*seq[k + depth])
            if k + 1 < len(seq):
                nt_a(*seq[k + 1])
            nt_b(s, tt)
            if prev_parts is not None:
                prev_parts[tt]()
            if tt == 3:
                prev_parts = mm_parts(s)
        for p_ in prev_parts:
            p_()

    PA1 = phaseA(None, ATR, True, None, load_only=True)
    ccol = sbat("ccol", [128, 2, 8], F32, A0)
    scol = sbat("scol", [128, 2, 8], BF16, A0 + 64)
    wmb = [sbat("wmb%d" % i, [128, 8, 512], BF16, A0 + 128 + i * 8192) for i in range(2)]
    brow = sbat("brow", [1, 512], F32, A0 + 128 + 16384)
    grow = sbat("grow", [1, 512], F32, A0 + 128 + 16384 + 2048)
    mrow = [sbat("mrow%d" % i, [1, 512], F32, A0 + 128 + 16384 + 4096 + i * 2048) for i in range(2)]
    with nc.allow_non_contiguous_dma(reason="tiny column loads"):
        T.dma("sp", ccol[:], cvec.rearrange("r (kc p) -> p r kc", p=128), writes=["ccol"])
    T.op("act", lambda e: e.activation(out=scol[:], in_=ccol[:], func=AF.Silu), reads=["ccol"], writes=["scol"])
    PCOL = 6
    for j in range(12):
        v, hf = j // 2, j % 2
        wb = wmb[j % 2]; wk = "wmb%d" % (j % 2)
        T.dma("pool", wb[:], w_mod[:, j * 512:(j + 1) * 512].rearrange("(kc p) c -> p kc c", p=128), writes=[wk])
        T.dma("sp", brow[:], b_mod[None, j * 512:(j + 1) * 512], writes=["brow"])
        if v in (1, 4):
            gsrc = g_attn if v == 1 else g_ffn
            T.dma("sp", grow[:], gsrc[None, hf * 512:(hf + 1) * 512], writes=["grow"])
        for r in range(2):
            if r == 1 and v > 1:
                continue
            pi = psn(0, 6)
            T.group("pe", [lambda e, kc=kc, pi=pi, r=r, wb=wb: e.matmul(PS[pi][0:1, :], lhsT=scol[:, r, kc:kc + 1], rhs=wb[:, kc, :],
                                                                    start=(kc == 0), stop=(kc == 7)) for kc in range(8)],
                    reads=["scol", wk], writes=["ps%d" % pi])
            mr = mrow[r]; mk = "mrow%d" % r
            T.op("dve", lambda e, pi=pi, mr=mr: e.tensor_tensor(out=mr[:], in0=PS[pi][0:1, :], in1=brow[:], op=ALU.add),
                 reads=["ps%d" % pi, "brow"], writes=[mk])
            if v in (1, 4):
                T.op("dve", lambda e, mr=mr: e.scalar_tensor_tensor(out=mr[:], in0=mr[:], scalar=1.0, in1=grow[:], op0=ALU.add, op1=ALU.mult),
                     reads=[mk, "grow"], writes=[mk])
            if v in (2, 5):
                gi = 0 if v == 2 else 1
                T.dma("sp", gscr[gi:gi + 1, hf * 512:(hf + 1) * 512], mr[:], reads=[mk], writes=["gscr_r"], key="gscr")
            else:
                if r == 0:
                    base = {1: 0, 0: 8, 4: 16, 3: 24}[v]
                else:
                    base = {1: 32, 0: 40}[v]
                T.group("pe", [lambda e, i=i, mr=mr, base=base, hf=hf: e.matmul(PS[PCOL][:, base + hf * 4 + i: base + hf * 4 + i + 1],
                                                                             lhsT=mr[0:1, i * 128:(i + 1) * 128], rhs=onesf[0:1, 0:1],
                                                                             start=True, stop=True) for i in range(4)],
                        reads=[mk, "onesf"], writes=["pscol"])
    T.op("dve", lambda e: e.tensor_copy(out=modc[:, 0:48], in_=PS[PCOL][:, 0:48]), reads=["pscol"], writes=["modc"])
    if stop_after == "adaln":
        dbg_out["modc"] = (modc, [128, 64], F32)
        return finish(nc, T, dbg_out, out)
    T.barrier()

    T.op("pool", lambda e: e.memset(VN[:, :, :, 64:65], 1.0), writes=["VN"])
    rv = phaseA(range(6), ATR, True, lambda s: [(krot_tmp[64:96, s * 512:(s + 1) * 512], "krot_tmp")], state=PA1)
    if rv == "stop":
        return finish(nc, T, dbg_out, out)
    if stop_after == "phaseA":
        dbg_out["kvn"] = (kvn, [128, 2, NSLOT], BF16); dbg_out["qlatn"] = (qlatn, [128, 3, 2048], BF16)
        dbg_out["KN"] = (KN, [128, 4, 2816], BF16); dbg_out["QN"] = (QN, [128, 4, 2048], BF16)
        dbg_out["VN"] = (VN, [128, 22, 8, 65], BF16); dbg_out["krot_tmp"] = (krot_tmp, [128, 3072], BF16)
        return finish(nc, T, dbg_out, out)
    T.barrier()

    nrm = [0]
    pending = []
    def run_pending(i, flush=False):
        for item in [x for x in pending if flush or x[0] <= i]:
            pending.remove(item)
            item[1]()
    def normalize_out(po, h_at, qsl, i, scr=3, off=0, act_evac=False):
        ob = osb[nrm[0] % 2]; ok = "osb%d" % (nrm[0] % 2); onb = onbs[nrm[0] % 2]; onk = "onb%d" % (nrm[0] % 2); nrm[0] += 1
        if act_evac:
            T.op("act", lambda e: e.activation(out=ob[0:65, :], in_=PS[po][0:65, :], func=AF.Copy), reads=["ps%d" % po], writes=[ok])
        else:
            T.op("dve", lambda e: e.tensor_copy(out=ob[0:65, :], in_=PS[po][0:65, :]), reads=["ps%d" % po], writes=[ok])
        nq = 2 if act_evac else 4
        wq = 512 // nq
        for q4 in range(nq):
            pending.append([i + 1 + off + q4, lambda q4=q4: T.op("dve", lambda e: e.reciprocal(out=ob[64:65, q4 * wq:(q4 + 1) * wq], in_=ob[64:65, q4 * wq:(q4 + 1) * wq]),
                                                             reads=[ok], writes=[ok])])
        def st3():
            T.group("pe", [lambda e: e.matmul(PS[scr][:, :], lhsT=shiftm[:], rhs=onb[:], start=True, stop=True)], reads=[onk, "shiftm"], writes=["ps%d" % scr])
            if act_evac:
                T.op("act", lambda e: e.activation(out=AT[64:128, h_at // 2, qsl], in_=PS[scr][64:128, :], func=AF.Copy), reads=["ps%d" % scr], writes=["AT"])
            else:
                T.op("dve", lambda e: e.tensor_copy(out=AT[64:128, h_at // 2, qsl], in_=PS[scr][64:128, :]), reads=["ps%d" % scr], writes=["AT"])
        def st2():
            T.group("pe", [lambda e: e.matmul(PS[scr][0:64, :], lhsT=onesf[64:65, 0:64], rhs=ob[64:65, :], start=True, stop=True)],
                    reads=[ok, "onesf"], writes=["ps%d" % scr])
            if h_at % 2 == 0:
                T.op("dve", lambda e: e.tensor_tensor(out=AT[0:64, h_at // 2, qsl], in0=ob[0:64, :], in1=PS[scr][0:64, :], op=ALU.mult),
                     reads=[ok, "ps%d" % scr], writes=["AT"])
            else:
                T.op("dve", lambda e: e.tensor_tensor(out=onb[:], in0=ob[0:64, :], in1=PS[scr][0:64, :], op=ALU.mult),
                     reads=[ok, "ps%d" % scr], writes=[onk])
                pending.append([i + 9 + off, st3])
        pending.append([i + 6 + off, st2])

    cur[0] = ATT_W
    nabt = [sb("nabt%d" % i, [128, 8, 512], BF16) for i in range(2)]
    assert cur[0] <= 229344
    stg = [sbat("stg%d" % i, [128, 1024], F32, TMP + 8192 + i * 4096) for i in range(2)]
    combt = [sbat("combt%d" % i, [128, 8, 512], BF16, ATR + i * 8192) for i in range(2)]
    na_steps = [(h, g, p) for h in range(8) for g in range(4) for p in range(5)]
    pti = [0]
    na_pt = {}
    def na_load(h):
        T.dma("pool", nabt[h % 2][:], nab[h].rearrange("c p f -> p c f"), writes=["nabt%d" % (h % 2)])
    def na_comb(h, g):
        k = (h * 4 + g) % 2
        T.op("pool", lambda e: e.tensor_tensor(out=combt[k][:].rearrange("p c (i q) -> p c i q", q=64),
                                               in0=nabt[h % 2][:].rearrange("p c (i q) -> p c i q", q=64),
                                               in1=rmc[:, g, :].rearrange("p (c i) -> p c i", i=8).unsqueeze(3).to_broadcast([128, 8, 8, 64]), op=ALU.add),
             reads=["nabt%d" % (h % 2), "rmc"], writes=["combt%d" % k])
    qz = [[sb("qz%d_%d" % (par, k_), [128, 512], BF16) for k_ in range(2)] for par in range(2)]
    assert cur[0] <= 229344
    for par in range(2):
        for k_ in range(2):
            T.op("pool", lambda e, par=par, k_=k_: e.memset(qz[par][k_][:], 0.0), writes=["qz%d_%d" % (par, k_)])
    def na_qz(h, g):
        par = h % 2; k_ = (h * 4 + g) // 1 % 2
        pb_ = par * 64
        T.op("pool", lambda e: e.tensor_copy(out=qz[par][k_][pb_:pb_ + 64, :], in_=QN[pb_:pb_ + 64, h // 2, g * 512:(g + 1) * 512]),
             reads=["QN"], writes=["qz%d_%d" % (par, k_)])
    na_load(0); na_load(1); na_comb(0, 0); na_qz(0, 0)
    def na_qk(i):
        h, g, p = na_steps[i]
        j = h // 2; pb = (h % 2) * 64
        if p == 0:
            nh, ng = (h, g + 1) if g < 3 else (h + 1, 0)
            if nh < 8:
                if ng == 0 and nh + 1 < 8 and False:
                    pass
                na_comb(nh, ng)
                na_qz(nh, ng)
        if p == 4 and g == 3 and h + 2 < 8:
            na_load(h + 2)
        qsl = slice(g * 512, (g + 1) * 512)
        pp = i % 2
        for half in range(2):
            c = 2 * p + half
            ks = na_row_slot(8 * g + 2 * c) if c < 8 else 2560 + (c - 8) * 128
            dst = PP[pp][:, half * 512:(half + 1) * 512]
            qzb = qz[h % 2][(h * 4 + g) % 2]
            T.group("pe", [lambda e, ks=ks, dst=dst: e.matmul(dst, lhsT=KN[:, j, ks:ks + 128], rhs=qzb[:, :], start=True, stop=True)],
                    reads=["KN", "qz%d_%d" % (h % 2, (h * 4 + g) % 2)], writes=["ps%d" % (4 + 2 * pp + half)])
        k = pti[0] % 4; pti[0] += 1
        na_pt[i] = k
        pk_ = ["ps%d" % (4 + 2 * pp), "ps%d" % (5 + 2 * pp)]
        if p < 4:
            ck = (h * 4 + g) % 2
            sg_ = stg[i % 2]; sgk = "stg%d" % (i % 2)
            T.op("dve", lambda e: e.tensor_tensor(out=sg_[:, :], in0=PP[pp][:, :], in1=combt[ck][:, 2 * p:2 * p + 2, :].rearrange("p c f -> p (c f)"), op=ALU.add),
                 reads=pk_ + ["combt%d" % ck], writes=[sgk])
            T.op("act", lambda e: e.activation(out=PT2[k][:, :], in_=sg_[:, :], func=AF.Exp), reads=[sgk], writes=["PT%d" % k])
        else:
            T.op("act", lambda e: e.activation(out=PT2[k][:, :], in_=PP[pp][:, :], func=AF.Exp), reads=pk_, writes=["PT%d" % k])
    def na_pv(i):
        h, g, p = na_steps[i]
        k = na_pt.pop(i)
        po = (h * 4 + g) % 2
        fns = []
        for half in range(2):
            c = 2 * p + half
            ks = na_row_slot(8 * g + 2 * c) if c < 8 else 2560 + (c - 8) * 128
            fns.append(lambda e, ks=ks, half=half, c=c: e.matmul(PS[po][0:65, :], lhsT=VN[:, ks // 128, h, 0:65], rhs=PT2[k][:, half * 512:(half + 1) * 512],
                                                               start=(c == 0), stop=(c == 9)))
        T.group("pe", fns, reads=["VN", "PT%d" % k], writes=["ps%d" % po])
        if p == 4:
            normalize_out(po, 8 + h, slice(g * 512, (g + 1) * 512), i, act_evac=True)
    LA = 3
    for i in range(len(na_steps) + LA):
        if i < len(na_steps):
            na_qk(i)
        if i >= LA:
            na_pv(i - LA)
        run_pending(i - LA)
    for _ in range(8):
        run_pending(0, flush=True)
    if stop_after == "na":
        dbg_out["AT"] = (AT, [128, 8, 2048], BF16)
        return finish(nc, T, dbg_out, out)
    T.barrier()

    for i in range(2):
        T.op("pool", lambda e, i=i: e.tensor_copy(out=KH[i][64:96, 0:3072], in_=krot_tmp[64:96, :]), reads=["krot_tmp"], writes=["KHr%d" % i])
    phaseA(range(6, NS), TAIL, False, lambda s: [(KH[i][64:96, s * 512:(s + 1) * 512], "KHr%d" % i) for i in range(2)], xn_base=ATR)
    T.barrier()

    cur[0] = ATT_W
    wuq_b = sb("wuq_b", [128, 3, 768], BF16)
    wuqs_b = sb("wuqs_b", [128, 3, 8, 96], BF16)
    wukv_b = sb("wukv_b", [128, 2, 1024], BF16)
    ctq = sbat("ctq", [128, 512], F32, TMP + 8192); stq = sbat("stq", [128, 512], F32, TMP + 10240)
    r1q = sbat("r1q", [128, 512], F32, TMP + 12288); r2q = sbat("r2q", [128, 512], F32, TMP + 14336)
    assert cur[0] <= 229344, cur[0]
    T.dma("pool", wuq_b[:], w_uq.rearrange("(kc p) c -> p kc c", p=128), writes=["wuq_b"])
    T.op("pool", lambda e: e.memset(wuqs_b[:], 0.0), writes=["wuqs_b"])
    for kc in range(3):
        T.dma("pool", wuqs_b[:, kc, :, 64:96], w_uqs[kc * 128:(kc + 1) * 128, :, :], writes=["wuqs_b"])
    T.op("pool", lambda e: e.tensor_scalar(out=wuqs_b[:, :, :, 64:80], in0=wuqs_b[:, :, :, 64:80], scalar1=-1.0, scalar2=None, op0=ALU.mult),
         reads=["wuqs_b"], writes=["wuqs_b"])
    T.dma("pool", wukv_b[:], w_ukv.rearrange("(kc p) c -> p kc c", p=128), writes=["wukv_b"])
    for i in range(2):
        T.op("pool", lambda e, i=i: e.memset(VH[i][:, :, 64:65], 1.0), writes=["VH%d" % i])
    SCALE = 96 ** -0.5

    def prep_units(h):
        b = h % 2
        units = []
        pbk = [0]
        def nb_():
            pbk[0] += 1
            return idle_set[0][pbk[0] % 2]
        for s in range(NS):
            def ku(s=s):
                PREP = nb_()
                T.group("pe", [lambda e, kc=kc: e.matmul(PS[PREP][0:64, :], lhsT=wukv_b[:, kc, h * 128:h * 128 + 64], rhs=kvn[:, kc, s * 512:(s + 1) * 512],
                                                         start=(kc == 0), stop=(kc == 1)) for kc in range(2)], reads=["kvn", "wukv_b"], writes=["ps%d" % PREP])
                T.op("dve", lambda e: e.tensor_copy(out=KH[b][0:64, s * 512:(s + 1) * 512], in_=PS[PREP][0:64, :]), reads=["ps%d" % PREP], writes=["KH%d" % b])
            units.append(ku)
            def vu(s=s):
                PREP = nb_()
                fns = []
                for tt in range(4):
                    for kc in range(2):
                        fns.append(lambda e, tt=tt, kc=kc: e.matmul(PS[PREP][:, tt * 64:(tt + 1) * 64], lhsT=kvn[:, kc, (4 * s + tt) * 128:(4 * s + tt + 1) * 128],
                                                                    rhs=wukv_b[:, kc, h * 128 + 64:h * 128 + 128], start=(kc == 0), stop=(kc == 1)))
                T.group("pe", fns, reads=["kvn", "wukv_b"], writes=["ps%d" % PREP])
                T.op("dve", lambda e: e.tensor_copy(out=VH[b][:, 4 * s:4 * s + 4, 0:64], in_=PS[PREP][:, 0:256].rearrange("p (t d) -> p t d", t=4)),
                     reads=["ps%d" % PREP], writes=["VH%d" % b])
            units.append(vu)
        for qc in range(4):
            def qu(qc=qc):
                PA_, PB_ = idle_set[0]
                qsl = slice(qc * 512, (qc + 1) * 512)
                T.dma("sp", ctq[64:96, :], ctab[:, qsl], writes=["ctq"])
                T.dma("sp", stq[64:96, :], stab[:, qsl], writes=["stq"])
                T.group("pe", [lambda e, kc=kc: e.matmul(PS[PA_][0:96, :], lhsT=wuq_b[:, kc, h * 96:(h + 1) * 96], rhs=qlatn[:, kc, qsl],
                                                         start=(kc == 0), stop=(kc == 2)) for kc in range(3)], reads=["qlatn", "wuq_b"], writes=["ps%d" % PA_])
                T.group("pe", [lambda e, kc=kc: e.matmul(PS[PB_][0:96, :], lhsT=wuqs_b[:, kc, h, :], rhs=qlatn[:, kc, qsl],
                                                         start=(kc == 0), stop=(kc == 2)) for kc in range(3)], reads=["qlatn", "wuqs_b"], writes=["ps%d" % PB_])
                T.op("dve", lambda e: e.tensor_copy(out=QH[b][0:64, qsl], in_=PS[PA_][0:64, :]), reads=["ps%d" % PA_], writes=["QH%d" % b])
                T.op("dve", lambda e: e.tensor_tensor(out=r1q[64:96, :], in0=PS[PA_][64:96, :], in1=ctq[64:96, :], op=ALU.mult), reads=["ps%d" % PA_, "ctq"], writes=["r1q"])
                T.op("dve", lambda e: e.tensor_tensor(out=r2q[64:96, :], in0=PS[PB_][64:96, :], in1=stq[64:96, :], op=ALU.mult), reads=["ps%d" % PB_, "stq"], writes=["r2q"])
                T.op("pool", lambda e: e.tensor_tensor(out=QH[b][64:96, qsl], in0=r1q[64:96, :], in1=r2q[64:96, :], op=ALU.add), reads=["r1q", "r2q"], writes=["QH%d" % b])
            units.append(qu)
        return units

    idle_set = [(2, 3)]
    for u in prep_units(0):
        u()
    gstep = [0]
    for h in range(8):
        b = h % 2
        nxt = prep_units(h + 1) if h < 7 else []
        ui = [0]
        steps = [(qp, kc) for qp in range(2) for kc in range(NT_MLA)]
        n = len(steps)
        ptm = {}
        def m_qk(i):
            qp, kc = steps[i]; pp = i % 2
            for half in range(2):
                qc = 2 * qp + half
                dst = PP[pp][:, half * 512:(half + 1) * 512]
                T.group("pe", [lambda e, dst=dst, kc=kc, qc=qc: e.matmul(dst, lhsT=KH[b][0:96, kc * 128:(kc + 1) * 128], rhs=QH[b][0:96, qc * 512:(qc + 1) * 512],
                                                                     start=True, stop=True)], reads=["KH%d" % b, "KHr%d" % b, "QH%d" % b], writes=["ps%d" % (4 + 2 * pp + half)])
            k = pti[0] % 3; pti[0] += 1
            ptm[i] = k
            T.op("act", lambda e: e.activation(out=PT2[k][:, :], in_=PP[pp][:, :], func=AF.Exp, bias=kmask[:, kc:kc + 1], scale=SCALE),
                 reads=["ps%d" % (4 + 2 * pp), "ps%d" % (5 + 2 * pp), "kmask"], writes=["PT%d" % k])
        def m_pv(i):
            qp, kc = steps[i]
            k = ptm.pop(i)
            ob_ = (0, 1) if qp == 0 else (2, 3)
            if kc == 0:
                idle_set[0] = (2, 3) if qp == 0 else (0, 1)
            for half in range(2):
                T.group("pe", [lambda e, half=half: e.matmul(PS[ob_[half]][0:65, :], lhsT=VH[b][:, kc, 0:65], rhs=PT2[k][:, half * 512:(half + 1) * 512],
                                                           start=(kc == 0), stop=(kc == NT_MLA - 1))], reads=["VH%d" % b, "PT%d" % k], writes=["ps%d" % ob_[half]])
            if kc == NT_MLA - 1:
                for half in range(2):
                    qc = 2 * qp + half
                    normalize_out(ob_[half], h, slice(qc * 512, (qc + 1) * 512), gstep[0], scr=ob_[half], off=4 * half)
            if nxt:
                want = min(len(nxt), (len(nxt) * (i + 1)) // (n - 16))
                while ui[0] < want:
                    nxt[ui[0]](); ui[0] += 1
        for i in range(n + 2):
            if i < n:
                m_qk(i)
            if i >= 2:
                gstep[0] += 1
                m_pv(i - 2)
                run_pending(gstep[0])
    for _ in range(8):
        run_pending(0, flush=True)
    if stop_after == "mla":
        dbg_out["AT"] = (AT, [128, 8, 2048], BF16)
        return finish(nc, T, dbg_out, out)
    T.barrier()

    T.op("pool", lambda e: e.memset(ss[:], 0.0), writes=["ss_all"])
    T.barrier()
    xnew = sbat("xnew", [128, 16, D], F32, A0)
    wout_b = sbat("wout_b", [128, 8, D], BF16, 117888)
    g1b = sbat("g1b", [128, D], F32, 134272)
    hx2T = sbat("hx2T", [128, 8, 2048], BF16, TAIL)
    xr = [sbat("xr%d" % i, [128, D], F32, TMP + i * 4096) for i in range(4)]
    xn2 = [sbat("xn2_%d" % i, [128, D], BF16, TMP + 16384 + i * 2048) for i in range(2)]
    T.dma("pool", wout_b[:], w_out.rearrange("(kc p) c -> p kc c", p=128), writes=["wout_b"])
    T.dma("sp", g1b[:], gscr[0:1, :].to_broadcast([128, D]), reads=["gscr_r"], writes=["g1b"], key="g1b")
    T.op("pool", lambda e: e.tensor_tensor(out=wout_b[:], in0=wout_b[:], in1=g1b[:].unsqueeze(1).to_broadcast([128, 8, D]), op=ALU.mult),
         reads=["wout_b", "g1b"], writes=["wout_b"])
    def wo_dma(t):
        T.dma("sp", xr[t % 4][:], xs[t * 128:(t + 1) * 128, :], writes=["xr%d" % (t % 4)])
    def wo_a(t):
        xb = xr[t % 4]; xk = "xr%d" % (t % 4)
        for hf in range(2):
            pi = psn(0, 7)
            T.group("pe", [lambda e, kc=kc, pi=pi, hf=hf: e.matmul(PS[pi][:, :], lhsT=AT[:, kc, t * 128:(t + 1) * 128], rhs=wout_b[:, kc, hf * 512:(hf + 1) * 512],
                                                               start=(kc == 0), stop=(kc == 7)) for kc in range(8)], reads=["AT", "wout_b"], writes=["ps%d" % pi])
            T.op("dve", lambda e, pi=pi, hf=hf: e.tensor_tensor(out=xnew[:, t, hf * 512:(hf + 1) * 512], in0=PS[pi][:, :], in1=xb[:, hf * 512:(hf + 1) * 512], op=ALU.add),
                 reads=["ps%d" % pi, xk], writes=["xnew%d" % t])
    def wo_b(t):
        norm_a(t, xnew[:, t, :], "xnew%d" % t, xn2[t % 2], "xn2_%d" % (t % 2), "rstd2_%d" % t)
    def wo_c(t):
        norm_b(xn2[t % 2], "xn2_%d" % (t % 2), lambda kc, t=t: hx2T[:, kc, t * 128:(t + 1) * 128], ["hx2T"] * 8, 16)
    for t in range(3):
        wo_dma(t)
    for k in range(16 + 2):
        if k + 3 < 16:
            wo_dma(k + 3)
        if k < 16:
            wo_a(k)
        if 1 <= k < 17:
            wo_b(k - 1)
        if k >= 2:
            wo_c(k - 2)
    if stop_after == "wout":
        dbg_out["xnew"] = (xnew, [128, 16, D], F32)
        return finish(nc, T, dbg_out, out)
    T.barrier()

    WB = 117888
    wexp = [[sbat("wg%d" % i, [128, 8, 512], BF16, WB + i * 24576),
             sbat("wu%d" % i, [128, 8, 512], BF16, WB + i * 24576 + 8192),
             sbat("wd%d" % i, [128, 4, D], BF16, WB + i * 24576 + 16384)] for i in range(2)]
    hidT = [sbat("hidT%d" % i, [128, 4, 512], BF16, WB + 49152 + i * 4096) for i in range(2)]
    g2b = sbat("g2b", [128, D], F32, WB + 57344)
    gfb = sbat("gfb", [128, D], F32, WB + 61440)
    assert WB + 65536 <= TAIL
    cur[0] = TMP
    wr_b = sb("wr_b", [128, 8, 20], BF16)
    brb = sb("brb", [128, 20], F32)
    lg = sb("lg", [128, 16, 20], F32)
    comb = sb("comb", [128, 16, 16], F32)
    rt = {n_: sb("rt_" + n_, [128, 16, 4], F32) for n_ in ("GE", "OH", "EL", "MK", "SEL", "EX", "TM")}
    rv = {n_: sb("rv_" + n_, [128, 16], F32) for n_ in ("GM", "SG", "GW", "M1", "M2", "SE", "RS", "WS")}
    sg = sb("sg", [128, 4, 512], BF16)
    ofin = [sb("ofin%d" % i, [128, D], F32) for i in range(2)]
    assert cur[0] <= A0, cur[0]
    T.dma("pool", wr_b[:], w_r.rearrange("(kc p) c -> p kc c", p=128), writes=["wr_b"])
    T.dma("sp", brb[:], b_r[None, :].to_broadcast([128, 20]), writes=["brb"])
    T.dma("sp", g2b[:], gscr[1:2, :].to_broadcast([128, D]), reads=["gscr_r"], writes=["g2b"], key="g2b")
    T.dma("sp", gfb[:], g_fin[None, :].to_broadcast([128, D]), writes=["gfb"])
    pr = psn(0, 4)
    for t in range(16):
        T.group("pe", [lambda e, kc=kc, t=t: e.matmul(PS[pr][:, t * 20:(t + 1) * 20], lhsT=hx2T[:, kc, t * 128:(t + 1) * 128], rhs=wr_b[:, kc, :], start=(kc == 0), stop=(kc == 7))
                       for kc in range(8)], reads=["hx2T", "wr_b"], writes=["ps%d" % pr])
    AXX = mybir.AxisListType.X
    def V(fn, rd, wr):
        T.op("dve", fn, reads=rd, writes=wr)
    def B4(ap):
        return ap.unsqueeze(2).to_broadcast([128, 16, 4])
    GL = lg[:, :, 0:4]
    V(lambda e: e.tensor_tensor(out=lg[:], in0=PS[pr][:, 0:320].rearrange("p (t c) -> p t c", c=20), in1=brb[:].unsqueeze(1).to_broadcast([128, 16, 20]), op=ALU.add),
      ["ps%d" % pr, "brb"], ["lg"])
    V(lambda e: e.reduce_max(out=rv["GM"][:], in_=GL, axis=AXX), ["lg"], ["GM"])
    V(lambda e: e.tensor_tensor(out=rt["GE"][:], in0=GL, in1=B4(rv["GM"][:]), op=ALU.subtract), ["lg", "GM"], ["GE"])
    T.op("act", lambda e: e.activation(out=rt["GE"][:], in_=rt["GE"][:], func=AF.Exp), reads=["GE"], writes=["GE"])
    V(lambda e: e.reduce_sum(out=rv["SG"][:], in_=rt["GE"][:], axis=AXX), ["GE"], ["SG"])
    V(lambda e: e.reciprocal(out=rv["GW"][:], in_=rv["SG"][:]), ["SG"], ["GW"])
    V(lambda e: e.tensor_tensor(out=rt["OH"][:], in0=GL, in1=B4(rv["GM"][:]), op=ALU.is_equal), ["lg", "GM"], ["OH"])
    for g in range(4):
        dst = rt["EL"] if g == 0 else rt["TM"]
        V(lambda e, g=g, dst=dst: e.tensor_tensor(out=dst[:], in0=lg[:, :, 4 + 4 * g:8 + 4 * g], in1=rt["OH"][:, :, g:g + 1].to_broadcast([128, 16, 4]), op=ALU.mult),
          ["lg", "OH"], ["EL" if g == 0 else "TM"])
        if g > 0:
            V(lambda e: e.tensor_tensor(out=rt["EL"][:], in0=rt["EL"][:], in1=rt["TM"][:], op=ALU.add), ["EL", "TM"], ["EL"])
    V(lambda e: e.reduce_max(out=rv["M1"][:], in_=rt["EL"][:], axis=AXX), ["EL"], ["M1"])
    V(lambda e: e.tensor_tensor(out=rt["MK"][:], in0=rt["EL"][:], in1=B4(rv["M1"][:]), op=ALU.is_equal), ["EL", "M1"], ["MK"])
    V(lambda e: e.scalar_tensor_tensor(out=rt["MK"][:], in0=rt["MK"][:], scalar=NEG, in1=rt["EL"][:], op0=ALU.mult, op1=ALU.add), ["MK", "EL"], ["MK"])
    V(lambda e: e.reduce_max(out=rv["M2"][:], in_=rt["MK"][:], axis=AXX), ["MK"], ["M2"])
    V(lambda e: e.tensor_tensor(out=rt["SEL"][:], in0=rt["EL"][:], in1=B4(rv["M2"][:]), op=ALU.is_ge), ["EL", "M2"], ["SEL"])
    V(lambda e: e.tensor_tensor(out=rt["EX"][:], in0=rt["EL"][:], in1=B4(rv["M1"][:]), op=ALU.subtract), ["EL", "M1"], ["EX"])
    T.op("act", lambda e: e.activation(out=rt["EX"][:], in_=rt["EX"][:], func=AF.Exp), reads=["EX"], writes=["EX"])
    V(lambda e: e.tensor_tensor(out=rt["EX"][:], in0=rt["EX"][:], in1=rt["SEL"][:], op=ALU.mult), ["EX", "SEL"], ["EX"])
    V(lambda e: e.reduce_sum(out=rv["SE"][:], in_=rt["EX"][:], axis=AXX), ["EX"], ["SE"])
    V(lambda e: e.reciprocal(out=rv["RS"][:], in_=rv["SE"][:]), ["SE"], ["RS"])
    V(lambda e: e.tensor_tensor(out=rv["WS"][:], in0=rv["RS"][:], in1=rv["GW"][:], op=ALU.mult), ["RS", "GW"], ["WS"])
    V(lambda e: e.tensor_tensor(out=rt["EX"][:], in0=rt["EX"][:], in1=B4(rv["WS"][:]), op=ALU.mult), ["EX", "WS"], ["EX"])
    for g in range(4):
        V(lambda e, g=g: e.tensor_tensor(out=comb[:, :, 4 * g:4 * g + 4], in0=rt["EX"][:], in1=rt["OH"][:, :, g:g + 1].to_broadcast([128, 16, 4]), op=ALU.mult),
          ["EX", "OH"], ["comb"])

    msteps = [(ex, s_) for ex in range(16) for s_ in range(4)]
    def moe_gu(i):
        ex, s_ = msteps[i]
        wg, wu, wd = wexp[ex % 2]; wk = "wexp%d" % (ex % 2)
        if s_ == 0:
            T.dma("pool", wg[:], w_gate[ex].rearrange("(kc p) f -> p kc f", p=128), writes=[wk + "g"])
            T.dma("pool", wu[:], w_up[ex].rearrange("(kc p) f -> p kc f", p=128), writes=[wk + "u"])
            T.dma("pool", wd[:], w_down[ex].rearrange("(fc p) d -> p fc d", p=128), writes=[wk + "d"])
            T.op("pool", lambda e: e.tensor_tensor(out=wd[:], in0=wd[:], in1=g2b[:].unsqueeze(1).to_broadcast([128, 4, D]), op=ALU.mult),
                 reads=[wk + "d", "g2b"], writes=[wk + "d"])
        hT = hidT[i % 2]; hk2 = "hidT%d" % (i % 2)
        tsl = slice(s_ * 512, (s_ + 1) * 512)
        for fc in range(4):
            pg = psn(0, 4); pu = psn(0, 4)
            T.group("pe", [lambda e, kc=kc, pg=pg, fc=fc: e.matmul(PS[pg][:, :], lhsT=wg[:, kc, fc * 128:(fc + 1) * 128], rhs=hx2T[:, kc, tsl], start=(kc == 0), stop=(kc == 7))
                           for kc in range(8)], reads=["hx2T", wk + "g"], writes=["ps%d" % pg])
            T.group("pe", [lambda e, kc=kc, pu=pu, fc=fc: e.matmul(PS[pu][:, :], lhsT=wu[:, kc, fc * 128:(fc + 1) * 128], rhs=hx2T[:, kc, tsl], start=(kc == 0), stop=(kc == 7))
                           for kc in range(8)], reads=["hx2T", wk + "u"], writes=["ps%d" % pu])
            T.op("act", lambda e, pg=pg, fc=fc: e.activation(out=sg[:, fc, :], in_=PS[pg][:, :], func=AF.Silu), reads=["ps%d" % pg], writes=["sg%d" % fc])
            T.op("dve", lambda e, pu=pu, fc=fc, hT=hT: e.tensor_tensor(out=hT[:, fc, :], in0=PS[pu][:, :], in1=sg[:, fc, :], op=ALU.mult),
                 reads=["ps%d" % pu, "sg%d" % fc], writes=[hk2 + "_%d" % fc])
    def moe_down(i):
        ex, s_ = msteps[i]
        wg, wu, wd = wexp[ex % 2]; wk = "wexp%d" % (ex % 2)
        hT = hidT[i % 2]; hk2 = "hidT%d" % (i % 2)
        for tt in range(4):
            t = 4 * s_ + tt
            for hf in range(2):
                py = psn(4, 7)
                T.group("pe", [lambda e, fc=fc, py=py, tt=tt, hf=hf: e.matmul(PS[py][:, :], lhsT=hT[:, fc, tt * 128:(tt + 1) * 128], rhs=wd[:, fc, hf * 512:(hf + 1) * 512],
                                                                         start=(fc == 0), stop=(fc == 3)) for fc in range(4)],
                        reads=[hk2 + "_%d" % fc for fc in range(4)] + [wk + "d"], writes=["ps%d" % py])
                T.op("dve", lambda e, py=py, hf=hf, t=t: e.scalar_tensor_tensor(out=xnew[:, t, hf * 512:(hf + 1) * 512], in0=PS[py][:, :], scalar=comb[:, t, ex:ex + 1],
                                                                         in1=xnew[:, t, hf * 512:(hf + 1) * 512], op0=ALU.mult, op1=ALU.add),
                     reads=["ps%d" % py, "comb", "xnew%d" % t], writes=["xnew%d" % t])
    for i in range(len(msteps) + 1):
        if i < len(msteps):
            moe_gu(i)
        if i >= 1:
            moe_down(i - 1)

    for t in range(16):
        ob = ofin[t % 2]; ok = "ofin%d" % (t % 2)
        T.op("act", lambda e, ob=ob, t=t: e.activation(out=ob[:], in_=xnew[:, t, :], func=AF.Square, accum_out=ss[:, 32 + t:33 + t]), reads=["xnew%d" % t], writes=[ok, "ss3_%d" % t])
        T.op("act", lambda e, t=t: e.activation(out=sd[:, 32 + t:33 + t], in_=ss[:, 32 + t:33 + t], func=AF.Sqrt, bias=epsc[:], scale=1.0 / D), reads=["ss3_%d" % t, "epsc"], writes=["sd3_%d" % t])
        T.op("dve", lambda e, t=t: e.reciprocal(out=rstd[:, 32 + t:33 + t], in_=sd[:, 32 + t:33 + t]), reads=["sd3_%d" % t], writes=["rstd3_%d" % t])
        T.op("dve", lambda e, ob=ob, t=t: e.scalar_tensor_tensor(out=ob[:], in0=xnew[:, t, :], scalar=rstd[:, 32 + t:33 + t], in1=gfb[:], op0=ALU.mult, op1=ALU.mult),
             reads=["xnew%d" % t, "rstd3_%d" % t, "gfb"], writes=[ok])
        T.dma("sp", out[t * 128:(t + 1) * 128, :], ob[:], reads=[ok], key="outst%d" % (t % 2))
    return finish(nc, T, dbg_out, out)


def finish(nc, T, dbg_out, out):
    for name, (tens, shape, dt) in dbg_out.items():
        d = nc.dram_tensor("dbg_" + name, list(shape), dt, kind="ExternalOutput").ap()
        idx = tuple(slice(None) for _ in shape)
        T.dma("sp", d[idx], tens[idx], reads=[name], key="dbg_" + name)
    T.finish()
    return nc


GRID_W = 64
_NC_CACHE = {}


def _rope_tables(rows, cols):
    half = 16
    inv_freq = (10000.0 ** (-np.arange(0, half, 2, dtype=np.float32) / half)).astype(np.float32)
    ang = np.concatenate([rows.astype(np.float32)[:, None] * inv_freq, cols.astype(np.float32)[:, None] * inv_freq], axis=-1)
    return np.cos(ang).astype(np.float32), np.sin(ang).astype(np.float32)


def _core_layout(j):
    R0 = 32 * j
    tok = np.full(NSLOT, -1, np.int64)
    tok[0:2048] = np.arange(R0 * 64, (R0 + 32) * 64)
    used = np.zeros(8192, bool); used[R0 * 64:(R0 + 32) * 64] = True
    for i, r in enumerate(list(range(R0 - 4, R0)) + list(range(R0 + 32, R0 + 36))):
        if 0 <= r < 128:
            tok[2048 + i * 64:2048 + (i + 1) * 64] = np.arange(r * 64, (r + 1) * 64)
            used[r * 64:(r + 1) * 64] = True
    tok[2560:2816] = -2 - np.arange(256)
    others = np.nonzero(~used)[0]
    free_halo = np.nonzero(tok[2048:2560] == -1)[0] + 2048
    nfill = len(free_halo)
    if nfill:
        tok[free_halo] = others[len(others) - nfill:]
        others = others[:len(others) - nfill]
    assert len(others) == 5632, len(others)
    tok[2816:2816 + len(others)] = others
    assert (tok[:8448] != -1).all() and (tok[8448:] == -1).all()
    return tok


def _host_inputs(inp):
    x = np.asarray(inp["x"], np.float32); ctx = np.asarray(inp["ctx"], np.float32)
    w_in = np.ascontiguousarray(np.asarray(inp["w_in"], np.float32)[0])
    w_uq = np.ascontiguousarray(np.asarray(inp["w_uq"], np.float32)[0])
    perm = np.concatenate([np.arange(16, 32), np.arange(0, 16)])
    w_krs = np.ascontiguousarray(w_in[:, C_KR:C_KR + 32][:, perm])
    w_uqs = np.ascontiguousarray(w_uq.reshape(384, 8, 96)[:, :, 64:96][:, :, perm])
    rel = np.asarray(inp["na_rel_bias"], np.float32)[0]
    a = np.arange(2)[:, None, None, None]; ck = np.arange(64)[None, :, None, None]
    i = np.arange(8)[None, None, :, None]; cq = np.arange(64)[None, None, None, :]
    cstart = np.clip(cq - 8, 0, 48)
    col_in = (ck >= cstart) & (ck < cstart + 16)
    dc = ck - cq + 15
    nab = np.full((8, 8, 2, 64, 8, 64), NEG, np.float32)
    for c in range(8):
        dr = 2 * c + a - i + 3
        ok = (dr >= 0) & (dr <= 14) & col_in
        okb = np.broadcast_to(ok, (2, 64, 8, 64))
        drb = np.broadcast_to(np.clip(dr, 0, 14), (2, 64, 8, 64)); dcb = np.broadcast_to(np.clip(dc, 0, 30), (2, 64, 8, 64))
        for h in range(8):
            g = rel[h][drb, dcb]
            nab[h, c] = np.where(okb, g, np.float32(NEG))
    nab = np.ascontiguousarray(nab.reshape(8, 8, 128, 512))
    rowsel = np.zeros((16, 8, 2, 64), np.float32)
    for c in range(8):
        for aa in range(2):
            rowsel[2 * c + aa, c, aa, :] = 1.0
    rowsel = rowsel.reshape(16, 8, 128)
    common = dict(
        w_mod=np.ascontiguousarray(np.asarray(inp["w_mod"], np.float32)[0]), b_mod=np.ascontiguousarray(np.asarray(inp["b_mod"], np.float32)[0]),
        g_attn=np.ascontiguousarray(np.asarray(inp["norm_attn_g"], np.float32)[0]), g_ffn=np.ascontiguousarray(np.asarray(inp["norm_ffn_g"], np.float32)[0]),
        g_fin=np.asarray(inp["final_norm_g"], np.float32), w_in=w_in, w_krs=w_krs,
        gq=np.ascontiguousarray(np.asarray(inp["q_a_norm_g"], np.float32)[0]), gkv=np.ascontiguousarray(np.asarray(inp["kv_a_norm_g"], np.float32)[0]),
        w_uq=w_uq, w_uqs=w_uqs, w_ukv=np.ascontiguousarray(np.asarray(inp["w_ukv"], np.float32)[0]),
        w_out=np.ascontiguousarray(np.asarray(inp["w_out"], np.float32)[0]),
        w_r=np.ascontiguousarray(np.concatenate([np.asarray(inp["w_router_group"], np.float32)[0], np.asarray(inp["w_router_expert"], np.float32)[0]], axis=1)),
        b_r=np.ascontiguousarray(np.concatenate([np.asarray(inp["b_router_group"], np.float32)[0], np.asarray(inp["b_router_expert"], np.float32)[0]])),
        w_gate=np.ascontiguousarray(np.asarray(inp["w_gate"], np.float32)[0]), w_up=np.ascontiguousarray(np.asarray(inp["w_up"], np.float32)[0]),
        w_down=np.ascontiguousarray(np.asarray(inp["w_down"], np.float32)[0]),
        nab=nab,
    )
    maps = []
    for core in range(8):
        b, j = core // 4, core % 4
        R0 = 32 * j
        tok = _core_layout(j)
        xs = np.zeros((NSLOT, D), np.float32)
        m = tok >= 0
        xs[m] = x[b][tok[m]]
        xs[2560:2816] = ctx[b]
        kmask = np.where(tok == -1, np.float32(NEG), np.float32(0)).astype(np.float32).reshape(NT, 128).T.copy()
        rows = np.where(m, tok // GRID_W, 0); cols = np.where(m, tok % GRID_W, 0)
        cos, sin = _rope_tables(rows, cols)
        cos[~m] = 1.0; sin[~m] = 0.0
        ctab = np.ascontiguousarray(np.concatenate([cos, cos], axis=1).T)
        stab = np.ascontiguousarray(np.concatenate([sin, sin], axis=1).T)
        narm = np.full((16, 4, 8, 64), NEG, np.float32)
        for g in range(4):
            for i_ in range(8):
                r = R0 + 8 * g + i_
                start = min(max(r - 4, 0), 120)
                for lp in range(16):
                    kr = R0 - 4 + 8 * g + lp
                    if start <= kr < start + 8 and 0 <= kr < 128:
                        narm[lp, g, i_, :] = 0.0
        d = dict(common)
        RM = narm[:, :, :, 0]
        rmc = np.zeros((128, 4, 8, 8), np.float32)
        for c_ in range(8):
            for a_ in range(2):
                rmc[a_ * 64:(a_ + 1) * 64, :, c_, :] = RM[2 * c_ + a_][None, :, :]
        d.update(xs=xs, rmc=np.ascontiguousarray(rmc.reshape(128, 4, 64)), cvec=np.ascontiguousarray(np.stack([np.asarray(inp["c"], np.float32)[b], np.asarray(inp["c_ctx"], np.float32)])),
                 kmask=kmask, ctab=ctab, stab=stab)
        maps.append(d)
    return maps


def kernel(**inputs):
    if "nc" not in _NC_CACHE:
        _NC_CACHE["nc"] = build()
    nc = _NC_CACHE["nc"]
    maps = _host_inputs(inputs)
    res = run_bass_kernel_spmd(nc, maps, core_ids=list(range(8)))
    out = np.zeros((2, 8192, D), np.float32)
    for core in range(8):
        b, j = core // 4, core % 4
        out[b, j * 2048:(j + 1) * 2048] = res.results[core]["out"]
    return out
```
